# Optimizing a Trainium2 kernel written in Bass

```python
import math
import jax
import jax.numpy as jnp
from jax import lax
import numpy as np

D_MODEL = 1024
BATCH = 4
SEQ = 8192
DEPTH = 2

CTX_LEN = 256
GRID_W = 64
HEAD_DIM = 64
NA_HEADS = 4
NA_ROWS = 8
NA_COLS = 16
SWA_HEADS = 4
SWA_KV_HEADS = 2
SWA_WINDOW = 128
SWA_BLOCK = 128
GMLP_WIDTH = 256
GMLP_GROUPS = 4
GMLP_CHUNK = 128
S5_WIDTH = 256
S5_GROUP = 16
S5_GROUPS = S5_WIDTH // S5_GROUP
S5_STATE = 64
N_BRANCH = 4
BRANCH_WIDTH = 256
FFN_DIM = 2816
N_EXPERTS = 8
TOP_K = 2
EXPERT_DIM = 3584
N_DENSE = (DEPTH + 1) // 2
N_MOE = DEPTH // 2
ROPE_BASE = 10000.0
EPS = 1e-6
NEG_INF = -1e30
F32 = jnp.float32

COL_SIZES = (NA_HEADS * HEAD_DIM, NA_HEADS * HEAD_DIM, NA_HEADS * HEAD_DIM,
             SWA_HEADS * HEAD_DIM, SWA_KV_HEADS * HEAD_DIM, SWA_KV_HEADS * HEAD_DIM,
             GMLP_WIDTH, GMLP_WIDTH, S5_WIDTH, N_BRANCH * D_MODEL)
COL_SPLITS = tuple(int(v) for v in np.cumsum(COL_SIZES)[:-1])
IN_COLS = int(sum(COL_SIZES))

kernel_name = 'hybrid_gated_mixers_diffusion_block'


def rmsnorm(x, g):
    x32 = x.astype(F32)
    y = x32 * lax.rsqrt(jnp.mean(x32 * x32, axis=-1, keepdims=True) + EPS)
    return (y * g.astype(F32)).astype(x.dtype)


def layernorm(x, g, b):
    x32 = x.astype(F32)
    mu = jnp.mean(x32, axis=-1, keepdims=True)
    xc = x32 - mu
    y = xc * lax.rsqrt(jnp.mean(xc * xc, axis=-1, keepdims=True) + EPS)
    return (y * g.astype(F32) + b.astype(F32)).astype(x.dtype)


def heads(t, n):
    return t.reshape(t.shape[0], t.shape[1], n, HEAD_DIM)


def axial_rope(x, rows, cols):
    nq = HEAD_DIM // 4
    inv = ROPE_BASE ** (-jnp.arange(nq, dtype=F32) / nq)

    def rot(xp, pos):
        ang = pos.astype(F32)[:, None] * inv[None, :]
        cos = jnp.cos(ang)[None, :, None, :]
        sin = jnp.sin(ang)[None, :, None, :]
        x1, x2 = xp[..., :nq], xp[..., nq:]
        return jnp.concatenate([x1 * cos - x2 * sin, x1 * sin + x2 * cos], axis=-1)

    half = HEAD_DIM // 2
    return jnp.concatenate([rot(x[..., :half], rows), rot(x[..., half:], cols)], axis=-1).astype(x.dtype)


def ctx_attn(q, k, v):
    b, l, h, hd = q.shape
    s = jnp.einsum('blhd,bmhd->bhlm', q, k).astype(F32) * (HEAD_DIM ** -0.5)
    p = jax.nn.softmax(s, axis=-1).astype(v.dtype)
    return jnp.einsum('bhlm,bmhd->blhd', p, v).reshape(b, l, h * hd)


def neighbourhood_attn(q, k, v, kc, vc, rpb):
    b, s, h, hd = q.shape
    rows = s // GRID_W
    wr = min(NA_ROWS, rows)
    n_win = wr * NA_COLS
    scale = HEAD_DIM ** -0.5
    qg = q.reshape(b, rows, GRID_W, h, hd)
    kg = k.reshape(b, rows, GRID_W, h, hd)
    vg = v.reshape(b, rows, GRID_W, h, hd)
    col = jnp.arange(GRID_W)
    col_start = jnp.clip(col - NA_COLS // 2, 0, GRID_W - NA_COLS)
    col_idx = col_start[:, None] + jnp.arange(NA_COLS)[None, :]
    bias_c = rpb.astype(F32)[:, :, col_idx - col[:, None] + NA_COLS - 1]

    def row_block(r):
        rs = jnp.clip(r - wr // 2, 0, rows - wr)
        q_r = lax.dynamic_index_in_dim(qg, r, axis=1, keepdims=False)
        k_w = lax.dynamic_slice_in_dim(kg, rs, wr, axis=1)[:, :, col_idx]
        v_w = lax.dynamic_slice_in_dim(vg, rs, wr, axis=1)[:, :, col_idx]
        row_off = rs + jnp.arange(wr) - r + NA_ROWS - 1
        bias = jnp.transpose(bias_c[:, row_off], (0, 2, 1, 3))
        s_w = jnp.einsum('bchd,brckhd->bhcrk', q_r, k_w).astype(F32) * scale + bias[None]
        s_c = jnp.einsum('bchd,blhd->bhcl', q_r, kc).astype(F32) * scale
        p = jax.nn.softmax(jnp.concatenate([s_w.reshape(b, h, GRID_W, n_win), s_c], axis=-1), axis=-1)
        p_w = p[..., :n_win].reshape(b, h, GRID_W, wr, NA_COLS).astype(v.dtype)
        p_c = p[..., n_win:].astype(v.dtype)
        return jnp.einsum('bhcrk,brckhd->bchd', p_w, v_w) + jnp.einsum('bhcl,blhd->bchd', p_c, vc)

    out = lax.map(row_block, jnp.arange(rows))
    return jnp.transpose(out, (1, 0, 2, 3, 4)).reshape(b, s, h * hd)


def window_gqa(q, k, v, kc, vc, sink):
    b, s, hq, hd = q.shape
    hkv = k.shape[2]
    g = hq // hkv
    nb = s // SWA_BLOCK
    scale = HEAD_DIM ** -0.5
    qb = q.reshape(b, nb, SWA_BLOCK, hkv, g, hd)
    pad = ((0, 0), (SWA_BLOCK, SWA_BLOCK), (0, 0), (0, 0))
    kp = jnp.pad(k, pad).reshape(b, nb + 2, SWA_BLOCK, hkv, hd)
    vp = jnp.pad(v, pad).reshape(b, nb + 2, SWA_BLOCK, hkv, hd)
    k_band = jnp.concatenate([kp[:, :-2], kp[:, 1:-1], kp[:, 2:]], axis=2)
    v_band = jnp.concatenate([vp[:, :-2], vp[:, 1:-1], vp[:, 2:]], axis=2)
    n_band = 3 * SWA_BLOCK
    blk = jnp.arange(nb)[:, None, None]
    qpos = blk * SWA_BLOCK + jnp.arange(SWA_BLOCK)[None, :, None]
    kpos = (blk - 1) * SWA_BLOCK + jnp.arange(n_band)[None, None, :]
    mask = (jnp.abs(kpos - qpos) <= SWA_WINDOW) & (kpos >= 0) & (kpos < s)
    s_w = jnp.einsum('bnqhgd,bnkhd->bnhgqk', qb, k_band).astype(F32) * scale
    s_w = jnp.where(mask[None, :, None, None], s_w, NEG_INF)
    s_c = jnp.einsum('bnqhgd,blhd->bnhgql', qb, kc).astype(F32) * scale
    sink_col = jnp.broadcast_to(sink.astype(F32).reshape(hkv, g)[None, None, :, :, None, None],
                                (b, nb, hkv, g, SWA_BLOCK, 1))
    p = jax.nn.softmax(jnp.concatenate([s_w, s_c, sink_col], axis=-1), axis=-1)
    n_ctx = kc.shape[1]
    p_w = p[..., :n_band].astype(v.dtype)
    p_c = p[..., n_band:n_band + n_ctx].astype(v.dtype)
    o = jnp.einsum('bnhgqk,bnkhd->bnqhgd', p_w, v_band) + jnp.einsum('bnhgql,blhd->bnqhgd', p_c, vc)
    return o.reshape(b, s, hq * hd)


def ctx_gqa(qc, kc, vc, sink):
    b, l, hq, hd = qc.shape
    hkv = kc.shape[2]
    g = hq // hkv
    qg = qc.reshape(b, l, hkv, g, hd)
    s = jnp.einsum('blhgd,bmhd->bhglm', qg, kc).astype(F32) * (HEAD_DIM ** -0.5)
    sink_col = jnp.broadcast_to(sink.astype(F32).reshape(hkv, g)[None, :, :, None, None], (b, hkv, g, l, 1))
    p = jax.nn.softmax(jnp.concatenate([s, sink_col], axis=-1), axis=-1)[..., :l].astype(vc.dtype)
    return jnp.einsum('bhglm,bmhd->blhgd', p, vc).reshape(b, l, hq * hd)


def chunk_gmlp(u, v, ln_g, ln_b, ws, bs):
    b, n, _ = u.shape
    u = jax.nn.gelu(u)
    v = layernorm(jax.nn.gelu(v), ln_g, ln_b)
    vg = v.reshape(b, n // GMLP_CHUNK, GMLP_CHUNK, GMLP_GROUPS, GMLP_WIDTH // GMLP_GROUPS)
    sg = jnp.einsum('gij,bnjgc->bnigc', ws, vg) + bs.T[None, None, :, :, None]
    return u * sg.reshape(b, n, GMLP_WIDTH)


def s5_discretize(a_re, a_im, log_dt, b_re, b_im):
    lam_re = jnp.minimum(a_re.astype(F32), -1e-4)
    lam_im = a_im.astype(F32)
    dt = jnp.exp(log_dt.astype(F32))[:, None]
    mag = jnp.exp(lam_re * dt)
    ab_re = mag * jnp.cos(lam_im * dt)
    ab_im = mag * jnp.sin(lam_im * dt)
    den = lam_re * lam_re + lam_im * lam_im
    k_re = ((ab_re - 1.0) * lam_re + ab_im * lam_im) / den
    k_im = (ab_im * lam_re - (ab_re - 1.0) * lam_im) / den
    br, bi = b_re.astype(F32), b_im.astype(F32)
    bb_re = k_re[..., None] * br - k_im[..., None] * bi
    bb_im = k_re[..., None] * bi + k_im[..., None] * br
    return ab_re, ab_im, bb_re, bb_im


def s5_scan(u, ab_re, ab_im, bb_re, bb_im, h0_re, h0_im, reverse):
    bu_re = jnp.einsum('bngc,gpc->bngp', u, bb_re)
    bu_im = jnp.einsum('bngc,gpc->bngp', u, bb_im)
    if h0_re is not None:
        first = -1 if reverse else 0
        bu_re = bu_re.at[:, first].add(ab_re * h0_re - ab_im * h0_im)
        bu_im = bu_im.at[:, first].add(ab_re * h0_im + ab_im * h0_re)
    a_re = jnp.broadcast_to(ab_re, bu_re.shape)
    a_im = jnp.broadcast_to(ab_im, bu_im.shape)

    def combine(e1, e2):
        a1r, a1i, b1r, b1i = e1
        a2r, a2i, b2r, b2i = e2
        return (a2r * a1r - a2i * a1i, a2r * a1i + a2i * a1r,
                a2r * b1r - a2i * b1i + b2r, a2r * b1i + a2i * b1r + b2i)

    _, _, x_re, x_im = lax.associative_scan(combine, (a_re, a_im, bu_re, bu_im), reverse=reverse, axis=1)
    return x_re, x_im


def s5_readout(x_re, x_im, c_re, c_im):
    return (jnp.einsum('bngp,gcp->bngc', x_re, c_re.astype(F32))
            - jnp.einsum('bngp,gcp->bngc', x_im, c_im.astype(F32)))


def s5_glu(y, w, b):
    y = jax.nn.gelu(y)
    return y * jax.nn.sigmoid(y @ w + b)


def s5_mixer(ux, uc, a_re, a_im, log_dt, b_re, b_im, c_re, c_im, d, glu_w, glu_b, ctx_out):
    bsz, s, _ = ux.shape
    l = uc.shape[1]
    ug_x = ux.astype(F32).reshape(bsz, s, S5_GROUPS, S5_GROUP)
    ug_c = uc.astype(F32).reshape(bsz, l, S5_GROUPS, S5_GROUP)
    y_x = d.astype(F32) * ux.astype(F32)
    y_c = d.astype(F32) * uc.astype(F32)
    for direction, rev in enumerate((False, True)):
        ab_re, ab_im, bb_re, bb_im = s5_discretize(a_re[direction], a_im[direction], log_dt[direction],
                                                   b_re[direction], b_im[direction])
        xc_re, xc_im = s5_scan(ug_c, ab_re, ab_im, bb_re, bb_im, None, None, rev)
        end = 0 if rev else -1
        xx_re, xx_im = s5_scan(ug_x, ab_re, ab_im, bb_re, bb_im, xc_re[:, end], xc_im[:, end], rev)
        y_x = y_x + s5_readout(xx_re, xx_im, c_re[direction], c_im[direction]).reshape(bsz, s, S5_WIDTH)
        if ctx_out:
            y_c = y_c + s5_readout(xc_re, xc_im, c_re[direction], c_im[direction]).reshape(bsz, l, S5_WIDTH)
    out_x = s5_glu(y_x.astype(ux.dtype), glu_w, glu_b)
    out_c = s5_glu(y_c.astype(uc.dtype), glu_w, glu_b) if ctx_out else None
    return out_x, out_c


def merge_branches(outs, gates, w_branch, w_out):
    dm = w_out.shape[0]
    m = None
    for i, o in enumerate(outs):
        term = jax.nn.sigmoid(gates[..., i * dm:(i + 1) * dm]) * (o @ w_branch[i])
        m = term if m is None else m + term
    return m @ w_out


def token_mixers(zx, zc, rows_pos, cols_pos, rpb, sink, gln_g, gln_b, gws, gbs,
                 a_re, a_im, log_dt, b_re, b_im, c_re, c_im, d, glu_w, glu_b, w_branch, w_out, ctx_out):
    qa, ka, va, qd, kd, vd, gu, gv, su, gates = jnp.split(zx, COL_SPLITS, axis=-1)
    qa_c, ka_c, va_c, qd_c, kd_c, vd_c, gu_c, gv_c, su_c, gates_c = jnp.split(zc, COL_SPLITS, axis=-1)
    ka_ch, va_ch = heads(ka_c, NA_HEADS), heads(va_c, NA_HEADS)
    kd_ch, vd_ch = heads(kd_c, SWA_KV_HEADS), heads(vd_c, SWA_KV_HEADS)
    o_a = neighbourhood_attn(heads(qa, NA_HEADS), heads(ka, NA_HEADS), heads(va, NA_HEADS), ka_ch, va_ch, rpb)
    o_b = chunk_gmlp(gu, gv, gln_g, gln_b, gws, gbs)
    o_cx, o_cc = s5_mixer(su, su_c, a_re, a_im, log_dt, b_re, b_im, c_re, c_im, d, glu_w, glu_b, ctx_out)
    q_d = axial_rope(heads(qd, SWA_HEADS), rows_pos, cols_pos)
    k_d = axial_rope(heads(kd, SWA_KV_HEADS), rows_pos, cols_pos)
    o_d = window_gqa(q_d, k_d, heads(vd, SWA_KV_HEADS), kd_ch, vd_ch, sink)
    out_x = merge_branches((o_a, o_b, o_cx, o_d), gates, w_branch, w_out)
    out_c = None
    if ctx_out:
        o_a_c = ctx_attn(heads(qa_c, NA_HEADS), ka_ch, va_ch)
        o_b_c = chunk_gmlp(gu_c, gv_c, gln_g, gln_b, gws, gbs)
        o_d_c = ctx_gqa(heads(qd_c, SWA_HEADS), kd_ch, vd_ch, sink)
        out_c = merge_branches((o_a_c, o_b_c, o_cc, o_d_c), gates_c, w_branch, w_out)
    return out_x, out_c


def swiglu(h, w1, w3, w2):
    return (jax.nn.silu(h @ w1) * (h @ w3)) @ w2


def moe_ffn(h, router, w1, w3, w2):
    shp = h.shape
    hf = h.reshape(-1, shp[-1])
    logits = (hf @ router).astype(F32)
    top_v, top_i = lax.top_k(logits, TOP_K)
    wts = jax.nn.softmax(top_v, axis=-1)
    comb = jnp.sum(jax.nn.one_hot(top_i, N_EXPERTS, dtype=F32) * wts[..., None], axis=1).astype(h.dtype)
    y = None
    for e in range(N_EXPERTS):
        term = comb[:, e:e + 1] * swiglu(hf, w1[e], w3[e], w2[e])
        y = term if y is None else y + term
    return y.reshape(shp)


def setup_inputs(seed: int = 0) -> dict:
    key = jax.random.key(seed)
    ks = iter(jax.random.split(key, 48))
    dm = D_MODEL

    def nrm(shape, scale):
        return jax.random.normal(next(ks), shape, F32) * scale

    n_idx = jnp.arange(S5_STATE, dtype=F32)
    return {
        'x': nrm((BATCH, SEQ, dm), 1.0),
        'c': nrm((BATCH, dm), 1.0),
        'ctx': nrm((BATCH, CTX_LEN, dm), 1.0),
        'c_ctx': nrm((dm,), 1.0),
        'w_mod': nrm((DEPTH, dm, 6 * dm), 0.5 * dm ** -0.5),
        'b_mod': nrm((DEPTH, 6 * dm), 0.02),
        'g_pre_mix': 1.0 + nrm((DEPTH, dm), 0.02),
        'g_post_mix': 1.0 + nrm((DEPTH, dm), 0.02),
        'g_pre_ffn': 1.0 + nrm((DEPTH, dm), 0.02),
        'g_post_ffn': 1.0 + nrm((DEPTH, dm), 0.02),
        'w_in': nrm((DEPTH, dm, IN_COLS), dm ** -0.5),
        'na_rpb': nrm((DEPTH, NA_HEADS, 2 * NA_ROWS - 1, 2 * NA_COLS - 1), 0.1),
        'swa_sink': nrm((DEPTH, SWA_HEADS), 0.5),
        'gmlp_ln_g': 1.0 + nrm((DEPTH, GMLP_WIDTH), 0.02),
        'gmlp_ln_b': nrm((DEPTH, GMLP_WIDTH), 0.02),
        'gmlp_ws': nrm((DEPTH, GMLP_GROUPS, GMLP_CHUNK, GMLP_CHUNK), GMLP_CHUNK ** -0.5),
        'gmlp_bs': 1.0 + nrm((DEPTH, GMLP_GROUPS, GMLP_CHUNK), 0.02),
        's5_a_re': -0.5 + nrm((DEPTH, 2, S5_GROUPS, S5_STATE), 0.01),
        's5_a_im': math.pi * n_idx + nrm((DEPTH, 2, S5_GROUPS, S5_STATE), 0.01),
        's5_log_dt': jax.random.uniform(next(ks), (DEPTH, 2, S5_GROUPS), F32, math.log(1e-3), math.log(1e-1)),
        's5_b_re': nrm((DEPTH, 2, S5_GROUPS, S5_STATE, S5_GROUP), (2 * S5_GROUP) ** -0.5),
        's5_b_im': nrm((DEPTH, 2, S5_GROUPS, S5_STATE, S5_GROUP), (2 * S5_GROUP) ** -0.5),
        's5_c_re': nrm((DEPTH, 2, S5_GROUPS, S5_GROUP, S5_STATE), S5_STATE ** -0.5),
        's5_c_im': nrm((DEPTH, 2, S5_GROUPS, S5_GROUP, S5_STATE), S5_STATE ** -0.5),
        's5_d': nrm((DEPTH, S5_WIDTH), 0.5),
        's5_glu_w': nrm((DEPTH, S5_WIDTH, S5_WIDTH), S5_WIDTH ** -0.5),
        's5_glu_b': nrm((DEPTH, S5_WIDTH), 0.02),
        'w_branch': nrm((DEPTH, N_BRANCH, BRANCH_WIDTH, dm), BRANCH_WIDTH ** -0.5),
        'w_out': nrm((DEPTH, dm, dm), dm ** -0.5),
        'ffn_w1': nrm((N_DENSE, dm, FFN_DIM), dm ** -0.5),
        'ffn_w3': nrm((N_DENSE, dm, FFN_DIM), dm ** -0.5),
        'ffn_w2': nrm((N_DENSE, FFN_DIM, dm), FFN_DIM ** -0.5),
        'moe_router': nrm((N_MOE, dm, N_EXPERTS), dm ** -0.5),
        'moe_w1': nrm((N_MOE, N_EXPERTS, dm, EXPERT_DIM), dm ** -0.5),
        'moe_w3': nrm((N_MOE, N_EXPERTS, dm, EXPERT_DIM), dm ** -0.5),
        'moe_w2': nrm((N_MOE, N_EXPERTS, EXPERT_DIM, dm), EXPERT_DIM ** -0.5),
    }


def reference(x, c, ctx, c_ctx, w_mod, b_mod, g_pre_mix, g_post_mix, g_pre_ffn, g_post_ffn, w_in,
              na_rpb, swa_sink, gmlp_ln_g, gmlp_ln_b, gmlp_ws, gmlp_bs,
              s5_a_re, s5_a_im, s5_log_dt, s5_b_re, s5_b_im, s5_c_re, s5_c_im, s5_d, s5_glu_w, s5_glu_b,
              w_branch, w_out, ffn_w1, ffn_w3, ffn_w2, moe_router, moe_w1, moe_w3, moe_w2):
    bsz, s, dm = x.shape
    t = jnp.arange(s)
    rows_pos = t // GRID_W
    cols_pos = t % GRID_W
    h_ctx = ctx
    for i in range(DEPTH):
        ctx_out = i < DEPTH - 1
        mod_x = (jax.nn.silu(c) @ w_mod[i] + b_mod[i]).reshape(bsz, 6, 1, dm)
        mod_c = (jax.nn.silu(c_ctx) @ w_mod[i] + b_mod[i]).reshape(6, dm)
        hx = rmsnorm(x, g_pre_mix[i]) * (1.0 + mod_x[:, 1]) + mod_x[:, 0]
        hc = rmsnorm(h_ctx, g_pre_mix[i]) * (1.0 + mod_c[1]) + mod_c[0]
        mx, mc = token_mixers(hx @ w_in[i], hc @ w_in[i], rows_pos, cols_pos, na_rpb[i], swa_sink[i],
                              gmlp_ln_g[i], gmlp_ln_b[i], gmlp_ws[i], gmlp_bs[i],
                              s5_a_re[i], s5_a_im[i], s5_log_dt[i], s5_b_re[i], s5_b_im[i],
                              s5_c_re[i], s5_c_im[i], s5_d[i], s5_glu_w[i], s5_glu_b[i],
                              w_branch[i], w_out[i], ctx_out)
        x = x + mod_x[:, 2] * rmsnorm(mx, g_post_mix[i])
        if ctx_out:
            h_ctx = h_ctx + mod_c[2] * rmsnorm(mc, g_post_mix[i])
        j = i // 2
        hx = rmsnorm(x, g_pre_ffn[i]) * (1.0 + mod_x[:, 4]) + mod_x[:, 3]
        if i % 2 == 0:
            fx = swiglu(hx, ffn_w1[j], ffn_w3[j], ffn_w2[j])
        else:
            fx = moe_ffn(hx, moe_router[j], moe_w1[j], moe_w3[j], moe_w2[j])
        x = x + mod_x[:, 5] * rmsnorm(fx, g_post_ffn[i])
        if ctx_out:
            hc = rmsnorm(h_ctx, g_pre_ffn[i]) * (1.0 + mod_c[4]) + mod_c[3]
            if i % 2 == 0:
                fc = swiglu(hc, ffn_w1[j], ffn_w3[j], ffn_w2[j])
            else:
                fc = moe_ffn(hc, moe_router[j], moe_w1[j], moe_w3[j], moe_w2[j])
            h_ctx = h_ctx + mod_c[5] * rmsnorm(fc, g_post_ffn[i])
    return x
```

```python
import contextlib
import math
import numpy as np
import ml_dtypes
ml_bf16 = ml_dtypes.bfloat16
import concourse.bass as bass
import concourse.mybir as mybir
from concourse.bass_utils import run_bass_kernel_spmd

F32 = mybir.dt.float32
BF16 = mybir.dt.bfloat16
AF = mybir.ActivationFunctionType
ALU = mybir.AluOpType
AX = mybir.AxisListType

NCORES = 8
DM = 1024
SEQ = 8192
BATCH = 4
CTX = 256
HALF = SEQ // 2
EPS = 1e-6


class Buf:
    __slots__ = ("name", "writer", "readers", "dsem", "dcount", "dmas")

    def __init__(self, name):
        self.name = name
        self.writer = None
        self.readers = []
        self.dsem = None
        self.dcount = 0
        self.dmas = {}


class Op:
    __slots__ = ("eng", "fn", "waits", "signals", "ordinal", "is_dma", "dbuf", "dval", "idx")


class Prog:
    ENGS = ("pe", "act", "dve", "pool", "sp")

    def __init__(self, nc):
        self.nc = nc
        self.ops = []
        self.stack = contextlib.ExitStack()
        self.esem = {}
        self.n_t = 0
        self.dma_bufs = []

    def sb(self, shape, dtype, name=None):
        self.n_t += 1
        t = self.stack.enter_context(self.nc.sbuf_tensor(name or f"sb{self.n_t}", list(shape), dtype))
        return t

    def ps(self, shape, dtype, name=None):
        self.n_t += 1
        t = self.stack.enter_context(self.nc.psum_tensor(name or f"ps{self.n_t}", list(shape), dtype))
        return t

    def buf(self, name):
        return Buf(name)

    def _deps(self, op, reads, writes):
        deps = []
        for b in reads:
            if b.writer is not None:
                deps.append(b.writer)
            if "w" in b.dmas:
                deps.append(b.dmas["w"])
        for b in writes:
            if b.writer is not None:
                deps.append(b.writer)
            deps.extend(b.readers)
            deps.extend(b.dmas.values())
        return deps

    def op(self, eng, fn, reads=(), writes=()):
        o = Op()
        o.eng = eng
        o.fn = fn
        o.is_dma = False
        o.signals = False
        o.ordinal = None
        o.idx = len(self.ops)
        waits = []
        for d in self._deps(o, reads, writes):
            if d.is_dma:
                waits.append(("d", d.dbuf, d.dbuf.dcount))
            else:
                if d.eng == "pe" and eng == "pe":
                    continue
                d.signals = True
                waits.append(("e", d))
        o.waits = waits
        for b in reads:
            b.readers.append(o)
        for b in writes:
            b.writer = o
            b.readers = []
            b.dmas = {}
        self.ops.append(o)
        return o

    def dma(self, eng, fn, sbuf, direction, reads=(), writes=()):
        o = Op()
        o.eng = eng
        o.fn = fn
        o.is_dma = True
        o.signals = False
        o.ordinal = None
        o.idx = len(self.ops)
        waits = []
        deps = []
        if direction == "w":
            if sbuf.writer is not None:
                deps.append(sbuf.writer)
            deps.extend(sbuf.readers)
            if "r" in sbuf.dmas:
                deps.append(sbuf.dmas["r"])
        else:
            if sbuf.writer is not None:
                deps.append(sbuf.writer)
            if "w" in sbuf.dmas:
                deps.append(sbuf.dmas["w"])
        deps.extend(self._deps(o, reads, writes))
        for d in deps:
            if d.is_dma:
                waits.append(("d", d.dbuf, d.dbuf.dcount))
            else:
                d.signals = True
                waits.append(("e", d))
        o.waits = waits
        if sbuf.dsem is None:
            sbuf.dsem = self.stack.enter_context(self.nc.semaphore(f"dq{len(self.dma_bufs)}"))
            self.dma_bufs.append(sbuf)
        sbuf.dcount += 1
        o.dbuf = sbuf
        o.dval = sbuf.dcount
        if direction == "w":
            sbuf.writer = None
            sbuf.readers = []
            sbuf.dmas.pop("r", None)
        sbuf.dmas[direction] = o
        for b in reads:
            b.readers.append(o)
        for b in writes:
            b.writer = o
            b.readers = []
            b.dmas = {}
        self.ops.append(o)
        return o

    def emit(self):
        nc = self.nc
        for e in self.ENGS:
            self.esem[e] = self.stack.enter_context(nc.semaphore(f"es_{e}"))
        counts = {e: 0 for e in self.ENGS}
        for o in self.ops:
            if (not o.is_dma) and o.signals:
                counts[o.eng] += 1
                o.ordinal = counts[o.eng]
        per = {e: [o for o in self.ops if o.eng == e] for e in self.ENGS}
        final_d = [(b.dsem, 16 * b.dcount) for b in self.dma_bufs]
        esem = self.esem

        def replay(engname, eng):
            seen = {}
            for o in per[engname]:
                for w in o.waits:
                    if w[0] == "d":
                        sem, val = w[1].dsem, 16 * w[2]
                    else:
                        sem, val = esem[w[1].eng], w[1].ordinal
                    k = id(sem)
                    if seen.get(k, 0) >= val:
                        continue
                    seen[k] = val
                    eng.wait_ge(sem, val)
                ins = o.fn(eng)
                if o.is_dma:
                    ins.then_inc(o.dbuf.dsem, 16)
                elif o.signals:
                    ins.then_inc(esem[engname], 1)
            if engname == "sp":
                for sem, val in final_d:
                    eng.wait_ge(sem, val)

        with nc.Block() as block:
            @block.tensor
            def _(eng):
                replay("pe", eng)

            @block.scalar
            def _(eng):
                replay("act", eng)

            @block.vector
            def _(eng):
                replay("dve", eng)

            @block.gpsimd
            def _(eng):
                replay("pool", eng)

            @block.sync
            def _(eng):
                replay("sp", eng)
        self.stack.close()


def _run(nc, in_maps):
    res = run_bass_kernel_spmd(nc, in_maps, core_ids=list(range(NCORES)))
    return res.results


def build_p0():
    nc = bass.Bass("TRN2", target_bir_lowering=False)
    cT = nc.dram_tensor("cT", [128, 8, 2], F32, kind="ExternalInput").ap()
    w_mod = nc.dram_tensor("w_mod", [2, DM, 6 * DM], F32, kind="ExternalInput").ap()
    b_mod = nc.dram_tensor("b_mod", [2, 6 * DM], F32, kind="ExternalInput").ap()
    mod = nc.dram_tensor("mod", [2, 2, 6 * DM], F32, kind="ExternalOutput").ap()
    P = Prog(nc)
    c_sb = P.sb([128, 8, 2], F32)
    s_sb = P.sb([128, 8, 2], BF16)
    b_c = P.buf("c")
    b_s = P.buf("s")
    P.dma("sp", lambda e: e.dma_start(out=c_sb[:], in_=cT), b_c, "w")
    P.op("act", lambda e: e.activation(out=s_sb[:], in_=c_sb[:], func=AF.Silu), reads=[b_c], writes=[b_s])
    NW = 3
    w_sb = [P.sb([128, 8, 512], BF16) for _ in range(NW)]
    b_w = [P.buf(f"w{i}") for i in range(NW)]
    bm_sb = [P.sb([2, 512], F32) for _ in range(2)]
    b_bm = [P.buf(f"bm{i}") for i in range(2)]
    o_sb = [P.sb([2, 512], F32) for _ in range(2)]
    b_o = [P.buf(f"o{i}") for i in range(2)]
    pss = [P.ps([2, 512], F32) for _ in range(2)]
    b_ps = [P.buf(f"ps{i}") for i in range(2)]
    it = 0
    for l in range(2):
        for n in range(12):
            wi = it % NW
            j = it % 2
            src = w_mod[l, :, n * 512:(n + 1) * 512].rearrange("(k p) n -> p k n", p=128)
            P.dma("pool", lambda e, wi=wi, src=src: e.dma_start(out=w_sb[wi][:], in_=src), b_w[wi], "w")
            for r in range(2):
                bsrc = b_mod[l:l + 1, n * 512:(n + 1) * 512]
                P.dma("sp", lambda e, j=j, r=r, bsrc=bsrc: e.dma_start(out=bm_sb[j][r:r + 1, :], in_=bsrc),
                      b_bm[j], "w")
            for k in range(8):
                P.op("pe", lambda e, j=j, wi=wi, k=k: e.matmul(pss[j][:], lhsT=s_sb[:, k, :], rhs=w_sb[wi][:, k, :],
                                                               start=(k == 0), stop=(k == 7)),
                     reads=[b_s, b_w[wi]], writes=[b_ps[j]])
            P.op("dve", lambda e, j=j: e.tensor_tensor(out=o_sb[j][:], in0=pss[j][:], in1=bm_sb[j][:], op=ALU.add),
                 reads=[b_ps[j], b_bm[j]], writes=[b_o[j]])
            dst = mod[l, :, n * 512:(n + 1) * 512]
            P.dma("sp", lambda e, j=j, dst=dst: e.dma_start(out=dst, in_=o_sb[j][:]), b_o[j], "r")
            it += 1
    P.emit()
    return nc


def run_p0(c, c_ctx, w_mod, b_mod):
    nc = build_p0()
    in_maps = []
    for core in range(NCORES):
        b = core // 2
        cc = np.stack([c[b], c_ctx], axis=-1)
        cT = np.ascontiguousarray(cc.reshape(8, 128, 2).transpose(1, 0, 2))
        in_maps.append({"cT": cT, "w_mod": w_mod, "b_mod": b_mod})
    res = _run(nc, in_maps)
    return [r["mod"] for r in res]


def rms_rstd(P, x_ap, scr_ap, ss_ap, rstd_ap, b_x, b_scr, b_st, d=DM):
    P.op("act", lambda e: e.activation(out=scr_ap, in_=x_ap, func=AF.Square, accum_out=ss_ap),
         reads=[b_x], writes=[b_scr, b_st])
    P.op("dve", lambda e: e.tensor_scalar(out=ss_ap, in0=ss_ap, scalar1=1.0 / d, scalar2=EPS, op0=ALU.mult, op1=ALU.add),
         reads=[b_st], writes=[b_st])
    P.op("act", lambda e: e.activation(out=ss_ap, in_=ss_ap, func=AF.Sqrt), reads=[b_st], writes=[b_st])
    P.op("dve", lambda e: e.reciprocal(out=rstd_ap, in_=ss_ap), reads=[b_st], writes=[b_st])


def make_ident(P, dtype=BF16):
    ident_f = P.sb([128, 128], F32)
    ident = P.sb([128, 128], dtype)
    b = P.buf("ident")
    P.op("pool", lambda e: e.memset(ident_f[:], 1.0), writes=[b])
    P.op("pool", lambda e: e.affine_select(out=ident_f[:], in_=ident_f[:], pattern=[[-1, 128]],
                                           compare_op=ALU.is_equal, fill=0.0, base=0, channel_multiplier=1),
         reads=[b], writes=[b])
    P.op("pool", lambda e: e.tensor_copy(out=ident[:], in_=ident_f[:]), reads=[b], writes=[b])
    return ident, ident_f, b


def build_p1(NT, NTX):
    nc = bass.Bass("TRN2", target_bir_lowering=False)
    T = NT * 128
    xin = nc.dram_tensor("xin", [T, DM], F32, kind="ExternalInput").ap()
    mod = nc.dram_tensor("mod", [2, 6 * DM], F32, kind="ExternalInput").ap()
    g_pre = nc.dram_tensor("g_pre", [DM], F32, kind="ExternalInput").ap()
    w_in = nc.dram_tensor("w_in", [DM, 6144], F32, kind="ExternalInput").ap()
    zmix = nc.dram_tensor("zmix", [T, 2048], F32, kind="ExternalOutput").ap()
    gates = nc.dram_tensor("gates", [T, 4096], BF16, kind="ExternalOutput").ap()
    rope_cs = nc.dram_tensor("rope_cs", [T, 2, 384], F32, kind="ExternalInput").ap()
    gm_ln = nc.dram_tensor("gm_ln", [2, 256], F32, kind="ExternalInput").ap()
    gm_wsT = nc.dram_tensor("gm_wsT", [128, 4, 128], F32, kind="ExternalInput").ap()
    gm_bsT = nc.dram_tensor("gm_bsT", [128, 4], F32, kind="ExternalInput").ap()
    qkr = nc.dram_tensor("qkr", [T, 384], F32, kind="ExternalOutput").ap()
    o_b = nc.dram_tensor("o_b", [T, 256], F32, kind="ExternalOutput").ap()
    P = Prog(nc)
    ident, _, b_id = make_ident(P)
    lng = P.sb([128, 256], F32)
    lnb = P.sb([128, 256], F32)
    wsT = P.sb([128, 4, 128], BF16)
    bsT = P.sb([128, 4], F32)
    b_gmp = P.buf("gmp")
    P.dma("sp", lambda e: e.dma_start(out=lng[:], in_=gm_ln[0, :].partition_broadcast(128)), b_gmp, "w")
    P.dma("sp", lambda e: e.dma_start(out=lnb[:], in_=gm_ln[1, :].partition_broadcast(128)), b_gmp, "w")
    P.dma("sp", lambda e: e.dma_start(out=bsT[:], in_=gm_bsT), b_gmp, "w")
    b_wsT = P.buf("wsT")
    P.dma("pool", lambda e: e.dma_start(out=wsT[:], in_=gm_wsT), b_wsT, "w")
    cs = [P.sb([128, 2, 384], F32) for _ in range(2)]
    b_cs = [P.buf(f"cs{i}") for i in range(2)]
    rp = [P.sb([128, 384], F32) for _ in range(2)]
    b_rp = [P.buf(f"rp{i}") for i in range(2)]
    rt = [P.sb([128, 192], F32) for _ in range(2)]
    b_rt = [P.buf(f"rt{i}") for i in range(2)]
    ge = P.sb([128, 512], F32)
    b_ge = P.buf("ge")
    g1 = P.sb([128, 512], F32)
    b_g1 = P.buf("g1")
    g2 = P.sb([128, 512], F32)
    b_g2 = P.buf("g2")
    vst = P.sb([128, 4], F32)
    b_vst = P.buf("vst")
    vc = P.sb([128, 256], F32)
    b_vc = P.buf("vc")
    vscr = P.sb([128, 256], F32)
    b_vscr = P.buf("vscr")
    vn = P.sb([128, 256], BF16)
    b_vn = P.buf("vn")
    pg = P.ps([128, 256], F32)
    b_pg = P.buf("pg")
    ob = [P.sb([128, 256], F32) for _ in range(2)]
    b_ob = [P.buf(f"ob{i}") for i in range(2)]
    w_sb = P.sb([128, 8, 6144], BF16)
    b_w = [P.buf(f"w{n}") for n in range(12)]
    for n in range(12):
        src = w_in[:, n * 512:(n + 1) * 512].rearrange("(k p) n -> p k n", p=128)
        P.dma("pool", lambda e, n=n, src=src: e.dma_start(out=w_sb[:, :, n * 512:(n + 1) * 512], in_=src), b_w[n], "w")
    g_sb = P.sb([128, DM], F32)
    b_g = P.buf("g")
    P.dma("sp", lambda e: e.dma_start(out=g_sb[:], in_=g_pre.partition_broadcast(128)), b_g, "w")
    G1 = [P.sb([128, DM], F32) for _ in range(2)]
    SH = [P.sb([128, DM], F32) for _ in range(2)]
    b_G1 = [P.buf(f"G1{i}") for i in range(2)]
    b_SH = [P.buf(f"SH{i}") for i in range(2)]
    for cls in range(2):
        P.dma("sp", lambda e, cls=cls: e.dma_start(out=SH[cls][:], in_=mod[cls, 0:DM].partition_broadcast(128)), b_SH[cls], "w")
        P.dma("sp", lambda e, cls=cls: e.dma_start(out=G1[cls][:], in_=mod[cls, DM:2 * DM].partition_broadcast(128)), b_G1[cls], "w")
        P.op("dve", lambda e, cls=cls: e.scalar_tensor_tensor(out=G1[cls][:], in0=G1[cls][:], scalar=1.0, in1=g_sb[:],
                                                             op0=ALU.add, op1=ALU.mult),
             reads=[b_G1[cls], b_g], writes=[b_G1[cls]])
    x_sb = [P.sb([128, DM], F32) for _ in range(2)]
    b_x = [P.buf(f"x{i}") for i in range(2)]
    scr = P.sb([128, DM], F32)
    b_scr = P.buf("scr")
    st = [P.sb([128, 2], F32) for _ in range(2)]
    b_st = [P.buf(f"st{i}") for i in range(2)]
    tmp = P.sb([128, DM], F32)
    b_tmp = P.buf("tmp")
    hb = P.sb([128, DM], BF16)
    b_hb = P.buf("hb")
    hT = [P.sb([128, 8, 128], BF16) for _ in range(2)]
    b_hT = [P.buf(f"hT{i}") for i in range(2)]
    pT = P.ps([128, 8, 128], BF16)
    b_pT = P.buf("pT")
    pm = [P.ps([128, 512], F32) for _ in range(4)]
    b_pm = [P.buf(f"pm{i}") for i in range(4)]

    def epilogue(t, j):
        z = zo[j]
        P.dma("sp", lambda e: e.dma_start(out=cs[j][:], in_=rope_cs[t * 128:(t + 1) * 128]), b_cs[j], "w")
        zv = z[:, 768:1152].rearrange("p (a two d) -> p a two d", two=2, d=16)
        x1, x2 = zv[:, :, 0, :], zv[:, :, 1, :]
        cv = cs[j][:, 0, :].rearrange("p (a two d) -> p a two d", two=2, d=16)
        sv = cs[j][:, 1, :].rearrange("p (a two d) -> p a two d", two=2, d=16)
        rv = rp[j][:].rearrange("p (a two d) -> p a two d", two=2, d=16)
        t1 = rt[0][:].rearrange("p (a d) -> p a d", d=16)
        t2 = rt[1][:].rearrange("p (a d) -> p a d", d=16)
        P.op("pool", lambda e: e.tensor_tensor(out=t1, in0=x1, in1=cv[:, :, 0, :], op=ALU.mult), reads=[b_zo[j], b_cs[j]], writes=[b_rt[0]])
        P.op("pool", lambda e: e.tensor_tensor(out=t2, in0=x2, in1=sv[:, :, 0, :], op=ALU.mult), reads=[b_zo[j], b_cs[j]], writes=[b_rt[1]])
        P.op("pool", lambda e: e.tensor_tensor(out=rv[:, :, 0, :], in0=t1, in1=t2, op=ALU.subtract), reads=[b_rt[0], b_rt[1]], writes=[b_rp[j]])
        P.op("pool", lambda e: e.tensor_tensor(out=t1, in0=x1, in1=sv[:, :, 0, :], op=ALU.mult), reads=[b_zo[j], b_cs[j], b_rp[j]], writes=[b_rt[0]])
        P.op("pool", lambda e: e.tensor_tensor(out=t2, in0=x2, in1=cv[:, :, 0, :], op=ALU.mult), reads=[b_zo[j], b_cs[j], b_rp[j]], writes=[b_rt[1]])
        P.op("pool", lambda e: e.tensor_tensor(out=rv[:, :, 1, :], in0=t1, in1=t2, op=ALU.add), reads=[b_rt[0], b_rt[1]], writes=[b_rp[j]])
        P.dma("sp", lambda e: e.dma_start(out=qkr[t * 128:(t + 1) * 128, :], in_=rp[j][:]), b_rp[j], "r")
        gx = z[:, 1280:1792]
        P.op("dve", lambda e: e.tensor_tensor(out=g1[:], in0=gx, in1=gx, op=ALU.mult), reads=[b_zo[j]], writes=[b_g1])
        P.op("dve", lambda e: e.tensor_scalar(out=g1[:], in0=g1[:], scalar1=0.044715, scalar2=1.0, op0=ALU.mult, op1=ALU.add), reads=[b_g1], writes=[b_g1])
        P.op("dve", lambda e: e.tensor_tensor(out=g1[:], in0=g1[:], in1=gx, op=ALU.mult), reads=[b_g1, b_zo[j]], writes=[b_g1])
        P.op("act", lambda e: e.activation(out=g2[:], in_=g1[:], func=AF.Sigmoid, scale=1.5957691216057308), reads=[b_g1], writes=[b_g2])
        P.op("dve", lambda e: e.tensor_tensor(out=ge[:], in0=g2[:], in1=gx, op=ALU.mult), reads=[b_g2, b_zo[j]], writes=[b_ge])
        gv = ge[:, 256:512]
        P.op("act", lambda e: e.activation(out=vscr[:], in_=gv, func=AF.Identity, accum_out=vst[:, 0:1]), reads=[b_ge], writes=[b_vscr, b_vst])
        P.op("dve", lambda e: e.tensor_scalar(out=vst[:, 0:1], in0=vst[:, 0:1], scalar1=1.0 / 256, scalar2=None, op0=ALU.mult), reads=[b_vst], writes=[b_vst])
        P.op("dve", lambda e: e.tensor_scalar(out=vc[:], in0=gv, scalar1=vst[:, 0:1], scalar2=None, op0=ALU.subtract), reads=[b_ge, b_vst], writes=[b_vc])
        rms_rstd(P, vc[:], vscr[:], vst[:, 1:2], vst[:, 2:3], b_vc, b_vscr, b_vst, d=256)
        P.op("dve", lambda e: e.scalar_tensor_tensor(out=vc[:], in0=vc[:], scalar=vst[:, 2:3], in1=lng[:], op0=ALU.mult, op1=ALU.mult),
             reads=[b_vc, b_vst, b_gmp], writes=[b_vc])
        P.op("dve", lambda e: e.tensor_tensor(out=vn[:], in0=vc[:], in1=lnb[:], op=ALU.add), reads=[b_vc, b_gmp], writes=[b_vn])
        for g in range(4):
            P.op("pe", lambda e, g=g: e.matmul(pg[:, g * 64:(g + 1) * 64], lhsT=wsT[:, g, :], rhs=vn[:, g * 64:(g + 1) * 64], start=True, stop=True),
                 reads=[b_wsT, b_vn], writes=[b_pg])
        for g in range(4):
            P.op("dve", lambda e, g=g: e.scalar_tensor_tensor(out=ob[j][:, g * 64:(g + 1) * 64], in0=pg[:, g * 64:(g + 1) * 64], scalar=bsT[:, g:g + 1],
                                                             in1=ge[:, g * 64:(g + 1) * 64], op0=ALU.add, op1=ALU.mult),
                 reads=[b_pg, b_ge, b_gmp], writes=[b_ob[j]])
        P.dma("sp", lambda e: e.dma_start(out=o_b[t * 128:(t + 1) * 128, :], in_=ob[j][:]), b_ob[j], "r")

    zo = [P.sb([128, 2048], F32) for _ in range(2)]
    b_zo = [P.buf(f"zo{i}") for i in range(2)]
    go = [P.sb([128, 4096], BF16) for _ in range(2)]
    b_go = [P.buf(f"go{i}") for i in range(2)]
    it = 0
    for t in range(NT):
        j = t % 2
        cls = 0 if t < NTX else 1
        P.dma("sp", lambda e, j=j, t=t: e.dma_start(out=x_sb[j][:], in_=xin[t * 128:(t + 1) * 128, :]), b_x[j], "w")
        rms_rstd(P, x_sb[j][:], scr[:], st[j][:, 0:1], st[j][:, 1:2], b_x[j], b_scr, b_st[j])
        P.op("dve", lambda e, j=j, cls=cls: e.scalar_tensor_tensor(out=tmp[:], in0=x_sb[j][:], scalar=st[j][:, 1:2], in1=G1[cls][:],
                                                                   op0=ALU.mult, op1=ALU.mult),
             reads=[b_x[j], b_st[j], b_G1[cls]], writes=[b_tmp])
        P.op("pool", lambda e, cls=cls: e.tensor_tensor(out=hb[:], in0=tmp[:], in1=SH[cls][:], op=ALU.add),
             reads=[b_tmp, b_SH[cls]], writes=[b_hb])
        for k in range(8):
            P.op("pe", lambda e, k=k: e.transpose(out=pT[:, k, :], in_=hb[:, k * 128:(k + 1) * 128], identity=ident[:]),
                 reads=[b_hb, b_id], writes=[b_pT])
        P.op("act", lambda e, j=j: e.copy(out=hT[j][:], in_=pT[:]), reads=[b_pT], writes=[b_hT[j]])
        for n in range(12):
            q = it % 4
            it += 1
            for k in range(8):
                P.op("pe", lambda e, q=q, j=j, k=k, n=n: e.matmul(pm[q][:], lhsT=hT[j][:, k, :], rhs=w_sb[:, k, n * 512:(n + 1) * 512],
                                                                  start=(k == 0), stop=(k == 7)),
                     reads=[b_hT[j], b_w[n]], writes=[b_pm[q]])
            if n < 4:
                eng = "dve" if n % 2 == 0 else "act"
                if eng == "dve":
                    P.op("dve", lambda e, q=q, j=j, n=n: e.tensor_copy(out=zo[j][:, n * 512:(n + 1) * 512], in_=pm[q][:]),
                         reads=[b_pm[q]], writes=[b_zo[j]])
                else:
                    P.op("act", lambda e, q=q, j=j, n=n: e.copy(out=zo[j][:, n * 512:(n + 1) * 512], in_=pm[q][:]),
                         reads=[b_pm[q]], writes=[b_zo[j]])
            else:
                m = n - 4
                P.op("act", lambda e, q=q, j=j, m=m: e.activation(out=go[j][:, m * 512:(m + 1) * 512], in_=pm[q][:], func=AF.Sigmoid),
                     reads=[b_pm[q]], writes=[b_go[j]])
            if n == 3:
                P.dma("sp", lambda e, j=j, t=t: e.dma_start(out=zmix[t * 128:(t + 1) * 128, :], in_=zo[j][:]), b_zo[j], "r")
                epilogue(t, j)
        P.dma("sp", lambda e, j=j, t=t: e.dma_start(out=gates[t * 128:(t + 1) * 128, :], in_=go[j][:]), b_go[j], "r")
    P.emit()
    return nc


def rope_table(rows, cols, on):
    nq = 16
    inv = 10000.0 ** (-np.arange(nq, dtype=np.float32) / nq)
    T = len(rows)
    ar = rows.astype(np.float32)[:, None] * inv[None, :]
    ac = cols.astype(np.float32)[:, None] * inv[None, :]
    ar = ar * on[:, None]
    ac = ac * on[:, None]
    out = np.zeros((T, 2, 6, 2, 2, 16), np.float32)
    for fi, f in enumerate((np.cos, np.sin)):
        out[:, fi, :, 0, :, :] = f(ar)[:, None, None, :]
        out[:, fi, :, 1, :, :] = f(ac)[:, None, None, :]
    return out.reshape(T, 2, 384)


NEG = -30000.0


def build_attn(H, HKV, NQT, NSEQCH, tiles, n_tabI, wI, n_tabE, wE, use_sink):
    nc = bass.Bass("TRN2", target_bir_lowering=False)
    TQ = NQT * 128
    NCH = NSEQCH + 2
    TK = NCH * 128
    qT = nc.dram_tensor("qT", [64, H, TQ], F32, kind="ExternalInput").ap()
    kT = nc.dram_tensor("kT", [64, HKV, TK], F32, kind="ExternalInput").ap()
    v = nc.dram_tensor("v", [128, NCH, HKV * 64], F32, kind="ExternalInput").ap()
    tabI = nc.dram_tensor("tabI", [128, n_tabI, wI], F32, kind="ExternalInput").ap()
    tabE = nc.dram_tensor("tabE", [128, n_tabE, wE], F32, kind="ExternalInput").ap()
    sink = nc.dram_tensor("sink", [H], F32, kind="ExternalInput").ap()
    out = nc.dram_tensor("o", [TQ, H * 64], F32, kind="ExternalOutput").ap()
    P = Prog(nc)
    ident, _, b_id = make_ident(P)
    qT_sb = P.sb([64, H, TQ], BF16)
    kT_sb = P.sb([64, HKV, TK], BF16)
    v_sb = P.sb([128, NCH, HKV * 64], BF16)
    tI_sb = P.sb([128, n_tabI, wI], F32)
    tE_sb = P.sb([128, n_tabE, wE], F32)
    b_q = [P.buf(f"q{h}") for h in range(H)]
    b_k = [P.buf(f"k{h}") for h in range(HKV)]
    b_v = P.buf("v")
    b_tI = P.buf("tI")
    b_tE = P.buf("tE")
    for h in range(HKV):
        P.dma("pool", lambda e, h=h: e.dma_start(out=kT_sb[:, h, :], in_=kT[:, h, :]), b_k[h], "w")
    for h in range(H):
        P.dma("pool", lambda e, h=h: e.dma_start(out=qT_sb[:, h, :], in_=qT[:, h, :]), b_q[h], "w")
    vstep = 8
    for c in range(0, NCH, vstep):
        c1 = min(NCH, c + vstep)
        P.dma("pool", lambda e, c=c, c1=c1: e.dma_start(out=v_sb[:, c:c1, :], in_=v[:, c:c1, :]), b_v, "w")
    for i in range(n_tabI):
        P.dma("sp", lambda e, i=i: e.dma_start(out=tI_sb[:, i, :], in_=tabI[:, i, :]), b_tI, "w")
    for i in range(n_tabE):
        P.dma("sp", lambda e, i=i: e.dma_start(out=tE_sb[:, i, :], in_=tabE[:, i, :]), b_tE, "w")
    WMAX = 2 + 7 * 128 + 256
    S = [P.sb([128, WMAX], F32) for _ in range(H)]
    b_S = [P.buf(f"S{i}") for i in range(H)]
    for i in range(H):
        h = i % H
        if use_sink:
            P.dma("sp", lambda e, i=i, h=h: e.dma_start(out=S[i][:, 1:2], in_=sink[h:h + 1].partition_broadcast(128)), b_S[i], "w")
        else:
            P.op("pool", lambda e, i=i: e.memset(S[i][:, 0:2], NEG), writes=[b_S[i]])
    Pb = [P.sb([128, WMAX], BF16) for _ in range(2)]
    b_Pb = [P.buf(f"Pb{i}") for i in range(2)]
    st = [P.sb([128, 4], F32) for _ in range(2)]
    b_st = [P.buf(f"st{i}") for i in range(2)]
    ps_s = [P.ps([128, 3, 512], F32) for _ in range(2)]
    b_pss = [P.buf(f"pss{i}") for i in range(2)]
    ps_T = P.ps([128, 8, 128], BF16)
    b_psT = [P.buf(f"psT{i}") for i in range(2)]
    PT = [P.sb([128, 12, 128], BF16) for _ in range(2)]
    b_PT = [[P.buf(f"PT{i}_{g}") for g in range(3)] for i in range(2)]
    ps_O = P.ps([128, H * 64], F32)
    b_psO = [P.buf(f"psO{h}") for h in range(H)]
    O_sb = [P.sb([128, H * 64], F32) for _ in range(2)]
    b_O = [P.buf(f"O{i}") for i in range(2)]
    scale = 0.125
    iters = [(t, h) for t in range(len(tiles)) for h in range(H)]
    g = H // HKV

    def segs(nseq):
        r = []
        n = nseq * 128
        if n > 0:
            r.append((0, min(n, 512), 0))
        if n > 512:
            r.append((512, n - 512, 1))
        return r

    def emit_scores(i):
        t, h = iters[i]
        c0, nseq, _ = tiles[t]
        par = i % 2
        kvh = h // g
        for (off, ln, bank) in segs(nseq):
            P.op("pe", lambda e, off=off, ln=ln, bank=bank: e.matmul(ps_s[par][:, bank, 0:ln], lhsT=qT_sb[:, h, t * 128:(t + 1) * 128],
                                                                      rhs=kT_sb[:, kvh, c0 * 128 + off:c0 * 128 + off + ln], start=True, stop=True),
                 reads=[b_q[h], b_k[kvh]], writes=[b_pss[par]])
        P.op("pe", lambda e: e.matmul(ps_s[par][:, 2, 0:256], lhsT=qT_sb[:, h, t * 128:(t + 1) * 128],
                                      rhs=kT_sb[:, kvh, NSEQCH * 128:NSEQCH * 128 + 256], start=True, stop=True),
             reads=[b_q[h], b_k[kvh]], writes=[b_pss[par]])

    def emit_rest(i):
        t, h = iters[i]
        c0, nseq, tab = tiles[t]
        par = i % 2
        kvh = h // g
        si = h
        Sx = S[si]
        nk = nseq * 128 + 256
        for (off, ln, bank) in segs(nseq):
            if tab[0] == "I":
                tap = tI_sb[:, tab[1](h), off:off + ln]
                bt = b_tI
            else:
                tap = tE_sb[:, tab[1](h), off:off + ln]
                bt = b_tE
            P.op("dve", lambda e, off=off, ln=ln, bank=bank, tap=tap: e.scalar_tensor_tensor(out=Sx[:, 2 + off:2 + off + ln], in0=ps_s[par][:, bank, 0:ln],
                                                                                          scalar=scale, in1=tap, op0=ALU.mult, op1=ALU.add),
                 reads=[b_pss[par], bt], writes=[b_S[si]])
        P.op("act", lambda e: e.activation(out=Sx[:, 2 + nseq * 128:2 + nk], in_=ps_s[par][:, 2, 0:256], func=AF.Copy, scale=scale),
             reads=[b_pss[par]], writes=[b_S[si]])
        P.op("dve", lambda e: e.reduce_max(out=st[par][:, 0:1], in_=Sx[:, 1:2 + nk], axis=AX.X), reads=[b_S[si]], writes=[b_st[par]])
        P.op("dve", lambda e: e.tensor_scalar(out=st[par][:, 1:2], in0=st[par][:, 0:1], scalar1=-1.0, scalar2=None, op0=ALU.mult),
             reads=[b_st[par]], writes=[b_st[par]])
        P.op("act", lambda e: e.activation(out=Pb[par][:, 1:2 + nk], in_=Sx[:, 1:2 + nk], func=AF.Exp, bias=st[par][:, 1:2], scale=1.0,
                                           accum_out=st[par][:, 2:3]),
             reads=[b_S[si], b_st[par]], writes=[b_Pb[par], b_st[par]])
        nch = nseq + 2
        ngrp = (nch + 7) // 8
        for gi in range(ngrp):
            ca, cb = gi * 8, min(nch, gi * 8 + 8)
            for c in range(ca, cb):
                P.op("pe", lambda e, c=c, ca=ca: e.transpose(out=ps_T[:, c - ca, :], in_=Pb[par][:, 2 + c * 128:2 + (c + 1) * 128],
                                                             identity=ident[:]),
                     reads=[b_Pb[par], b_id], writes=[b_psT[0]])
            if gi % 2 == 0:
                P.op("act", lambda e, ca=ca, cb=cb: e.copy(out=PT[par][:, ca:cb, :], in_=ps_T[:, 0:(cb - ca), :]),
                     reads=[b_psT[0]], writes=[b_PT[par][gi]])
            else:
                P.op("dve", lambda e, ca=ca, cb=cb: e.tensor_copy(out=PT[par][:, ca:cb, :], in_=ps_T[:, 0:(cb - ca), :]),
                     reads=[b_psT[0]], writes=[b_PT[par][gi]])
        for c in range(nch):
            vc = (c0 + c) if c < nseq else (NSEQCH + c - nseq)
            P.op("pe", lambda e, c=c, vc=vc: e.matmul(ps_O[:, h * 64:(h + 1) * 64], lhsT=PT[par][:, c, :], rhs=v_sb[:, vc, kvh * 64:(kvh + 1) * 64],
                                                      start=(c == 0), stop=(c == nch - 1)),
                 reads=[b_PT[par][c // 8], b_v], writes=[b_psO[0]])
        tp = t % 2
        P.op("dve", lambda e: e.reciprocal(out=st[par][:, 3:4], in_=st[par][:, 2:3]), reads=[b_st[par]], writes=[b_st[par]])
        P.op("dve", lambda e: e.tensor_scalar(out=O_sb[tp][:, h * 64:(h + 1) * 64], in0=ps_O[:, h * 64:(h + 1) * 64], scalar1=st[par][:, 3:4],
                                              scalar2=None, op0=ALU.mult),
             reads=[b_psO[0], b_st[par]], writes=[b_O[tp]])
        if h == H - 1:
            P.dma("sp", lambda e: e.dma_start(out=out[t * 128:(t + 1) * 128, :], in_=O_sb[tp][:]), b_O[tp], "r")

    emit_scores(0)
    for i in range(len(iters)):
        if i + 1 < len(iters):
            emit_scores(i + 1)
        emit_rest(i)
    P.emit()
    return nc


def na_tiles(n_ctx_tiles):
    tiles = []
    for i in range(32):
        if 2 <= i <= 29:
            tiles.append((i + 1, 5, ("I", lambda h: h)))
        else:
            e = {0: 0, 1: 1, 30: 2, 31: 3}[i]
            tiles.append((i, 7, ("E", lambda h, e=e: e * 4 + h)))
    for i in range(n_ctx_tiles):
        tiles.append((0, 0, ("I", lambda h: h)))
    return tiles


def swa_tiles(n_ctx_tiles):
    tiles = []
    for i in range(32):
        case = 0 if i == 0 else (2 if i == 31 else 1)
        tiles.append((i, 3, ("I", lambda h, case=case: case)))
    for i in range(n_ctx_tiles):
        tiles.append((0, 0, ("I", lambda h: 0)))
    return tiles


def na_table(rpb, r0, row_start, nrows):
    H = rpb.shape[0]
    dr = np.arange(2)[:, None, None, None]
    c = np.arange(64)[None, :, None, None]
    wr = np.arange(nrows)[None, None, :, None]
    kc = np.arange(64)[None, None, None, :]
    r = r0 + dr
    kr = row_start + wr
    rs = np.clip(r - 4, 0, 120)
    cs = np.clip(c - 8, 0, 48)
    valid = (kr >= rs) & (kr < rs + 8) & (kc >= cs) & (kc < cs + 16) & (kr >= 0) & (kr < 128)
    ri = np.clip(kr - r + 7, 0, 14)
    ci = np.clip(kc - c + 15, 0, 30)
    ri, ci, valid = np.broadcast_arrays(ri, ci, valid)
    tab = rpb[:, ri, ci]
    tab = np.where(valid[None], tab, np.float32(NEG)).astype(np.float32)
    return tab.reshape(H, 128, nrows * 64)


def swa_table(n):
    kk = np.arange(384)[None, :]
    qq = np.arange(128)[:, None]
    kpos = (n - 1) * 128 + kk
    qpos = n * 128 + qq
    valid = (np.abs(kpos - qpos) <= 128) & (kpos >= 0) & (kpos < SEQ)
    return np.where(valid, np.float32(0), np.float32(NEG)).astype(np.float32)


def chunked(a, nch):
    return np.ascontiguousarray(a.reshape(nch, 128, a.shape[-1]).transpose(1, 0, 2))


def seq_window(full, lo, hi):
    S = full.shape[0]
    out = np.zeros((hi - lo,) + full.shape[1:], full.dtype)
    a, b = max(lo, 0), min(hi, S)
    out[a - lo:b - lo] = full[a:b]
    return out


def run_attn(kind, q, k, v, qc, kc, vc, rpb, sink, ctx_out):
    nct = 2 if ctx_out else 0
    if kind == "na":
        H, HKV, halo = 4, 4, 3
        tiles = na_tiles(nct)
    else:
        H, HKV, halo = 4, 2, 1
        tiles = swa_tiles(nct)
    NSEQCH = 32 + 2 * halo
    NQT = 32 + nct
    if kind == "na":
        nc = build_attn(H, HKV, NQT, NSEQCH, tiles, 4, 640, 16, 896, False)
    else:
        nc = build_attn(H, HKV, NQT, NSEQCH, tiles, 3, 384, 1, 8, True)
    in_maps = []
    for core in range(NCORES):
        b, hf = core // 2, core % 2
        t0 = hf * HALF
        qx = q[b, t0:t0 + HALF]
        if ctx_out:
            qx = np.concatenate([qx, qc[b]], axis=0)
        qT = np.ascontiguousarray(qx.reshape(NQT * 128, H, 64).transpose(2, 1, 0))
        kx = np.concatenate([seq_window(k[b], t0 - halo * 128, t0 + HALF + halo * 128), kc[b]], axis=0)
        kT = np.ascontiguousarray(kx.reshape(-1, HKV, 64).transpose(2, 1, 0))
        vx = np.concatenate([seq_window(v[b], t0 - halo * 128, t0 + HALF + halo * 128), vc[b]], axis=0)
        vch = chunked(vx, NSEQCH + 2)
        if kind == "na":
            R0 = hf * 64
            tabI = na_table(rpb, 68, 64, 10)
            tabE = np.stack([na_table(rpb, R0 + 2 * i, R0 + 2 * i - 6, 14) for i in (0, 1, 30, 31)], axis=0)
            tabI = np.ascontiguousarray(tabI.transpose(1, 0, 2))
            tabE = np.ascontiguousarray(tabE.reshape(16, 128, 896).transpose(1, 0, 2))
            snk = np.zeros(4, np.float32)
        else:
            n0 = hf * 32
            tabI = np.ascontiguousarray(np.stack([swa_table(n0), swa_table(n0 + 1), swa_table(n0 + 31)], axis=0).transpose(1, 0, 2))
            tabE = np.zeros((128, 1, 8), np.float32)
            snk = sink
        in_maps.append({"qT": qT, "kT": kT, "v": vch, "tabI": tabI, "tabE": tabE, "sink": snk})
    res = _run(nc, in_maps)
    B = q.shape[0]
    o_x = np.zeros((B, SEQ, H * 64), np.float32)
    o_c = np.zeros((B, CTX, H * 64), np.float32)
    for core in range(NCORES):
        b, hf = core // 2, core % 2
        o = res[core]["o"]
        o_x[b, hf * HALF:(hf + 1) * HALF] = o[:HALF]
        if ctx_out:
            o_c[b] = o[HALF:]
    return o_x, o_c


def build_s5(NTOK, TC=512):
    nc = bass.Bass("TRN2", target_bir_lowering=False)
    uT = nc.dram_tensor("uT", [2, 128, NTOK], F32, kind="ExternalInput").ap()
    prm = nc.dram_tensor("prm", [128, 3, 8], F32, kind="ExternalInput").ap()
    bmat = nc.dram_tensor("bmat", [128, 2, 8, 16], F32, kind="ExternalInput").ap()
    cmat = nc.dram_tensor("cmat", [128, 2, 8, 16], F32, kind="ExternalInput").ap()
    yT = nc.dram_tensor("yT", [2, 128, NTOK], F32, kind="ExternalOutput").ap()
    P = Prog(nc)
    _, ident_f, b_id = make_ident(P)
    DJ = 8
    prm_sb = P.sb([128, 3, DJ], F32)
    b_sb = P.sb([128, 2, DJ, 16], F32)
    c_sb = P.sb([128, 2, DJ, 16], F32)
    bp = P.buf("prm")
    P.dma("sp", lambda e: e.dma_start(out=prm_sb[:], in_=prm), bp, "w")
    P.dma("sp", lambda e: e.dma_start(out=b_sb[:], in_=bmat), bp, "w")
    P.dma("sp", lambda e: e.dma_start(out=c_sb[:], in_=cmat), bp, "w")
    W = P.sb([128, 16, DJ], F32)
    bw = P.buf("W")
    LRE, DT, MAG, TH, SN, CS, T1, T2, ABR, ABI, DEN, KRE, KIM, LIM = range(14)

    def ts(out_i, in_i, s1, s2, o0, o1=None):
        if o1 is None:
            P.op("dve", lambda e: e.tensor_scalar(out=W[:, out_i, :], in0=W[:, in_i, :], scalar1=s1, scalar2=None, op0=o0), reads=[bw], writes=[bw])
        else:
            P.op("dve", lambda e: e.tensor_scalar(out=W[:, out_i, :], in0=W[:, in_i, :], scalar1=s1, scalar2=s2, op0=o0, op1=o1), reads=[bw], writes=[bw])

    def tt(out_i, a_i, b_i, o):
        P.op("dve", lambda e: e.tensor_tensor(out=W[:, out_i, :], in0=W[:, a_i, :], in1=W[:, b_i, :], op=o), reads=[bw], writes=[bw])

    P.op("dve", lambda e: e.tensor_scalar(out=W[:, LRE, :], in0=prm_sb[:, 0, :], scalar1=-1e-4, scalar2=None, op0=ALU.min), reads=[bp], writes=[bw])
    P.op("dve", lambda e: e.tensor_copy(out=W[:, LIM, :], in_=prm_sb[:, 1, :]), reads=[bp, bw], writes=[bw])
    P.op("act", lambda e: e.activation(out=W[:, DT, :], in_=prm_sb[:, 2, :], func=AF.Exp), reads=[bp, bw], writes=[bw])
    tt(T1, LRE, DT, ALU.mult)
    P.op("act", lambda e: e.activation(out=W[:, MAG, :], in_=W[:, T1, :], func=AF.Exp), reads=[bw], writes=[bw])
    tt(TH, LIM, DT, ALU.mult)
    P.op("act", lambda e: e.activation(out=W[:, SN, :], in_=W[:, TH, :], func=AF.Sin, scale=1.0 / 64), reads=[bw], writes=[bw])
    tt(T1, SN, SN, ALU.mult)
    ts(CS, T1, -2.0, 1.0, ALU.mult, ALU.add)
    P.op("act", lambda e: e.activation(out=W[:, SN, :], in_=W[:, TH, :], func=AF.Sin, scale=1.0 / 32), reads=[bw], writes=[bw])
    for _ in range(5):
        tt(T1, CS, CS, ALU.mult)
        tt(T2, SN, SN, ALU.mult)
        tt(SN, SN, CS, ALU.mult)
        ts(SN, SN, 2.0, None, ALU.mult)
        tt(CS, T1, T2, ALU.subtract)
    tt(ABR, MAG, CS, ALU.mult)
    tt(ABI, MAG, SN, ALU.mult)
    tt(T1, LRE, LRE, ALU.mult)
    tt(T2, LIM, LIM, ALU.mult)
    tt(DEN, T1, T2, ALU.add)
    P.op("dve", lambda e: e.reciprocal(out=W[:, DEN, :], in_=W[:, DEN, :]), reads=[bw], writes=[bw])
    ts(T1, ABR, -1.0, None, ALU.add)
    tt(KRE, T1, LRE, ALU.mult)
    tt(T2, ABI, LIM, ALU.mult)
    tt(KRE, KRE, T2, ALU.add)
    tt(KRE, KRE, DEN, ALU.mult)
    tt(KIM, ABI, LRE, ALU.mult)
    tt(T2, T1, LIM, ALU.mult)
    tt(KIM, KIM, T2, ALU.subtract)
    tt(KIM, KIM, DEN, ALU.mult)
    bbE = P.sb([128, 2, DJ, 128], F32)
    cE = P.sb([128, 2, DJ, 128], F32)
    b_bbE = P.buf("bbE")
    b_cE = P.buf("cE")
    P.op("pool", lambda e: e.memset(bbE[:], 0.0), writes=[b_bbE])
    P.op("pool", lambda e: e.memset(cE[:], 0.0), writes=[b_cE])
    tmpb = P.sb([128, 2, 16], F32)
    b_tmpb = P.buf("tmpb")
    for dj in range(DJ):
        j = dj % 4
        for half in range(2):
            p0, p1 = half * 64, half * 64 + 64
            ch0 = (2 * j + half) * 16
            kre = W[p0:p1, KRE, dj:dj + 1]
            kim = W[p0:p1, KIM, dj:dj + 1]
            bre = b_sb[p0:p1, 0, dj, :]
            bim = b_sb[p0:p1, 1, dj, :]
            P.op("dve", lambda e, p0=p0, p1=p1, kim=kim, bim=bim: e.tensor_scalar(out=tmpb[p0:p1, 0, :], in0=bim, scalar1=kim, scalar2=None, op0=ALU.mult),
                 reads=[bw, bp], writes=[b_tmpb])
            P.op("dve", lambda e, p0=p0, p1=p1, kre=kre, bre=bre, dj=dj, ch0=ch0: e.scalar_tensor_tensor(out=bbE[p0:p1, 0, dj, ch0:ch0 + 16], in0=bre, scalar=kre,
                                                                                                     in1=tmpb[p0:p1, 0, :], op0=ALU.mult, op1=ALU.subtract),
                 reads=[bw, bp, b_tmpb], writes=[b_bbE])
            P.op("dve", lambda e, p0=p0, p1=p1, kim=kim, bre=bre: e.tensor_scalar(out=tmpb[p0:p1, 1, :], in0=bre, scalar1=kim, scalar2=None, op0=ALU.mult),
                 reads=[bw, bp], writes=[b_tmpb])
            P.op("dve", lambda e, p0=p0, p1=p1, kre=kre, bim=bim, dj=dj, ch0=ch0: e.scalar_tensor_tensor(out=bbE[p0:p1, 1, dj, ch0:ch0 + 16], in0=bim, scalar=kre,
                                                                                                     in1=tmpb[p0:p1, 1, :], op0=ALU.mult, op1=ALU.add),
                 reads=[bw, bp, b_tmpb], writes=[b_bbE])
            P.op("dve", lambda e, p0=p0, p1=p1, dj=dj, ch0=ch0: e.tensor_copy(out=cE[p0:p1, 0, dj, ch0:ch0 + 16], in_=c_sb[p0:p1, 0, dj, :]),
                 reads=[bp], writes=[b_cE])
            P.op("dve", lambda e, p0=p0, p1=p1, dj=dj, ch0=ch0: e.tensor_scalar(out=cE[p0:p1, 1, dj, ch0:ch0 + 16], in0=c_sb[p0:p1, 1, dj, :], scalar1=-1.0,
                                                                              scalar2=None, op0=ALU.mult),
                 reads=[bp], writes=[b_cE])
    lhsB = P.sb([128, 2, DJ, 128], F32)
    b_lhsB = P.buf("lhsB")
    ptr = P.ps([128, 128], F32)
    b_ptr = P.buf("ptr")
    for ri in range(2):
        for dj in range(DJ):
            P.op("pe", lambda e, ri=ri, dj=dj: e.transpose(out=ptr[:], in_=bbE[:, ri, dj, :], identity=ident_f[:]), reads=[b_bbE, b_id], writes=[b_ptr])
            P.op("act", lambda e, ri=ri, dj=dj: e.copy(out=lhsB[:, ri, dj, :], in_=ptr[:]), reads=[b_ptr], writes=[b_lhsB])
    cosT = P.sb([128, DJ, TC], F32)
    sinT = P.sb([128, DJ, TC], F32)
    magT = P.sb([128, DJ, TC], F32)
    b_tab = [P.buf(f"tab{dj}") for dj in range(DJ)]
    ttmp = P.sb([128, TC // 2], F32)
    b_ttmp = P.buf("ttmp")
    ones = P.sb([128, TC], F32)
    b_ones = P.buf("ones")
    P.op("pool", lambda e: e.memset(ones[:], 1.0), writes=[b_ones])
    for dj in range(DJ):
        bt = b_tab[dj]
        P.op("dve", lambda e, dj=dj: e.tensor_scalar(out=magT[:, dj, :], in0=ones[:], scalar1=W[:, MAG, dj:dj + 1], scalar2=None, op0=ALU.mult),
             reads=[b_ones, bw], writes=[bt])
        P.op("dve", lambda e, dj=dj: e.tensor_copy(out=cosT[:, dj, 0:1], in_=W[:, CS, dj:dj + 1]), reads=[bw], writes=[bt])
        P.op("dve", lambda e, dj=dj: e.tensor_copy(out=sinT[:, dj, 0:1], in_=W[:, SN, dj:dj + 1]), reads=[bw], writes=[bt])
        m = 1
        while m < TC:
            cm = cosT[:, dj, m - 1:m]
            sm = sinT[:, dj, m - 1:m]
            P.op("dve", lambda e, dj=dj, m=m, sm=sm: e.tensor_scalar(out=ttmp[:, 0:m], in0=sinT[:, dj, 0:m], scalar1=sm, scalar2=None, op0=ALU.mult),
                 reads=[bt], writes=[b_ttmp])
            P.op("dve", lambda e, dj=dj, m=m, cm=cm: e.scalar_tensor_tensor(out=cosT[:, dj, m:2 * m], in0=cosT[:, dj, 0:m], scalar=cm, in1=ttmp[:, 0:m],
                                                                          op0=ALU.mult, op1=ALU.subtract),
                 reads=[bt, b_ttmp], writes=[bt])
            P.op("dve", lambda e, dj=dj, m=m, sm=sm: e.tensor_scalar(out=ttmp[:, 0:m], in0=cosT[:, dj, 0:m], scalar1=sm, scalar2=None, op0=ALU.mult),
                 reads=[bt], writes=[b_ttmp])
            P.op("dve", lambda e, dj=dj, m=m, cm=cm: e.scalar_tensor_tensor(out=sinT[:, dj, m:2 * m], in0=sinT[:, dj, 0:m], scalar=cm, in1=ttmp[:, 0:m],
                                                                          op0=ALU.mult, op1=ALU.add),
                 reads=[bt, b_ttmp], writes=[bt])
            m *= 2
    nchunks = (NTOK + TC - 1) // TC
    u_sb = [P.sb([128, TC], F32) for _ in range(2)]
    b_u = [P.buf(f"u{i}") for i in range(2)]
    ps_b = [P.ps([128, 2, 512], F32) for _ in range(2)]
    b_psb = [P.buf(f"psb{i}") for i in range(2)]
    bu = [P.sb([128, 2, TC], F32) for _ in range(2)]
    b_bu = [P.buf(f"bu{i}") for i in range(2)]
    r1 = P.sb([128, TC], F32)
    r2 = P.sb([128, TC], F32)
    b_r1, b_r2 = P.buf("r1"), P.buf("r2")
    vv = [P.sb([128, 2, TC], F32) for _ in range(2)]
    b_vv = [P.buf(f"vv{i}") for i in range(2)]
    ww = P.sb([128, 2, TC], F32)
    b_ww = P.buf("ww")
    q1 = P.sb([128, TC], F32)
    q2 = P.sb([128, TC], F32)
    b_q1, b_q2 = P.buf("q1"), P.buf("q2")
    xx = [P.sb([128, 2, TC], F32) for _ in range(2)]
    b_xx = [P.buf(f"xx{i}") for i in range(2)]
    carry = P.sb([128, 4, 2], F32)
    b_carry = [P.buf(f"carry{j}") for j in range(4)]
    ps_y = [P.ps([128, 512], F32) for _ in range(2)]
    b_psy = [P.buf(f"psy{i}") for i in range(2)]
    y_sb = [P.sb([128, TC], F32) for _ in range(2)]
    b_y = [P.buf(f"y{i}") for i in range(2)]
    it = 0
    for d in range(2):
        for j in range(4):
            P.op("pool", lambda e, j=j: e.memset(carry[:, j, :], 0.0), writes=[b_carry[j]])
        for n in range(nchunks):
            L = min(TC, NTOK - n * TC)
            un = n % 2
            P.dma("sp", lambda e, d=d, n=n, L=L, un=un: e.dma_start(out=u_sb[un][:, 0:L], in_=uT[d, :, n * TC:n * TC + L]), b_u[un], "w")
            yn = n % 2
            for j in range(4):
                dj = d * 4 + j
                par = it % 2
                it += 1
                for ri in range(2):
                    P.op("pe", lambda e, ri=ri, dj=dj, par=par, L=L, un=un: e.matmul(ps_b[par][:, ri, 0:L], lhsT=lhsB[:, ri, dj, :], rhs=u_sb[un][:, 0:L], start=True, stop=True),
                         reads=[b_lhsB, b_u[un]], writes=[b_psb[par]])
                P.op("act", lambda e, par=par, L=L: e.copy(out=bu[par][:, :, 0:L], in_=ps_b[par][:, :, 0:L]), reads=[b_psb[par]], writes=[b_bu[par]])
                cT, sT, mT = cosT[:, dj, 0:L], sinT[:, dj, 0:L], magT[:, dj, 0:L]
                bt = b_tab[dj]
                bre, bim = bu[par][:, 0, 0:L], bu[par][:, 1, 0:L]
                P.op("pool", lambda e, L=L, bre=bre, cT=cT: e.tensor_tensor(out=r1[:, 0:L], in0=bre, in1=cT, op=ALU.mult), reads=[b_bu[par], bt], writes=[b_r1])
                P.op("pool", lambda e, L=L, bim=bim, sT=sT: e.tensor_tensor(out=r2[:, 0:L], in0=bim, in1=sT, op=ALU.mult), reads=[b_bu[par], bt], writes=[b_r2])
                P.op("pool", lambda e, L=L, par=par: e.tensor_tensor(out=vv[par][:, 0, 0:L], in0=r1[:, 0:L], in1=r2[:, 0:L], op=ALU.add), reads=[b_r1, b_r2], writes=[b_vv[par]])
                P.op("pool", lambda e, L=L, bim=bim, cT=cT: e.tensor_tensor(out=r1[:, 0:L], in0=bim, in1=cT, op=ALU.mult), reads=[b_bu[par], bt, b_vv[par]], writes=[b_r1])
                P.op("pool", lambda e, L=L, bre=bre, sT=sT: e.tensor_tensor(out=r2[:, 0:L], in0=bre, in1=sT, op=ALU.mult), reads=[b_bu[par], bt, b_vv[par]], writes=[b_r2])
                P.op("pool", lambda e, L=L, par=par: e.tensor_tensor(out=vv[par][:, 1, 0:L], in0=r1[:, 0:L], in1=r2[:, 0:L], op=ALU.subtract), reads=[b_r1, b_r2], writes=[b_vv[par]])
                for ri in range(2):
                    P.op("dve", lambda e, L=L, par=par, ri=ri, mT=mT, j=j: e.tensor_tensor_scan(out=ww[:, ri, 0:L], data0=mT, data1=vv[par][:, ri, 0:L],
                                                                                             initial=carry[:, j, ri:ri + 1], op0=ALU.mult, op1=ALU.add),
                         reads=[bt, b_vv[par], b_carry[j]], writes=[b_ww])
                wre, wim = ww[:, 0, 0:L], ww[:, 1, 0:L]
                P.op("dve", lambda e, L=L, wre=wre, cT=cT: e.tensor_tensor(out=q1[:, 0:L], in0=wre, in1=cT, op=ALU.mult), reads=[b_ww, bt], writes=[b_q1])
                P.op("dve", lambda e, L=L, wim=wim, sT=sT: e.tensor_tensor(out=q2[:, 0:L], in0=wim, in1=sT, op=ALU.mult), reads=[b_ww, bt], writes=[b_q2])
                P.op("dve", lambda e, L=L, par=par: e.tensor_tensor(out=xx[par][:, 0, 0:L], in0=q1[:, 0:L], in1=q2[:, 0:L], op=ALU.subtract), reads=[b_q1, b_q2], writes=[b_xx[par]])
                P.op("dve", lambda e, L=L, wre=wre, sT=sT: e.tensor_tensor(out=q1[:, 0:L], in0=wre, in1=sT, op=ALU.mult), reads=[b_ww, bt, b_xx[par]], writes=[b_q1])
                P.op("dve", lambda e, L=L, wim=wim, cT=cT: e.tensor_tensor(out=q2[:, 0:L], in0=wim, in1=cT, op=ALU.mult), reads=[b_ww, bt, b_xx[par]], writes=[b_q2])
                P.op("dve", lambda e, L=L, par=par: e.tensor_tensor(out=xx[par][:, 1, 0:L], in0=q1[:, 0:L], in1=q2[:, 0:L], op=ALU.add), reads=[b_q1, b_q2], writes=[b_xx[par]])
                P.op("act", lambda e, L=L, par=par, j=j: e.copy(out=carry[:, j, :], in_=xx[par][:, :, L - 1]), reads=[b_xx[par]], writes=[b_carry[j]])
                for ri in range(2):
                    P.op("pe", lambda e, ri=ri, dj=dj, par=par, L=L, yn=yn, j=j: e.matmul(ps_y[yn][:, 0:L], lhsT=cE[:, ri, dj, :], rhs=xx[par][:, ri, 0:L],
                                                                                       start=(j == 0 and ri == 0), stop=(j == 3 and ri == 1)),
                         reads=[b_cE, b_xx[par]], writes=[b_psy[yn]])
            P.op("act", lambda e, L=L, yn=yn: e.copy(out=y_sb[yn][:, 0:L], in_=ps_y[yn][:, 0:L]), reads=[b_psy[yn]], writes=[b_y[yn]])
            P.dma("sp", lambda e, d=d, n=n, L=L, yn=yn: e.dma_start(out=yT[d, :, n * TC:n * TC + L], in_=y_sb[yn][:, 0:L]), b_y[yn], "r")
    P.emit()
    return nc


def s5_host_inputs(su_x, su_c, a_re, a_im, log_dt, b_re, b_im, c_re, c_im, gh):
    ch = slice(gh * 128, gh * 128 + 128)
    fwd = np.concatenate([su_c[:, ch], su_x[:, ch]], axis=0)
    bwd = np.concatenate([su_x[:, ch], su_c[:, ch]], axis=0)[::-1]
    uT = np.ascontiguousarray(np.stack([fwd.T, bwd.T], axis=0))
    g0 = gh * 8
    prm = np.zeros((128, 3, 8), np.float32)
    bm = np.zeros((128, 2, 8, 16), np.float32)
    cm = np.zeros((128, 2, 8, 16), np.float32)
    for d in range(2):
        for j in range(4):
            for half in range(2):
                g = g0 + 2 * j + half
                sl = slice(half * 64, half * 64 + 64)
                prm[sl, 0, d * 4 + j] = a_re[d, g]
                prm[sl, 1, d * 4 + j] = a_im[d, g]
                prm[sl, 2, d * 4 + j] = log_dt[d, g]
                bm[sl, 0, d * 4 + j] = b_re[d, g]
                bm[sl, 1, d * 4 + j] = b_im[d, g]
                cm[sl, 0, d * 4 + j] = c_re[d, g].T
                cm[sl, 1, d * 4 + j] = c_im[d, g].T
    return {"uT": uT, "prm": prm, "bmat": bm, "cmat": cm}


def gelu_ops(P, x_ap, out_ap, g1, g2, b_x, b_g1, b_g2, b_out):
    P.op("dve", lambda e: e.tensor_tensor(out=g1, in0=x_ap, in1=x_ap, op=ALU.mult), reads=[b_x], writes=[b_g1])
    P.op("dve", lambda e: e.tensor_scalar(out=g1, in0=g1, scalar1=0.044715, scalar2=1.0, op0=ALU.mult, op1=ALU.add), reads=[b_g1], writes=[b_g1])
    P.op("dve", lambda e: e.tensor_tensor(out=g1, in0=g1, in1=x_ap, op=ALU.mult), reads=[b_g1, b_x], writes=[b_g1])
    P.op("act", lambda e: e.activation(out=g2, in_=g1, func=AF.Sigmoid, scale=1.5957691216057308), reads=[b_g1], writes=[b_g2])
    P.op("dve", lambda e: e.tensor_tensor(out=out_ap, in0=g2, in1=x_ap, op=ALU.mult), reads=[b_g2, b_x], writes=[b_out])


def build_p3a(NT, NTX, moe):
    nc = bass.Bass("TRN2", target_bir_lowering=False)
    T = NT * 128
    xin = nc.dram_tensor("xin", [T, DM], F32, kind="ExternalInput").ap()
    br = nc.dram_tensor("br", [T, 6, 256], F32, kind="ExternalInput").ap()
    gates = nc.dram_tensor("gates", [T, 4096], BF16, kind="ExternalInput").ap()
    mod = nc.dram_tensor("mod", [2, 6 * DM], F32, kind="ExternalInput").ap()
    gvec = nc.dram_tensor("gvec", [2, DM], F32, kind="ExternalInput").ap()
    s5v = nc.dram_tensor("s5v", [2, 256], F32, kind="ExternalInput").ap()
    glu_w = nc.dram_tensor("glu_w", [256, 256], F32, kind="ExternalInput").ap()
    w_br = nc.dram_tensor("w_br", [1024, DM], F32, kind="ExternalInput").ap()
    w_out = nc.dram_tensor("w_out", [DM, DM], F32, kind="ExternalInput").ap()
    router = nc.dram_tensor("router", [DM, 8], F32, kind="ExternalInput").ap()
    x1o = nc.dram_tensor("x1", [T, DM], F32, kind="ExternalOutput").ap()
    h2o = nc.dram_tensor("h2", [T, DM], F32, kind="ExternalOutput").ap()
    combo = nc.dram_tensor("comb", [T, 8], F32, kind="ExternalOutput").ap()
    P = Prog(nc)
    ident, ident_f, b_id = make_ident(P)
    wbr = P.sb([128, 8, DM], BF16)
    wo = P.sb([128, 8, DM], BF16)
    wg = P.sb([128, 2, 256], BF16)
    rt_sb = P.sb([128, 8, 8], F32)
    b_wt = P.buf("wt")
    P.dma("pool", lambda e: e.dma_start(out=wbr[:], in_=w_br.rearrange("(k p) n -> p k n", p=128)), b_wt, "w")
    P.dma("pool", lambda e: e.dma_start(out=wo[:], in_=w_out.rearrange("(k p) n -> p k n", p=128)), b_wt, "w")
    P.dma("pool", lambda e: e.dma_start(out=wg[:], in_=glu_w.rearrange("(k p) n -> p k n", p=128)), b_wt, "w")
    b_wr = P.buf("wr")
    P.dma("sp", lambda e: e.dma_start(out=rt_sb[:], in_=router.rearrange("(k p) n -> p k n", p=128)), b_wr, "w")
    drow = P.sb([128, 256], F32)
    gbrow = P.sb([128, 256], F32)
    P.dma("sp", lambda e: e.dma_start(out=drow[:], in_=s5v[0, :].partition_broadcast(128)), b_wr, "w")
    P.dma("sp", lambda e: e.dma_start(out=gbrow[:], in_=s5v[1, :].partition_broadcast(128)), b_wr, "w")
    gpm = P.sb([128, DM], F32)
    gpf = P.sb([128, DM], F32)
    b_gv = P.buf("gv")
    P.dma("sp", lambda e: e.dma_start(out=gpm[:], in_=gvec[0, :].partition_broadcast(128)), b_gv, "w")
    P.dma("sp", lambda e: e.dma_start(out=gpf[:], in_=gvec[1, :].partition_broadcast(128)), b_gv, "w")
    G2 = [P.sb([128, DM], F32) for _ in range(2)]
    G4 = [P.sb([128, DM], F32) for _ in range(2)]
    SH3 = [P.sb([128, DM], F32) for _ in range(2)]
    b_G = [P.buf(f"G{c}") for c in range(2)]
    for c in range(2):
        P.dma("sp", lambda e, c=c: e.dma_start(out=G2[c][:], in_=mod[c, 2 * DM:3 * DM].partition_broadcast(128)), b_G[c], "w")
        P.dma("sp", lambda e, c=c: e.dma_start(out=SH3[c][:], in_=mod[c, 3 * DM:4 * DM].partition_broadcast(128)), b_G[c], "w")
        P.dma("sp", lambda e, c=c: e.dma_start(out=G4[c][:], in_=mod[c, 4 * DM:5 * DM].partition_broadcast(128)), b_G[c], "w")
        P.op("dve", lambda e, c=c: e.tensor_tensor(out=G2[c][:], in0=G2[c][:], in1=gpm[:], op=ALU.mult), reads=[b_G[c], b_gv], writes=[b_G[c]])
        P.op("dve", lambda e, c=c: e.scalar_tensor_tensor(out=G4[c][:], in0=G4[c][:], scalar=1.0, in1=gpf[:], op0=ALU.add, op1=ALU.mult),
             reads=[b_G[c], b_gv], writes=[b_G[c]])
    x_sb = [P.sb([128, DM], F32) for _ in range(2)]
    b_x = [P.buf(f"x{i}") for i in range(2)]
    br_sb = [P.sb([128, 6, 256], F32) for _ in range(2)]
    b_br = [P.buf(f"br{i}") for i in range(2)]
    gt_sb = [P.sb([128, 4096], BF16) for _ in range(2)]
    b_gt = [P.buf(f"gt{i}") for i in range(2)]
    yv = P.sb([128, 256], F32); b_yv = P.buf("yv")
    g1 = P.sb([128, 256], F32); b_g1 = P.buf("g1")
    g2 = P.sb([128, 256], F32); b_g2 = P.buf("g2")
    gy = P.sb([128, 256], F32); b_gy = P.buf("gy")
    gyb = P.sb([128, 256], BF16); b_gyb = P.buf("gyb")
    gyT = P.sb([128, 2, 128], BF16); b_gyT = P.buf("gyT")
    brb = P.sb([128, 4, 256], BF16); b_brb = P.buf("brb")
    brT = P.sb([128, 8, 128], BF16); b_brT = P.buf("brT")
    m_sb = P.sb([128, DM], F32); b_m = P.buf("m")
    tmp = P.sb([128, DM], F32); b_tmp = P.buf("tmp")
    mb = P.sb([128, DM], BF16); b_mb = P.buf("mb")
    mT = P.sb([128, 8, 128], BF16); b_mT = P.buf("mT")
    x1 = [P.sb([128, DM], F32) for _ in range(2)]
    b_x1 = [P.buf(f"x1{i}") for i in range(2)]
    h2 = [P.sb([128, DM], F32) for _ in range(2)]
    b_h2 = [P.buf(f"h2{i}") for i in range(2)]
    scr = P.sb([128, DM], F32); b_scr = P.buf("scr")
    st = P.sb([128, 8], F32); b_st = P.buf("st")
    hTf = P.sb([128, 8, 128], F32); b_hTf = P.buf("hTf")
    lg = P.sb([128, 4, 8], F32); b_lg = P.buf("lg")
    cmb = [P.sb([128, 8], F32) for _ in range(2)]
    b_cmb = [P.buf(f"cmb{i}") for i in range(2)]
    psT = P.ps([128, 8, 128], BF16); b_psT = P.buf("psT")
    psg = P.ps([128, 256], F32); b_psg = P.buf("psg")
    psb = [P.ps([128, 512], F32) for _ in range(2)]
    b_psb = [P.buf(f"psb{i}") for i in range(2)]
    psm = [P.ps([128, 512], F32) for _ in range(2)]
    b_psm = [P.buf(f"psm{i}") for i in range(2)]
    psTf = [P.ps([128, 4, 128], F32) for _ in range(2)]
    b_psTf = [P.buf(f"psTf{i}") for i in range(2)]
    for t in range(NT):
        j = t % 2
        cls = 0 if t < NTX else 1
        rows = slice(t * 128, (t + 1) * 128)
        P.dma("sp", lambda e, j=j, rows=rows: e.dma_start(out=x_sb[j][:], in_=xin[rows, :]), b_x[j], "w")
        P.dma("sp", lambda e, j=j, rows=rows: e.dma_start(out=br_sb[j][:], in_=br[rows]), b_br[j], "w")
        P.dma("sp", lambda e, j=j, rows=rows: e.dma_start(out=gt_sb[j][:], in_=gates[rows, :]), b_gt[j], "w")
        B = br_sb[j]
        P.op("dve", lambda e, B=B: e.tensor_tensor(out=yv[:], in0=B[:, 3, :], in1=drow[:], op=ALU.mult), reads=[b_br[j], b_wr], writes=[b_yv])
        P.op("dve", lambda e, B=B: e.tensor_tensor(out=yv[:], in0=yv[:], in1=B[:, 4, :], op=ALU.add), reads=[b_br[j], b_yv], writes=[b_yv])
        P.op("dve", lambda e, B=B: e.tensor_tensor(out=yv[:], in0=yv[:], in1=B[:, 5, :], op=ALU.add), reads=[b_br[j], b_yv], writes=[b_yv])
        gelu_ops(P, yv[:], gy[:], g1[:], g2[:], b_yv, b_g1, b_g2, b_gy)
        P.op("act", lambda e: e.copy(out=gyb[:], in_=gy[:]), reads=[b_gy], writes=[b_gyb])
        for k in range(2):
            P.op("pe", lambda e, k=k: e.transpose(out=psT[:, k, :], in_=gyb[:, k * 128:(k + 1) * 128], identity=ident[:]), reads=[b_gyb, b_id], writes=[b_psT])
        P.op("act", lambda e: e.copy(out=gyT[:], in_=psT[:, 0:2, :]), reads=[b_psT], writes=[b_gyT])
        for k in range(2):
            P.op("pe", lambda e, k=k: e.matmul(psg[:], lhsT=gyT[:, k, :], rhs=wg[:, k, :], start=(k == 0), stop=(k == 1)), reads=[b_gyT, b_wt], writes=[b_psg])
        P.op("dve", lambda e: e.tensor_tensor(out=g1[:], in0=psg[:], in1=gbrow[:], op=ALU.add), reads=[b_psg, b_wr], writes=[b_g1])
        P.op("act", lambda e: e.activation(out=g2[:], in_=g1[:], func=AF.Sigmoid), reads=[b_g1], writes=[b_g2])
        P.op("dve", lambda e: e.tensor_tensor(out=brb[:, 2, :], in0=gy[:], in1=g2[:], op=ALU.mult), reads=[b_gy, b_g2], writes=[b_brb])
        P.op("act", lambda e, B=B: e.copy(out=brb[:, 0:2, :], in_=B[:, 0:2, :]), reads=[b_br[j], b_brb], writes=[b_brb])
        P.op("act", lambda e, B=B: e.copy(out=brb[:, 3, :], in_=B[:, 2, :]), reads=[b_br[j], b_brb], writes=[b_brb])
        for i in range(4):
            for k in range(2):
                P.op("pe", lambda e, i=i, k=k: e.transpose(out=psT[:, i * 2 + k, :], in_=brb[:, i, k * 128:(k + 1) * 128], identity=ident[:]),
                     reads=[b_brb, b_id], writes=[b_psT])
        P.op("act", lambda e: e.copy(out=brT[:], in_=psT[:]), reads=[b_psT], writes=[b_brT])
        for i in range(4):
            for hf in range(2):
                for k in range(2):
                    P.op("pe", lambda e, i=i, hf=hf, k=k: e.matmul(psb[hf][:], lhsT=brT[:, i * 2 + k, :], rhs=wbr[:, i * 2 + k, hf * 512:(hf + 1) * 512],
                                                                   start=(k == 0), stop=(k == 1)),
                         reads=[b_brT, b_wt], writes=[b_psb[hf]])
                gap = gt_sb[j][:, i * DM + hf * 512:i * DM + (hf + 1) * 512]
                if i == 0:
                    P.op("dve", lambda e, hf=hf, gap=gap: e.tensor_tensor(out=m_sb[:, hf * 512:(hf + 1) * 512], in0=psb[hf][:], in1=gap, op=ALU.mult),
                         reads=[b_psb[hf], b_gt[j]], writes=[b_m])
                else:
                    P.op("dve", lambda e, hf=hf, gap=gap: e.tensor_tensor(out=tmp[:, hf * 512:(hf + 1) * 512], in0=psb[hf][:], in1=gap, op=ALU.mult),
                         reads=[b_psb[hf], b_gt[j]], writes=[b_tmp])
                    P.op("pool", lambda e, hf=hf: e.tensor_tensor(out=m_sb[:, hf * 512:(hf + 1) * 512], in0=m_sb[:, hf * 512:(hf + 1) * 512],
                                                                  in1=tmp[:, hf * 512:(hf + 1) * 512], op=ALU.add),
                         reads=[b_tmp, b_m], writes=[b_m])
        P.op("act", lambda e: e.copy(out=mb[:], in_=m_sb[:]), reads=[b_m], writes=[b_mb])
        for k in range(8):
            P.op("pe", lambda e, k=k: e.transpose(out=psT[:, k, :], in_=mb[:, k * 128:(k + 1) * 128], identity=ident[:]), reads=[b_mb, b_id], writes=[b_psT])
        P.op("act", lambda e: e.copy(out=mT[:], in_=psT[:]), reads=[b_psT], writes=[b_mT])
        for hf in range(2):
            for k in range(8):
                P.op("pe", lambda e, hf=hf, k=k: e.matmul(psm[hf][:], lhsT=mT[:, k, :], rhs=wo[:, k, hf * 512:(hf + 1) * 512], start=(k == 0), stop=(k == 7)),
                     reads=[b_mT, b_wt], writes=[b_psm[hf]])
        for hf in range(2):
            P.op("act", lambda e, hf=hf: e.activation(out=scr[:, hf * 512:(hf + 1) * 512], in_=psm[hf][:], func=AF.Square, accum_out=st[:, hf:hf + 1]),
                 reads=[b_psm[hf]], writes=[b_scr, b_st])
        P.op("dve", lambda e: e.tensor_tensor(out=st[:, 2:3], in0=st[:, 0:1], in1=st[:, 1:2], op=ALU.add), reads=[b_st], writes=[b_st])
        P.op("dve", lambda e: e.tensor_scalar(out=st[:, 2:3], in0=st[:, 2:3], scalar1=1.0 / DM, scalar2=EPS, op0=ALU.mult, op1=ALU.add), reads=[b_st], writes=[b_st])
        P.op("act", lambda e: e.activation(out=st[:, 2:3], in_=st[:, 2:3], func=AF.Sqrt), reads=[b_st], writes=[b_st])
        P.op("dve", lambda e: e.reciprocal(out=st[:, 3:4], in_=st[:, 2:3]), reads=[b_st], writes=[b_st])
        for hf in range(2):
            P.op("dve", lambda e, hf=hf, cls=cls: e.scalar_tensor_tensor(out=tmp[:, hf * 512:(hf + 1) * 512], in0=psm[hf][:], scalar=st[:, 3:4],
                                                                       in1=G2[cls][:, hf * 512:(hf + 1) * 512], op0=ALU.mult, op1=ALU.mult),
                 reads=[b_psm[hf], b_st, b_G[cls]], writes=[b_tmp])
        P.op("pool", lambda e, j=j: e.tensor_tensor(out=x1[j][:], in0=tmp[:], in1=x_sb[j][:], op=ALU.add), reads=[b_tmp, b_x[j]], writes=[b_x1[j]])
        P.dma("sp", lambda e, j=j, rows=rows: e.dma_start(out=x1o[rows, :], in_=x1[j][:]), b_x1[j], "r")
        rms_rstd(P, x1[j][:], scr[:], st[:, 4:5], st[:, 5:6], b_x1[j], b_scr, b_st)
        P.op("dve", lambda e, j=j, cls=cls: e.scalar_tensor_tensor(out=tmp[:], in0=x1[j][:], scalar=st[:, 5:6], in1=G4[cls][:], op0=ALU.mult, op1=ALU.mult),
             reads=[b_x1[j], b_st, b_G[cls]], writes=[b_tmp])
        P.op("pool", lambda e, j=j, cls=cls: e.tensor_tensor(out=h2[j][:], in0=tmp[:], in1=SH3[cls][:], op=ALU.add), reads=[b_tmp, b_G[cls]], writes=[b_h2[j]])
        P.dma("sp", lambda e, j=j, rows=rows: e.dma_start(out=h2o[rows, :], in_=h2[j][:]), b_h2[j], "r")
        if moe:
            for r in range(2):
                for k in range(4):
                    kk = r * 4 + k
                    P.op("pe", lambda e, r=r, k=k, kk=kk, j=j: e.transpose(out=psTf[r][:, k, :], in_=h2[j][:, kk * 128:(kk + 1) * 128], identity=ident_f[:]),
                         reads=[b_h2[j], b_id], writes=[b_psTf[r]])
                P.op("act", lambda e, r=r: e.copy(out=hTf[:, r * 4:(r + 1) * 4, :], in_=psTf[r][:]), reads=[b_psTf[r]], writes=[b_hTf])
            for k in range(8):
                P.op("pe", lambda e, k=k: e.matmul(psg[:, 0:8], lhsT=hTf[:, k, :], rhs=rt_sb[:, k, :], start=(k == 0), stop=(k == 7)),
                     reads=[b_hTf, b_wr], writes=[b_psg])
            L0, M1, L2, M2 = lg[:, 0, :], lg[:, 1, :], lg[:, 2, :], lg[:, 3, :]
            sa, sb_, sc, sd = st[:, 6:7], st[:, 7:8], st[:, 0:1], st[:, 1:2]
            P.op("dve", lambda e: e.tensor_copy(out=L0, in_=psg[:, 0:8]), reads=[b_psg], writes=[b_lg])
            P.op("dve", lambda e: e.reduce_max(out=sa, in_=L0, axis=AX.X), reads=[b_lg], writes=[b_st])
            P.op("dve", lambda e: e.tensor_scalar(out=M1, in0=L0, scalar1=sa, scalar2=None, op0=ALU.is_equal), reads=[b_lg, b_st], writes=[b_lg])
            P.op("dve", lambda e: e.scalar_tensor_tensor(out=L2, in0=M1, scalar=-1e30, in1=L0, op0=ALU.mult, op1=ALU.add), reads=[b_lg], writes=[b_lg])
            P.op("dve", lambda e: e.reduce_max(out=sb_, in_=L2, axis=AX.X), reads=[b_lg], writes=[b_st])
            P.op("dve", lambda e: e.tensor_scalar(out=M2, in0=L2, scalar1=sb_, scalar2=None, op0=ALU.is_equal), reads=[b_lg, b_st], writes=[b_lg])
            P.op("dve", lambda e: e.tensor_tensor(out=sc, in0=sb_, in1=sa, op=ALU.subtract), reads=[b_st], writes=[b_st])
            P.op("act", lambda e: e.activation(out=sc, in_=sc, func=AF.Exp), reads=[b_st], writes=[b_st])
            P.op("dve", lambda e: e.tensor_scalar(out=sd, in0=sc, scalar1=1.0, scalar2=None, op0=ALU.add), reads=[b_st], writes=[b_st])
            P.op("dve", lambda e: e.reciprocal(out=sd, in_=sd), reads=[b_st], writes=[b_st])
            P.op("dve", lambda e: e.tensor_tensor(out=sc, in0=sc, in1=sd, op=ALU.mult), reads=[b_st], writes=[b_st])
            P.op("dve", lambda e: e.tensor_scalar(out=M2, in0=M2, scalar1=sc, scalar2=None, op0=ALU.mult), reads=[b_lg, b_st], writes=[b_lg])
            P.op("dve", lambda e, j=j: e.scalar_tensor_tensor(out=cmb[j][:], in0=M1, scalar=sd, in1=M2, op0=ALU.mult, op1=ALU.add),
                 reads=[b_lg, b_st], writes=[b_cmb[j]])
        else:
            P.op("pool", lambda e, j=j: e.memset(cmb[j][:], 1.0), writes=[b_cmb[j]])
        P.dma("sp", lambda e, j=j, rows=rows: e.dma_start(out=combo[rows, :], in_=cmb[j][:]), b_cmb[j], "r")
    P.emit()
    return nc


def build_p3b(NT, NTX, E, FCH, TBT=8):
    nc = bass.Bass("TRN2", target_bir_lowering=False)
    T = NT * 128
    F = FCH * 128
    hT = nc.dram_tensor("hT", [DM, T], F32, kind="ExternalInput").ap()
    x1d = nc.dram_tensor("x1", [T, DM], F32, kind="ExternalInput").ap()
    combd = nc.dram_tensor("comb", [T, 8], F32, kind="ExternalInput").ap()
    mod = nc.dram_tensor("mod", [2, 6 * DM], F32, kind="ExternalInput").ap()
    gpost = nc.dram_tensor("gpost", [DM], F32, kind="ExternalInput").ap()
    w1 = nc.dram_tensor("w1", [E, DM, F], F32, kind="ExternalInput").ap()
    w3 = nc.dram_tensor("w3", [E, DM, F], F32, kind="ExternalInput").ap()
    w2 = nc.dram_tensor("w2", [E, F, DM], F32, kind="ExternalInput").ap()
    xo = nc.dram_tensor("xo", [T, DM], F32, kind="ExternalOutput").ap()
    P = Prog(nc)
    grow = P.sb([128, DM], F32)
    b_grow = P.buf("grow")
    P.dma("sp", lambda e: e.dma_start(out=grow[:], in_=gpost.partition_broadcast(128)), b_grow, "w")
    G5 = [P.sb([128, DM], F32) for _ in range(2)]
    b_G5 = [P.buf(f"G5{c}") for c in range(2)]
    for c in range(2):
        P.dma("sp", lambda e, c=c: e.dma_start(out=G5[c][:], in_=mod[c, 5 * DM:6 * DM].partition_broadcast(128)), b_G5[c], "w")
        P.op("dve", lambda e, c=c: e.tensor_tensor(out=G5[c][:], in0=G5[c][:], in1=grow[:], op=ALU.mult), reads=[b_G5[c], b_grow], writes=[b_G5[c]])
    TBK = TBT * 128
    hx = P.sb([128, 8, TBK], BF16); b_hx = P.buf("hx")
    aT = P.sb([128, FCH, TBK], BF16)
    b_aT = [P.buf(f"aT{f}") for f in range(FCH)]
    w2_sb = P.sb([128, FCH, DM], BF16); b_w2 = P.buf("w2")
    FG = 2
    w1g = [P.sb([128, 8, FG * 128], BF16) for _ in range(2)]
    w3g = [P.sb([128, 8, FG * 128], BF16) for _ in range(2)]
    b_w1g = [P.buf(f"w1g{i}") for i in range(2)]
    b_w3g = [P.buf(f"w3g{i}") for i in range(2)]
    yacc = P.sb([128, TBT, DM], F32)
    b_yacc = [P.buf(f"yacc{i}") for i in range(TBT)]
    cmb = P.sb([128, TBT, 8], F32); b_cmb = P.buf("cmb")
    s_sb = [P.sb([128, 512], BF16) for _ in range(2)]
    b_s = [P.buf(f"s{i}") for i in range(2)]
    x1_sb = [P.sb([128, DM], F32) for _ in range(2)]
    b_x1 = [P.buf(f"x1{i}") for i in range(2)]
    scr = P.sb([128, DM], F32); b_scr = P.buf("scr")
    st = [P.sb([128, 2], F32) for _ in range(2)]
    b_st = [P.buf(f"st{i}") for i in range(2)]
    ps1 = [P.ps([128, 512], F32) for _ in range(2)]
    ps3 = [P.ps([128, 512], F32) for _ in range(2)]
    b_ps1 = [P.buf(f"ps1{i}") for i in range(2)]
    b_ps3 = [P.buf(f"ps3{i}") for i in range(2)]
    psy = [P.ps([128, 512], F32) for _ in range(2)]
    b_psy = [P.buf(f"psy{i}") for i in range(2)]
    it1 = 0
    it2 = 0
    itg = 0
    ito = 0
    for tb0 in range(0, NT, TBT):
        ntile = min(TBT, NT - tb0)
        ntok = ntile * 128
        tok0 = tb0 * 128
        for k in range(8):
            P.dma("pool", lambda e, k=k, ntok=ntok, tok0=tok0: e.dma_start(out=hx[:, k, 0:ntok], in_=hT[k * 128:(k + 1) * 128, tok0:tok0 + ntok]), b_hx, "w")
        P.dma("sp", lambda e, ntile=ntile, tok0=tok0, ntok=ntok: e.dma_start(out=cmb[:, 0:ntile, :],
                                                                  in_=combd[tok0:tok0 + ntok, :].rearrange("(t p) e -> p t e", p=128)), b_cmb, "w")
        subs = [(s0, min(512, ntok - s0)) for s0 in range(0, ntok, 512)]
        for ex in range(E):
            for f0 in range(0, FCH, 4):
                f1 = min(FCH, f0 + 4)
                P.dma("pool", lambda e, ex=ex, f0=f0, f1=f1: e.dma_start(out=w2_sb[:, f0:f1, :],
                                                                    in_=w2[ex, f0 * 128:f1 * 128, :].rearrange("(f p) n -> p f n", p=128)), b_w2, "w")
            for f0 in range(0, FCH, FG):
                f1 = min(FCH, f0 + FG)
                gi = itg % 2
                itg += 1
                wcols = (f1 - f0) * 128
                P.dma("pool", lambda e, ex=ex, f0=f0, wcols=wcols, gi=gi: e.dma_start(out=w1g[gi][:, :, 0:wcols],
                                                                            in_=w1[ex, :, f0 * 128:f0 * 128 + wcols].rearrange("(k p) n -> p k n", p=128)), b_w1g[gi], "w")
                P.dma("pool", lambda e, ex=ex, f0=f0, wcols=wcols, gi=gi: e.dma_start(out=w3g[gi][:, :, 0:wcols],
                                                                            in_=w3[ex, :, f0 * 128:f0 * 128 + wcols].rearrange("(k p) n -> p k n", p=128)), b_w3g[gi], "w")
                for f in range(f0, f1):
                    fo = (f - f0) * 128
                    for (s0, sl) in subs:
                        q = it1 % 2
                        it1 += 1
                        for k in range(8):
                            P.op("pe", lambda e, q=q, k=k, gi=gi, fo=fo, s0=s0, sl=sl: e.matmul(ps1[q][:, 0:sl], lhsT=w1g[gi][:, k, fo:fo + 128], rhs=hx[:, k, s0:s0 + sl],
                                                                                             start=(k == 0), stop=(k == 7)),
                                 reads=[b_w1g[gi], b_hx], writes=[b_ps1[q]])
                        for k in range(8):
                            P.op("pe", lambda e, q=q, k=k, gi=gi, fo=fo, s0=s0, sl=sl: e.matmul(ps3[q][:, 0:sl], lhsT=w3g[gi][:, k, fo:fo + 128], rhs=hx[:, k, s0:s0 + sl],
                                                                                             start=(k == 0), stop=(k == 7)),
                                 reads=[b_w3g[gi], b_hx], writes=[b_ps3[q]])
                        P.op("act", lambda e, q=q, sl=sl: e.activation(out=s_sb[q][:, 0:sl], in_=ps1[q][:, 0:sl], func=AF.Silu), reads=[b_ps1[q]], writes=[b_s[q]])
                        P.op("dve", lambda e, q=q, sl=sl, f=f, s0=s0: e.tensor_tensor(out=aT[:, f, s0:s0 + sl], in0=ps3[q][:, 0:sl], in1=s_sb[q][:, 0:sl], op=ALU.mult),
                             reads=[b_ps3[q], b_s[q]], writes=[b_aT[f]])
            for tl in range(ntile):
                for hf in range(2):
                    q = it2 % 2
                    it2 += 1
                    for f in range(FCH):
                        P.op("pe", lambda e, q=q, f=f, tl=tl, hf=hf: e.matmul(psy[q][:], lhsT=aT[:, f, tl * 128:(tl + 1) * 128], rhs=w2_sb[:, f, hf * 512:(hf + 1) * 512],
                                                                             start=(f == 0), stop=(f == FCH - 1)),
                             reads=[b_aT[f], b_w2], writes=[b_psy[q]])
                    ya = yacc[:, tl, hf * 512:(hf + 1) * 512]
                    if ex == 0:
                        P.op("dve", lambda e, q=q, ya=ya, tl=tl: e.tensor_scalar(out=ya, in0=psy[q][:], scalar1=cmb[:, tl, 0:1], scalar2=None, op0=ALU.mult),
                             reads=[b_psy[q], b_cmb], writes=[b_yacc[tl]])
                    else:
                        P.op("dve", lambda e, q=q, ya=ya, tl=tl, ex=ex: e.scalar_tensor_tensor(out=ya, in0=psy[q][:], scalar=cmb[:, tl, ex:ex + 1], in1=ya,
                                                                                            op0=ALU.mult, op1=ALU.add),
                             reads=[b_psy[q], b_cmb, b_yacc[tl]], writes=[b_yacc[tl]])
        for tl in range(ntile):
            t = tb0 + tl
            cls = 0 if t < NTX else 1
            j = ito % 2
            ito += 1
            rows = slice(t * 128, (t + 1) * 128)
            P.dma("sp", lambda e, j=j, rows=rows: e.dma_start(out=x1_sb[j][:], in_=x1d[rows, :]), b_x1[j], "w")
            rms_rstd(P, yacc[:, tl, :], scr[:], st[j][:, 0:1], st[j][:, 1:2], b_yacc[tl], b_scr, b_st[j])
            P.op("dve", lambda e, j=j, tl=tl, cls=cls: e.scalar_tensor_tensor(out=scr[:], in0=yacc[:, tl, :], scalar=st[j][:, 1:2], in1=G5[cls][:],
                                                                            op0=ALU.mult, op1=ALU.mult),
                 reads=[b_yacc[tl], b_st[j], b_G5[cls]], writes=[b_scr])
            P.op("pool", lambda e, j=j: e.tensor_tensor(out=x1_sb[j][:], in0=scr[:], in1=x1_sb[j][:], op=ALU.add), reads=[b_scr, b_x1[j]], writes=[b_x1[j]])
            P.dma("sp", lambda e, j=j, rows=rows: e.dma_start(out=xo[rows, :], in_=x1_sb[j][:]), b_x1[j], "r")
    P.emit()
    return nc


def kernel(x, c, ctx, c_ctx, w_mod, b_mod, g_pre_mix, g_post_mix, g_pre_ffn, g_post_ffn, w_in,
           na_rpb, swa_sink, gmlp_ln_g, gmlp_ln_b, gmlp_ws, gmlp_bs,
           s5_a_re, s5_a_im, s5_log_dt, s5_b_re, s5_b_im, s5_c_re, s5_c_im, s5_d, s5_glu_w, s5_glu_b,
           w_branch, w_out, ffn_w1, ffn_w3, ffn_w2, moe_router, moe_w1, moe_w3, moe_w2):
    A = lambda a: np.ascontiguousarray(np.asarray(a, dtype=np.float32))
    x = A(x).copy()
    h_ctx = A(ctx).copy()
    depth = w_in.shape[0]
    mods = run_p0(A(c), A(c_ctx), A(w_mod), A(b_mod))
    pos = np.arange(SEQ)
    for l in range(depth):
        ctx_out = l < depth - 1
        nc = build_p1(34, 32)
        in_maps = []
        for core in range(NCORES):
            b, hf = core // 2, core % 2
            xin = np.concatenate([x[b, hf * HALF:(hf + 1) * HALF], h_ctx[b]], axis=0)
            pp = pos[hf * HALF:(hf + 1) * HALF]
            rope = rope_table(np.concatenate([pp // 64, np.zeros(CTX, np.int64)]), np.concatenate([pp % 64, np.zeros(CTX, np.int64)]),
                              np.concatenate([np.ones(HALF, np.float32), np.zeros(CTX, np.float32)]))
            in_maps.append({"xin": xin, "mod": A(mods[core][l]), "g_pre": A(g_pre_mix[l]), "w_in": A(w_in[l]), "rope_cs": rope,
                            "gm_ln": A(np.stack([gmlp_ln_g[l], gmlp_ln_b[l]])),
                            "gm_wsT": A(np.transpose(gmlp_ws[l], (2, 0, 1))), "gm_bsT": A(np.transpose(gmlp_bs[l], (1, 0)))})
        r1 = _run(nc, in_maps)
        xins = [m["xin"] for m in in_maps]

        def gather(name, width):
            fx = np.zeros((BATCH, SEQ, width), np.float32)
            fc = np.zeros((BATCH, CTX, width), np.float32)
            for core in range(NCORES):
                b, hf = core // 2, core % 2
                a = np.asarray(r1[core][name]).astype(np.float32)
                fx[b, hf * HALF:(hf + 1) * HALF] = a[:HALF]
                if hf == 0:
                    fc[b] = a[HALF:]
            return fx, fc
        zx, zc = gather("zmix", 2048)
        qx, qc = gather("qkr", 384)
        obx, obc = gather("o_b", 256)
        oax, oac = run_attn("na", zx[..., 0:256], zx[..., 256:512], zx[..., 512:768], zc[..., 0:256], zc[..., 256:512], zc[..., 512:768],
                            A(na_rpb[l]), None, ctx_out)
        odx, odc = run_attn("swa", qx[..., 0:256], qx[..., 256:384], zx[..., 1152:1280], qc[..., 0:256], qc[..., 256:384], zc[..., 1152:1280],
                            None, A(swa_sink[l]), ctx_out)
        nc = build_s5(SEQ + CTX)
        in_maps = []
        for core in range(NCORES):
            b, gh = core // 2, core % 2
            in_maps.append(s5_host_inputs(zx[b, :, 1792:2048], zc[b, :, 1792:2048], A(s5_a_re[l]), A(s5_a_im[l]), A(s5_log_dt[l]),
                                          A(s5_b_re[l]), A(s5_b_im[l]), A(s5_c_re[l]), A(s5_c_im[l]), gh))
        rs = _run(nc, in_maps)
        yfx = np.zeros((BATCH, SEQ, 256), np.float32); yfc = np.zeros((BATCH, CTX, 256), np.float32)
        ybx = np.zeros((BATCH, SEQ, 256), np.float32); ybc = np.zeros((BATCH, CTX, 256), np.float32)
        for core in range(NCORES):
            b, gh = core // 2, core % 2
            yT = np.asarray(rs[core]["yT"])
            yf = yT[0].T
            yb = yT[1].T[::-1]
            ch = slice(gh * 128, gh * 128 + 128)
            yfc[b, :, ch] = yf[:CTX]; yfx[b, :, ch] = yf[CTX:]
            ybx[b, :, ch] = yb[:SEQ]; ybc[b, :, ch] = yb[SEQ:]
        NT3 = 34 if ctx_out else 32
        moe = (l % 2 == 1)
        jj = l // 2
        nc = build_p3a(NT3, 32, moe)
        in_maps = []
        for core in range(NCORES):
            b, hf = core // 2, core % 2
            sl = slice(hf * HALF, (hf + 1) * HALF)
            parts_x = [oax[b, sl], obx[b, sl], odx[b, sl], zx[b, sl, 1792:2048], yfx[b, sl], ybx[b, sl]]
            brx = np.stack(parts_x, axis=1)
            if ctx_out:
                brc = np.stack([oac[b], obc[b], odc[b], zc[b, :, 1792:2048], yfc[b], ybc[b]], axis=1)
                brx = np.concatenate([brx, brc], axis=0)
            T3 = NT3 * 128
            in_maps.append({"xin": A(xins[core][:T3]), "br": A(brx), "gates": np.ascontiguousarray(np.asarray(r1[core]["gates"])[:T3]),
                            "mod": A(mods[core][l]), "gvec": A(np.stack([g_post_mix[l], g_pre_ffn[l]])),
                            "s5v": A(np.stack([s5_d[l], s5_glu_b[l]])), "glu_w": A(s5_glu_w[l]),
                            "w_br": A(np.reshape(w_branch[l], (1024, DM))), "w_out": A(w_out[l]),
                            "router": A(moe_router[jj]) if moe else np.zeros((DM, 8), np.float32)})
        r3 = _run(nc, in_maps)
        if moe:
            E, FCH = 8, 28
            W1, W3, W2 = A(moe_w1[jj]), A(moe_w3[jj]), A(moe_w2[jj])
        else:
            E, FCH = 1, 22
            W1, W3, W2 = A(ffn_w1[jj])[None], A(ffn_w3[jj])[None], A(ffn_w2[jj])[None]
        nc = build_p3b(NT3, 32, E, FCH)
        in_maps = []
        gp = A(g_post_ffn[l])
        for core in range(NCORES):
            in_maps.append({"hT": np.ascontiguousarray(np.asarray(r3[core]["h2"]).T), "x1": np.asarray(r3[core]["x1"]),
                            "comb": np.asarray(r3[core]["comb"]), "mod": A(mods[core][l]), "gpost": gp, "w1": W1, "w3": W3, "w2": W2})
        r4 = _run(nc, in_maps)
        for core in range(NCORES):
            b, hf = core // 2, core % 2
            xo = np.asarray(r4[core]["xo"])
            x[b, hf * HALF:(hf + 1) * HALF] = xo[:HALF]
            if ctx_out and hf == 0:
                h_ctx[b] = xo[HALF:]
    return x
```

```python
import contextlib
import math
import numpy as np
import ml_dtypes
ml_bf16 = ml_dtypes.bfloat16
import concourse.bass as bass
import concourse.mybir as mybir
from concourse.bass_utils import run_bass_kernel_spmd

F32 = mybir.dt.float32
BF16 = mybir.dt.bfloat16
AF = mybir.ActivationFunctionType
ALU = mybir.AluOpType
AX = mybir.AxisListType

NCORES = 8
DM = 1024
SEQ = 8192
BATCH = 4
CTX = 256
HALF = SEQ // 2
EPS = 1e-6


class Buf:
    __slots__ = ("name", "writer", "readers", "dsem", "dcount", "dmas")

    def __init__(self, name):
        self.name = name
        self.writer = None
        self.readers = []
        self.dsem = None
        self.dcount = 0
        self.dmas = {}


class Op:
    __slots__ = ("eng", "fn", "waits", "signals", "ordinal", "is_dma", "dbuf", "dval", "idx")


class Prog:
    ENGS = ("pe", "act", "dve", "pool", "sp")

    def __init__(self, nc, prefix=""):
        self.nc = nc
        self.prefix = prefix
        self.ops = []
        self.stack = contextlib.ExitStack()
        self.esem = {}
        self.n_t = 0
        self.dma_bufs = []
        self.sems = []

    def sb(self, shape, dtype, name=None):
        self.n_t += 1
        t = self.stack.enter_context(self.nc.sbuf_tensor(self.prefix + (name or f"sb{self.n_t}"), list(shape), dtype))
        return t

    def ps(self, shape, dtype, name=None):
        self.n_t += 1
        t = self.stack.enter_context(self.nc.psum_tensor(self.prefix + (name or f"ps{self.n_t}"), list(shape), dtype))
        return t

    def buf(self, name):
        return Buf(name)

    def _deps(self, op, reads, writes):
        deps = []
        for b in reads:
            if b.writer is not None:
                deps.append(b.writer)
            if "w" in b.dmas:
                deps.append(b.dmas["w"])
        for b in writes:
            if b.writer is not None:
                deps.append(b.writer)
            deps.extend(b.readers)
            deps.extend(b.dmas.values())
        return deps

    def op(self, eng, fn, reads=(), writes=()):
        o = Op()
        o.eng = eng
        o.fn = fn
        o.is_dma = False
        o.signals = False
        o.ordinal = None
        o.idx = len(self.ops)
        waits = []
        for d in self._deps(o, reads, writes):
            if d.is_dma:
                waits.append(("d", d.dbuf, d.dbuf.dcount))
            else:
                if d.eng == "pe" and eng == "pe":
                    continue
                d.signals = True
                waits.append(("e", d))
        o.waits = waits
        for b in reads:
            b.readers.append(o)
        for b in writes:
            b.writer = o
            b.readers = []
            b.dmas = {}
        self.ops.append(o)
        return o

    def dma(self, eng, fn, sbuf, direction, reads=(), writes=()):
        o = Op()
        o.eng = eng
        o.fn = fn
        o.is_dma = True
        o.signals = False
        o.ordinal = None
        o.idx = len(self.ops)
        waits = []
        deps = []
        if direction == "w":
            if sbuf.writer is not None:
                deps.append(sbuf.writer)
            deps.extend(sbuf.readers)
            if "r" in sbuf.dmas:
                deps.append(sbuf.dmas["r"])
        else:
            if sbuf.writer is not None:
                deps.append(sbuf.writer)
            if "w" in sbuf.dmas:
                deps.append(sbuf.dmas["w"])
        deps.extend(self._deps(o, reads, writes))
        for d in deps:
            if d.is_dma:
                waits.append(("d", d.dbuf, d.dbuf.dcount))
            else:
                d.signals = True
                waits.append(("e", d))
        o.waits = waits
        if sbuf.dsem is None:
            sbuf.dsem = self.nc.alloc_semaphore(name=f"{self.prefix}dq{len(self.dma_bufs)}")
            self.sems.append(sbuf.dsem)
            self.dma_bufs.append(sbuf)
        sbuf.dcount += 1
        o.dbuf = sbuf
        o.dval = sbuf.dcount
        if direction == "w":
            sbuf.writer = None
            sbuf.readers = []
            sbuf.dmas.pop("r", None)
        sbuf.dmas[direction] = o
        for b in reads:
            b.readers.append(o)
        for b in writes:
            b.writer = o
            b.readers = []
            b.dmas = {}
        self.ops.append(o)
        return o

    def emit(self):
        nc = self.nc
        for e in self.ENGS:
            self.esem[e] = nc.alloc_semaphore(name=f"{self.prefix}es_{e}")
            self.sems.append(self.esem[e])
        counts = {e: 0 for e in self.ENGS}
        for o in self.ops:
            if (not o.is_dma) and o.signals:
                counts[o.eng] += 1
                o.ordinal = counts[o.eng]
        per = {e: [o for o in self.ops if o.eng == e] for e in self.ENGS}
        final_d = [(b.dsem, 16 * b.dcount) for b in self.dma_bufs]
        esem = self.esem

        def replay(engname, eng):
            seen = {}
            for o in per[engname]:
                for w in o.waits:
                    if w[0] == "d":
                        sem, val = w[1].dsem, 16 * w[2]
                    else:
                        sem, val = esem[w[1].eng], w[1].ordinal
                    k = id(sem)
                    if seen.get(k, 0) >= val:
                        continue
                    seen[k] = val
                    eng.wait_ge(sem, val)
                ins = o.fn(eng)
                if o.is_dma:
                    ins.then_inc(o.dbuf.dsem, 16)
                elif o.signals:
                    ins.then_inc(esem[engname], 1)
            if engname == "sp":
                for sem, val in final_d:
                    eng.wait_ge(sem, val)

        with nc.Block(self.prefix + "blk") as block:
            @block.tensor
            def _(eng):
                replay("pe", eng)

            @block.scalar
            def _(eng):
                replay("act", eng)

            @block.vector
            def _(eng):
                replay("dve", eng)

            @block.gpsimd
            def _(eng):
                replay("pool", eng)

            @block.sync
            def _(eng):
                replay("sp", eng)
        nc.clear_and_free_semaphores(self.sems)
        nc.all_engine_barrier()
        self.stack.close()


def _run(nc, in_maps):
    res = run_bass_kernel_spmd(nc, in_maps, core_ids=list(range(NCORES)))
    return res.results


T_ALL = SEQ + CTX
NT_ALL = T_ALL // 128
NTXA = SEQ // 128


def rms_rstd(P, x_ap, scr_ap, ss_ap, rstd_ap, b_x, b_scr, b_st, d=DM):
    P.op("act", lambda e: e.activation(out=scr_ap, in_=x_ap, func=AF.Square, accum_out=ss_ap),
         reads=[b_x], writes=[b_scr, b_st])
    P.op("dve", lambda e: e.tensor_scalar(out=ss_ap, in0=ss_ap, scalar1=1.0 / d, scalar2=EPS, op0=ALU.mult, op1=ALU.add),
         reads=[b_st], writes=[b_st])
    P.op("act", lambda e: e.activation(out=ss_ap, in_=ss_ap, func=AF.Sqrt), reads=[b_st], writes=[b_st])
    P.op("dve", lambda e: e.reciprocal(out=rstd_ap, in_=ss_ap), reads=[b_st], writes=[b_st])


def make_ident(P, dtype=BF16):
    ident_f = P.sb([128, 128], F32)
    ident = P.sb([128, 128], dtype)
    b = P.buf("ident")
    P.op("pool", lambda e: e.memset(ident_f[:], 1.0), writes=[b])
    P.op("pool", lambda e: e.affine_select(out=ident_f[:], in_=ident_f[:], pattern=[[-1, 128]],
                                           compare_op=ALU.is_equal, fill=0.0, base=0, channel_multiplier=1),
         reads=[b], writes=[b])
    P.op("pool", lambda e: e.tensor_copy(out=ident[:], in_=ident_f[:]), reads=[b], writes=[b])
    return ident, ident_f, b


def gelu_ops(P, x_ap, out_ap, g1, g2, b_x, b_g1, b_g2, b_out):
    P.op("dve", lambda e: e.tensor_tensor(out=g1, in0=x_ap, in1=x_ap, op=ALU.mult), reads=[b_x], writes=[b_g1])
    P.op("dve", lambda e: e.tensor_scalar(out=g1, in0=g1, scalar1=0.044715, scalar2=1.0, op0=ALU.mult, op1=ALU.add), reads=[b_g1], writes=[b_g1])
    P.op("dve", lambda e: e.tensor_tensor(out=g1, in0=g1, in1=x_ap, op=ALU.mult), reads=[b_g1, b_x], writes=[b_g1])
    P.op("act", lambda e: e.activation(out=g2, in_=g1, func=AF.Sigmoid, scale=1.5957691216057308), reads=[b_g1], writes=[b_g2])
    P.op("dve", lambda e: e.tensor_tensor(out=out_ap, in0=g2, in1=x_ap, op=ALU.mult), reads=[b_g2, b_x], writes=[b_out])


NEG = -30000.0

def na_table(rpb, r0, row_start, nrows):
    H = rpb.shape[0]
    dr = np.arange(2)[:, None, None, None]
    c = np.arange(64)[None, :, None, None]
    wr = np.arange(nrows)[None, None, :, None]
    kc = np.arange(64)[None, None, None, :]
    r = r0 + dr
    kr = row_start + wr
    rs = np.clip(r - 4, 0, 120)
    cs = np.clip(c - 8, 0, 48)
    valid = (kr >= rs) & (kr < rs + 8) & (kc >= cs) & (kc < cs + 16) & (kr >= 0) & (kr < 128)
    ri = np.clip(kr - r + 7, 0, 14)
    ci = np.clip(kc - c + 15, 0, 30)
    ri, ci, valid = np.broadcast_arrays(ri, ci, valid)
    tab = rpb[:, ri, ci]
    tab = np.where(valid[None], tab, np.float32(NEG)).astype(np.float32)
    return tab.reshape(H, 128, nrows * 64)


def swa_table(n):
    kk = np.arange(384)[None, :]
    qq = np.arange(128)[:, None]
    kpos = (n - 1) * 128 + kk
    qpos = n * 128 + qq
    valid = (np.abs(kpos - qpos) <= 128) & (kpos >= 0) & (kpos < SEQ)
    return np.where(valid, np.float32(0), np.float32(NEG)).astype(np.float32)


def chunked(a, nch):
    return np.ascontiguousarray(a.reshape(nch, 128, a.shape[-1]).transpose(1, 0, 2))


def seq_window(full, lo, hi):
    S = full.shape[0]
    out = np.zeros((hi - lo,) + full.shape[1:], full.dtype)
    a, b = max(lo, 0), min(hi, S)
    out[a - lo:b - lo] = full[a:b]
    return out


def stage_p0(nc, D):
    cT, w_mod, b_mod, mod = D["cT"], D["w_mod"], D["b_mod"], D["mod_d"]
    P = Prog(nc, "p0_")
    c_sb = P.sb([128, 8, 2], F32)
    s_sb = P.sb([128, 8, 2], BF16)
    b_c = P.buf("c")
    b_s = P.buf("s")
    P.dma("sp", lambda e: e.dma_start(out=c_sb[:], in_=cT), b_c, "w")
    P.op("act", lambda e: e.activation(out=s_sb[:], in_=c_sb[:], func=AF.Silu), reads=[b_c], writes=[b_s])
    NW = 3
    w_sb = [P.sb([128, 8, 512], BF16) for _ in range(NW)]
    b_w = [P.buf(f"w{i}") for i in range(NW)]
    bm_sb = [P.sb([2, 512], F32) for _ in range(2)]
    b_bm = [P.buf(f"bm{i}") for i in range(2)]
    o_sb = [P.sb([2, 512], F32) for _ in range(2)]
    b_o = [P.buf(f"o{i}") for i in range(2)]
    pss = [P.ps([2, 512], F32) for _ in range(2)]
    b_ps = [P.buf(f"ps{i}") for i in range(2)]
    it = 0
    for l in range(2):
        for n in range(12):
            wi = it % NW
            j = it % 2
            src = w_mod[l, :, n * 512:(n + 1) * 512].rearrange("(k p) n -> p k n", p=128)
            P.dma("pool", lambda e, wi=wi, src=src: e.dma_start(out=w_sb[wi][:], in_=src), b_w[wi], "w")
            for r in range(2):
                bsrc = b_mod[l:l + 1, n * 512:(n + 1) * 512]
                P.dma("sp", lambda e, j=j, r=r, bsrc=bsrc: e.dma_start(out=bm_sb[j][r:r + 1, :], in_=bsrc), b_bm[j], "w")
            for k in range(8):
                P.op("pe", lambda e, j=j, wi=wi, k=k: e.matmul(pss[j][:], lhsT=s_sb[:, k, :], rhs=w_sb[wi][:, k, :], start=(k == 0), stop=(k == 7)),
                     reads=[b_s, b_w[wi]], writes=[b_ps[j]])
            P.op("dve", lambda e, j=j: e.tensor_tensor(out=o_sb[j][:], in0=pss[j][:], in1=bm_sb[j][:], op=ALU.add),
                 reads=[b_ps[j], b_bm[j]], writes=[b_o[j]])
            dst = mod[l, :, n * 512:(n + 1) * 512]
            P.dma("sp", lambda e, j=j, dst=dst: e.dma_start(out=dst, in_=o_sb[j][:]), b_o[j], "r")
            it += 1
    P.emit()


def stage_p1(nc, D, l, xsrc):
    NT, NTX = NT_ALL, NTXA
    mod = D["mod_d"][l]
    g_pre = D["g_pre_mix"][l]
    w_in = D["w_in"][l]
    zmix, gates, o_b = D["zmix_d"], D["gates_d"], D["ob_d"]
    qTa, kTa, qTd, kTd = D["qTa_d"], D["kTa_d"], D["qTd_d"], D["kTd_d"]
    suTf = D["suTf_d"].rearrange("(m p) t -> p m t", p=128)
    suTb = D["suTb_d"].rearrange("(m p) t -> p m t", p=128)
    ropeT = D["ropeT"]
    gm_ln, gm_wsT, gm_bsT = D["gm_ln"][l], D["gm_wsT"][l], D["gm_bsT"][l]
    P = Prog(nc, f"p1{l}_")
    ident, _, b_id = make_ident(P)
    lng = P.sb([128, 256], F32)
    lnb = P.sb([128, 256], F32)
    wsT = P.sb([128, 4, 128], BF16)
    bsT = P.sb([128, 4], F32)
    b_gmp = P.buf("gmp")
    P.dma("sp", lambda e: e.dma_start(out=lng[:], in_=gm_ln[0, :].partition_broadcast(128)), b_gmp, "w")
    P.dma("sp", lambda e: e.dma_start(out=lnb[:], in_=gm_ln[1, :].partition_broadcast(128)), b_gmp, "w")
    P.dma("sp", lambda e: e.dma_start(out=bsT[:], in_=gm_bsT), b_gmp, "w")
    b_wsT = P.buf("wsT")
    P.dma("pool", lambda e: e.dma_start(out=wsT[:], in_=gm_wsT), b_wsT, "w")
    ge = P.sb([128, 512], F32); b_ge = P.buf("ge")
    g1 = P.sb([128, 512], F32); b_g1 = P.buf("g1")
    g2 = P.sb([128, 512], F32); b_g2 = P.buf("g2")
    vst = P.sb([128, 4], F32); b_vst = P.buf("vst")
    vc = P.sb([128, 256], F32); b_vc = P.buf("vc")
    vscr = P.sb([128, 256], F32); b_vscr = P.buf("vscr")
    vn = P.sb([128, 256], BF16); b_vn = P.buf("vn")
    pg = P.ps([128, 256], F32); b_pg = P.buf("pg")
    ob = [P.sb([128, 256], F32) for _ in range(2)]
    b_ob = [P.buf(f"ob{i}") for i in range(2)]
    w_sb = P.sb([128, 8, 6144], BF16)
    b_w = [P.buf(f"w{n}") for n in range(12)]
    for n in range(12):
        src = w_in[:, n * 512:(n + 1) * 512].rearrange("(k p) n -> p k n", p=128)
        P.dma("pool", lambda e, n=n, src=src: e.dma_start(out=w_sb[:, :, n * 512:(n + 1) * 512], in_=src), b_w[n], "w")
    w_rot = P.sb([128, 8, 384], BF16)
    b_wrot = P.buf("wrot")
    for k in range(8):
        src = w_sb[:, k, 768:1152].rearrange("p (a two d) -> p a two d", two=2, d=16)
        dst = w_rot[:, k, :].rearrange("p (a two d) -> p a two d", two=2, d=16)
        P.op("act", lambda e, src=src, dst=dst: e.activation(out=dst[:, :, 0, :], in_=src[:, :, 1, :], func=AF.Copy, scale=-1.0),
             reads=[b_w[1], b_w[2]], writes=[b_wrot])
        P.op("act", lambda e, src=src, dst=dst: e.copy(out=dst[:, :, 1, :], in_=src[:, :, 0, :]), reads=[b_w[1], b_w[2]], writes=[b_wrot])
    g_sb = P.sb([128, DM], F32)
    b_g = P.buf("g")
    P.dma("sp", lambda e: e.dma_start(out=g_sb[:], in_=g_pre.partition_broadcast(128)), b_g, "w")
    G1 = [P.sb([128, DM], F32) for _ in range(2)]
    SH = [P.sb([128, DM], F32) for _ in range(2)]
    b_G1 = [P.buf(f"G1{i}") for i in range(2)]
    b_SH = [P.buf(f"SH{i}") for i in range(2)]
    for cls in range(2):
        P.dma("sp", lambda e, cls=cls: e.dma_start(out=SH[cls][:], in_=mod[cls, 0:DM].partition_broadcast(128)), b_SH[cls], "w")
        P.dma("sp", lambda e, cls=cls: e.dma_start(out=G1[cls][:], in_=mod[cls, DM:2 * DM].partition_broadcast(128)), b_G1[cls], "w")
        P.op("dve", lambda e, cls=cls: e.scalar_tensor_tensor(out=G1[cls][:], in0=G1[cls][:], scalar=1.0, in1=g_sb[:], op0=ALU.add, op1=ALU.mult),
             reads=[b_G1[cls], b_g], writes=[b_G1[cls]])
    x_sb = [P.sb([128, DM], F32) for _ in range(2)]
    b_x = [P.buf(f"x{i}") for i in range(2)]
    scr = P.sb([128, DM], F32); b_scr = P.buf("scr")
    st = [P.sb([128, 2], F32) for _ in range(2)]
    b_st = [P.buf(f"st{i}") for i in range(2)]
    tmp = P.sb([128, DM], F32); b_tmp = P.buf("tmp")
    hb = P.sb([128, DM], BF16); b_hb = P.buf("hb")
    hT = [P.sb([128, 8, 128], BF16) for _ in range(2)]
    b_hT = [P.buf(f"hT{i}") for i in range(2)]
    pT = P.ps([128, 8, 128], BF16); b_pT = P.buf("pT")
    pm = [P.ps([128, 512], F32) for _ in range(4)]
    b_pm = [P.buf(f"pm{i}") for i in range(4)]
    zo = [P.sb([128, 2048], F32) for _ in range(2)]
    b_zo = [P.buf(f"zo{i}") for i in range(2)]
    go1 = P.sb([128, 4096], BF16)
    go = [go1, go1]
    b_go1 = P.buf("go")
    b_go = [b_go1, b_go1]
    fm = [P.sb([64, 8, 128], BF16) for _ in range(2)]
    b_fm = [P.buf(f"fm{i}") for i in range(2)]
    fmd = [P.sb([64, 6, 128], BF16) for _ in range(2)]
    b_fmd = [P.buf(f"fmd{i}") for i in range(2)]
    cs1 = P.sb([64, 2, 4, 128], F32)
    cs = [cs1, cs1]
    b_cs1 = P.buf("cs")
    b_cs = [b_cs1, b_cs1]
    r1 = P.sb([64, 4, 128], F32); b_r1 = P.buf("r1")
    r2 = P.sb([64, 4, 128], F32); b_r2 = P.buf("r2")
    su_sb = [P.sb([128, 2, 128], F32) for _ in range(2)]
    b_su = [P.buf(f"su{i}") for i in range(2)]

    def epilogue(t, j):
        z = zo[j]
        gx = z[:, 1280:1792]
        P.op("dve", lambda e: e.tensor_tensor(out=g1[:], in0=gx, in1=gx, op=ALU.mult), reads=[b_zo[j]], writes=[b_g1])
        P.op("dve", lambda e: e.tensor_scalar(out=g1[:], in0=g1[:], scalar1=0.044715, scalar2=1.0, op0=ALU.mult, op1=ALU.add), reads=[b_g1], writes=[b_g1])
        P.op("dve", lambda e: e.tensor_tensor(out=g1[:], in0=g1[:], in1=gx, op=ALU.mult), reads=[b_g1, b_zo[j]], writes=[b_g1])
        P.op("act", lambda e: e.activation(out=g2[:], in_=g1[:], func=AF.Sigmoid, scale=1.5957691216057308), reads=[b_g1], writes=[b_g2])
        P.op("dve", lambda e: e.tensor_tensor(out=ge[:], in0=g2[:], in1=gx, op=ALU.mult), reads=[b_g2, b_zo[j]], writes=[b_ge])
        gv = ge[:, 256:512]
        P.op("act", lambda e: e.activation(out=vscr[:], in_=gv, func=AF.Identity, accum_out=vst[:, 0:1]), reads=[b_ge], writes=[b_vscr, b_vst])
        P.op("dve", lambda e: e.tensor_scalar(out=vst[:, 0:1], in0=vst[:, 0:1], scalar1=1.0 / 256, scalar2=None, op0=ALU.mult), reads=[b_vst], writes=[b_vst])
        P.op("dve", lambda e: e.tensor_scalar(out=vc[:], in0=gv, scalar1=vst[:, 0:1], scalar2=None, op0=ALU.subtract), reads=[b_ge, b_vst], writes=[b_vc])
        rms_rstd(P, vc[:], vscr[:], vst[:, 1:2], vst[:, 2:3], b_vc, b_vscr, b_vst, d=256)
        P.op("dve", lambda e: e.scalar_tensor_tensor(out=vc[:], in0=vc[:], scalar=vst[:, 2:3], in1=lng[:], op0=ALU.mult, op1=ALU.mult),
             reads=[b_vc, b_vst, b_gmp], writes=[b_vc])
        P.op("dve", lambda e: e.tensor_tensor(out=vn[:], in0=vc[:], in1=lnb[:], op=ALU.add), reads=[b_vc, b_gmp], writes=[b_vn])
        for g in range(4):
            P.op("pe", lambda e, g=g: e.matmul(pg[:, g * 64:(g + 1) * 64], lhsT=wsT[:, g, :], rhs=vn[:, g * 64:(g + 1) * 64], start=True, stop=True),
                 reads=[b_wsT, b_vn], writes=[b_pg])
        for g in range(4):
            P.op("dve", lambda e, g=g: e.scalar_tensor_tensor(out=ob[j][:, g * 64:(g + 1) * 64], in0=pg[:, g * 64:(g + 1) * 64], scalar=bsT[:, g:g + 1],
                                                             in1=ge[:, g * 64:(g + 1) * 64], op0=ALU.add, op1=ALU.mult),
                 reads=[b_pg, b_ge, b_gmp], writes=[b_ob[j]])
        P.dma("sp", lambda e: e.dma_start(out=o_b[t * 128:(t + 1) * 128, :], in_=ob[j][:]), b_ob[j], "r")

    it = 0
    for t in range(NT):
        j = t % 2
        cls = 0 if t < NTX else 1
        tok = slice(t * 128, (t + 1) * 128)
        posf = (2 + t) if t < NTX else (t - NTX)
        xs = xsrc(t)
        P.dma("sp", lambda e, j=j, xs=xs: e.dma_start(out=x_sb[j][:], in_=xs), b_x[j], "w")
        P.dma("sp", lambda e, j=j, tok=tok: e.dma_start(out=cs[j][:], in_=ropeT[:, :, :, tok]), b_cs[j], "w")
        rms_rstd(P, x_sb[j][:], scr[:], st[j][:, 0:1], st[j][:, 1:2], b_x[j], b_scr, b_st[j])
        P.op("dve", lambda e, j=j, cls=cls: e.scalar_tensor_tensor(out=tmp[:], in0=x_sb[j][:], scalar=st[j][:, 1:2], in1=G1[cls][:],
                                                                   op0=ALU.mult, op1=ALU.mult),
             reads=[b_x[j], b_st[j], b_G1[cls]], writes=[b_tmp])
        P.op("pool", lambda e, cls=cls: e.tensor_tensor(out=hb[:], in0=tmp[:], in1=SH[cls][:], op=ALU.add), reads=[b_tmp, b_SH[cls]], writes=[b_hb])
        for k in range(8):
            P.op("pe", lambda e, k=k: e.transpose(out=pT[:, k, :], in_=hb[:, k * 128:(k + 1) * 128], identity=ident[:]), reads=[b_hb, b_id], writes=[b_pT])
        P.op("act", lambda e, j=j: e.copy(out=hT[j][:], in_=pT[:]), reads=[b_pT], writes=[b_hT[j]])
        for n in range(12):
            q = it % 4
            it += 1
            for k in range(8):
                P.op("pe", lambda e, q=q, j=j, k=k, n=n: e.matmul(pm[q][:], lhsT=hT[j][:, k, :], rhs=w_sb[:, k, n * 512:(n + 1) * 512], start=(k == 0), stop=(k == 7)),
                     reads=[b_hT[j], b_w[n]], writes=[b_pm[q]])
            if n < 4:
                if n % 2 == 0:
                    P.op("dve", lambda e, q=q, j=j, n=n: e.tensor_copy(out=zo[j][:, n * 512:(n + 1) * 512], in_=pm[q][:]), reads=[b_pm[q]], writes=[b_zo[j]])
                else:
                    P.op("act", lambda e, q=q, j=j, n=n: e.copy(out=zo[j][:, n * 512:(n + 1) * 512], in_=pm[q][:]), reads=[b_pm[q]], writes=[b_zo[j]])
            else:
                m = n - 4
                P.op("act", lambda e, q=q, j=j, m=m: e.activation(out=go[j][:, m * 512:(m + 1) * 512], in_=pm[q][:], func=AF.Sigmoid),
                     reads=[b_pm[q]], writes=[b_go[j]])
            if n == 3:
                P.dma("sp", lambda e, j=j, tok=tok: e.dma_start(out=zmix[tok, :], in_=zo[j][:]), b_zo[j], "r")
                epilogue(t, j)
        P.dma("sp", lambda e, j=j, tok=tok: e.dma_start(out=gates[tok, :], in_=go[j][:]), b_go[j], "r")
        for grp, col0 in enumerate((0, 256)):
            q = it % 4
            it += 1
            pv = pm[q][0:64, :].rearrange("p (h t) -> p h t", h=4)
            for h in range(4):
                for k in range(8):
                    P.op("pe", lambda e, pv=pv, h=h, k=k, col0=col0, j=j: e.matmul(pv[:, h, :], lhsT=w_sb[:, k, col0 + h * 64:col0 + (h + 1) * 64],
                                                                                rhs=hT[j][:, k, :], start=(k == 0), stop=(k == 7)),
                         reads=[b_hT[j], b_w[0]], writes=[b_pm[q]])
            P.op("act", lambda e, pv=pv, grp=grp, j=j: e.copy(out=fm[j][:, grp * 4:(grp + 1) * 4, :], in_=pv), reads=[b_pm[q]], writes=[b_fm[j]])
        P.dma("sp", lambda e, j=j, tok=tok: e.dma_start(out=qTa[:, :, tok], in_=fm[j][:, 0:4, :]), b_fm[j], "r")
        P.dma("sp", lambda e, j=j, tok=tok: e.dma_start(out=kTa[:, :, tok], in_=fm[j][:, 4:8, :]), b_fm[j], "r")
        for (h0, nh) in ((0, 4), (4, 2)):
            qx = it % 4
            it += 1
            qr = it % 4
            it += 1
            pvx = pm[qx][0:64, :].rearrange("p (h t) -> p h t", h=4)
            pvr = pm[qr][0:64, :].rearrange("p (h t) -> p h t", h=4)
            for hh in range(nh):
                h6 = h0 + hh
                for k in range(8):
                    P.op("pe", lambda e, pvx=pvx, hh=hh, k=k, h6=h6, j=j: e.matmul(pvx[:, hh, :], lhsT=w_sb[:, k, 768 + h6 * 64:768 + (h6 + 1) * 64],
                                                                                rhs=hT[j][:, k, :], start=(k == 0), stop=(k == 7)),
                         reads=[b_hT[j], b_w[1], b_w[2]], writes=[b_pm[qx]])
                for k in range(8):
                    P.op("pe", lambda e, pvr=pvr, hh=hh, k=k, h6=h6, j=j: e.matmul(pvr[:, hh, :], lhsT=w_rot[:, k, h6 * 64:(h6 + 1) * 64],
                                                                                rhs=hT[j][:, k, :], start=(k == 0), stop=(k == 7)),
                         reads=[b_hT[j], b_wrot], writes=[b_pm[qr]])
            P.op("dve", lambda e, pvx=pvx, nh=nh, j=j: e.tensor_tensor(out=r1[:, 0:nh, :], in0=pvx[:, 0:nh, :], in1=cs[j][:, 0, 0:nh, :], op=ALU.mult),
                 reads=[b_pm[qx], b_cs[j]], writes=[b_r1])
            P.op("dve", lambda e, pvr=pvr, nh=nh, j=j: e.tensor_tensor(out=r2[:, 0:nh, :], in0=pvr[:, 0:nh, :], in1=cs[j][:, 1, 0:nh, :], op=ALU.mult),
                 reads=[b_pm[qr], b_cs[j]], writes=[b_r2])
            P.op("pool", lambda e, nh=nh, h0=h0, j=j: e.tensor_tensor(out=fmd[j][:, h0:h0 + nh, :], in0=r1[:, 0:nh, :], in1=r2[:, 0:nh, :], op=ALU.add),
                 reads=[b_r1, b_r2], writes=[b_fmd[j]])
        P.dma("sp", lambda e, j=j, tok=tok: e.dma_start(out=qTd[:, :, tok], in_=fmd[j][:, 0:4, :]), b_fmd[j], "r")
        P.dma("sp", lambda e, j=j, tok=tok: e.dma_start(out=kTd[:, :, tok], in_=fmd[j][:, 4:6, :]), b_fmd[j], "r")
        for m in range(2):
            q = it % 4
            it += 1
            for k in range(8):
                P.op("pe", lambda e, q=q, m=m, k=k, j=j: e.matmul(pm[q][:, 0:128], lhsT=w_sb[:, k, 1792 + m * 128:1792 + (m + 1) * 128], rhs=hT[j][:, k, :],
                                                               start=(k == 0), stop=(k == 7)),
                     reads=[b_hT[j], b_w[3]], writes=[b_pm[q]])
            P.op("dve", lambda e, q=q, m=m, j=j: e.tensor_copy(out=su_sb[j][:, m, :], in_=pm[q][:, 0:128]), reads=[b_pm[q]], writes=[b_su[j]])
        P.dma("sp", lambda e, j=j, posf=posf: e.dma_start(out=suTf[:, :, posf * 128:(posf + 1) * 128], in_=su_sb[j][:]), b_su[j], "r")
        P.dma("sp", lambda e, j=j, tok=tok: e.dma_start(out=suTb[:, :, tok], in_=su_sb[j][:]), b_su[j], "r")
    P.emit()


def rope_table_fm():
    nq = 16
    inv = 10000.0 ** (-np.arange(nq, dtype=np.float32) / nq)
    pos = np.arange(SEQ)
    ang = np.zeros((64, T_ALL), np.float32)
    d = np.arange(64)
    i = d % 16
    is_col = (d // 32) == 1
    p = np.where(is_col[:, None], (pos % 64)[None, :], (pos // 64)[None, :]).astype(np.float32)
    ang[:, :SEQ] = p * inv[i][:, None]
    out = np.zeros((64, 2, 4, T_ALL), np.float32)
    out[:, 0] = np.cos(ang)[:, None, :]
    out[:, 1] = np.sin(ang)[:, None, :]
    return out


def stage_attn(nc, D, l, kind, ctx_out):
    if kind == "na":
        H, HKV, PAD = 4, 4, 3
        qT, kT = D["qTa_d"], D["kTa_d"]
        v_src = D["zmix_d"][:, 512:768]
        tabI, tabE = D["na_tabI"][l], D["na_tabE"][l]
        n_tabI, wI = 4, 640
        out = D["oa_d"]
        use_sink = False
    else:
        H, HKV, PAD = 4, 2, 1
        qT, kT = D["qTd_d"], D["kTd_d"]
        v_src = D["zmix_d"][:, 1152:1280]
        tabI, tabE = D["swa_tab"], None
        n_tabI, wI = 3, 384
        out = D["od_d"]
        use_sink = True
        sink = D["swa_sink"][l]
    NX = NTXA
    NSEQCH = NX + 2 * PAD
    NCH = NSEQCH + 2
    TK = NCH * 128
    tiles = []
    for i in range(NX):
        if kind == "na":
            if 2 <= i <= NX - 3:
                tiles.append((i, PAD + i - 2, 5, ("I", None)))
            else:
                e = {0: 0, 1: 1, NX - 2: 2, NX - 1: 3}[i]
                tiles.append((i, PAD + i - 3, 7, ("E", e)))
        else:
            case = 0 if i == 0 else (2 if i == NX - 1 else 1)
            tiles.append((i, PAD + i - 1, 3, ("I", case)))
    if ctx_out:
        for c in range(2):
            tiles.append((NX + c, 0, 0, ("I", 0)))
    P = Prog(nc, f"at{kind}{l}_")
    ident, _, b_id = make_ident(P)
    QH = 33 * 128
    qT_sb = P.sb([64, H, QH], BF16)
    kT_sb = P.sb([64, HKV, TK], BF16)
    v_sb = P.sb([128, NCH, HKV * 64], BF16)
    tI_sb = P.sb([128, n_tabI, wI], F32)
    b_q = P.buf("q")
    b_k = [P.buf(f"k{h}") for h in range(HKV)]
    b_v = P.buf("v")
    b_tI = P.buf("tI")
    for h in range(HKV):
        P.op("pool", lambda e, h=h: e.memset(kT_sb[:, h, :], 0.0), writes=[b_k[h]])
        P.dma("sp", lambda e, h=h: e.dma_start(out=kT_sb[:, h, PAD * 128:PAD * 128 + SEQ], in_=kT[:, h, 0:SEQ]), b_k[h], "w")
        P.dma("sp", lambda e, h=h: e.dma_start(out=kT_sb[:, h, NSEQCH * 128:NSEQCH * 128 + CTX], in_=kT[:, h, SEQ:SEQ + CTX]), b_k[h], "w")
    P.op("pool", lambda e: e.memset(v_sb[:], 0.0), writes=[b_v])
    for c in range(0, NX, 8):
        P.dma("pool", lambda e, c=c: e.dma_start(out=v_sb[:, PAD + c:PAD + c + 8, :],
                                               in_=v_src[c * 128:(c + 8) * 128, :].rearrange("(c p) f -> p c f", p=128)), b_v, "w")
    P.dma("pool", lambda e: e.dma_start(out=v_sb[:, NSEQCH:NSEQCH + 2, :], in_=v_src[SEQ:SEQ + CTX, :].rearrange("(c p) f -> p c f", p=128)), b_v, "w")
    for i in range(n_tabI):
        P.dma("sp", lambda e, i=i: e.dma_start(out=tI_sb[:, i, :], in_=tabI[:, i, :]), b_tI, "w")
    tE_sb = [P.sb([128, 896], F32) for _ in range(2)]
    b_tE = [P.buf(f"tE{i}") for i in range(2)]
    WMAX = 2 + 7 * 128 + 256
    S = [P.sb([128, WMAX], F32) for _ in range(H)]
    b_S = [P.buf(f"S{i}") for i in range(H)]
    for i in range(H):
        if use_sink:
            P.dma("sp", lambda e, i=i: e.dma_start(out=S[i][:, 1:2], in_=sink[i:i + 1].partition_broadcast(128)), b_S[i], "w")
        else:
            P.op("pool", lambda e, i=i: e.memset(S[i][:, 0:2], NEG), writes=[b_S[i]])
    Pb = [P.sb([128, WMAX], BF16) for _ in range(2)]
    b_Pb = [P.buf(f"Pb{i}") for i in range(2)]
    st = [P.sb([128, 4], F32) for _ in range(2)]
    b_st = [P.buf(f"st{i}") for i in range(2)]
    ps_s = [P.ps([128, 3, 512], F32) for _ in range(2)]
    b_pss = [P.buf(f"pss{i}") for i in range(2)]
    ps_T = P.ps([128, 8, 128], BF16)
    b_psT = P.buf("psT")
    PT = [P.sb([128, 12, 128], BF16) for _ in range(2)]
    b_PT = [[P.buf(f"PT{i}_{g}") for g in range(2)] for i in range(2)]
    ps_O = P.ps([128, H * 64], F32)
    b_psO = P.buf("psO")
    O_sb = [P.sb([128, H * 64], F32) for _ in range(2)]
    b_O = [P.buf(f"O{i}") for i in range(2)]
    scale = 0.125
    g = H // HKV
    iters = [(ti, h) for ti in range(len(tiles)) for h in range(H)]
    loaded_half = [-1]
    te_cnt = [0]
    te_slot = {}

    def segs(nseq):
        r = []
        n = nseq * 128
        if n > 0:
            r.append((0, min(n, 512), 0))
        if n > 512:
            r.append((512, n - 512, 1))
        return r

    def emit_scores(i):
        ti, h = iters[i]
        qt, c0, nseq, tab = tiles[ti]
        half = qt // 33
        if half != loaded_half[0]:
            loaded_half[0] = half
            n_tok = min(QH, T_ALL - half * QH)
            for hh in range(H):
                P.dma("sp", lambda e, hh=hh, half=half, n_tok=n_tok: e.dma_start(out=qT_sb[:, hh, 0:n_tok], in_=qT[:, hh, half * QH:half * QH + n_tok]), b_q, "w")
        ql = (qt - half * 33) * 128
        par = i % 2
        kvh = h // g
        if tab[0] == "E":
            slot = te_cnt[0] % 2
            te_cnt[0] += 1
            te_slot[i] = slot
            idx = tab[1] * 4 + h
            P.dma("sp", lambda e, slot=slot, idx=idx: e.dma_start(out=tE_sb[slot][:], in_=tabE[:, idx, :]), b_tE[slot], "w")
        for (off, ln, bank) in segs(nseq):
            P.op("pe", lambda e, off=off, ln=ln, bank=bank, par=par, h=h, ql=ql, kvh=kvh, c0=c0: e.matmul(
                ps_s[par][:, bank, 0:ln], lhsT=qT_sb[:, h, ql:ql + 128], rhs=kT_sb[:, kvh, c0 * 128 + off:c0 * 128 + off + ln], start=True, stop=True),
                reads=[b_q, b_k[kvh]], writes=[b_pss[par]])
        P.op("pe", lambda e, par=par, h=h, ql=ql, kvh=kvh: e.matmul(ps_s[par][:, 2, 0:256], lhsT=qT_sb[:, h, ql:ql + 128],
                                                                  rhs=kT_sb[:, kvh, NSEQCH * 128:NSEQCH * 128 + 256], start=True, stop=True),
             reads=[b_q, b_k[kvh]], writes=[b_pss[par]])

    def emit_rest(i):
        ti, h = iters[i]
        qt, c0, nseq, tab = tiles[ti]
        par = i % 2
        kvh = h // g
        Sx = S[h]
        nk = nseq * 128 + 256
        for (off, ln, bank) in segs(nseq):
            if tab[0] == "I":
                tidx = h if kind == "na" else tab[1]
                tap = tI_sb[:, tidx, off:off + ln]
                bt = b_tI
            else:
                slot = te_slot[i]
                tap = tE_sb[slot][:, off:off + ln]
                bt = b_tE[slot]
            P.op("dve", lambda e, off=off, ln=ln, bank=bank, tap=tap, Sx=Sx, par=par: e.scalar_tensor_tensor(
                out=Sx[:, 2 + off:2 + off + ln], in0=ps_s[par][:, bank, 0:ln], scalar=scale, in1=tap, op0=ALU.mult, op1=ALU.add),
                reads=[b_pss[par], bt], writes=[b_S[h]])
        P.op("act", lambda e, Sx=Sx, par=par, nseq=nseq, nk=nk: e.activation(out=Sx[:, 2 + nseq * 128:2 + nk], in_=ps_s[par][:, 2, 0:256], func=AF.Copy, scale=scale),
             reads=[b_pss[par]], writes=[b_S[h]])
        P.op("dve", lambda e, Sx=Sx, par=par, nk=nk: e.reduce_max(out=st[par][:, 0:1], in_=Sx[:, 1:2 + nk], axis=AX.X), reads=[b_S[h]], writes=[b_st[par]])
        P.op("dve", lambda e, par=par: e.tensor_scalar(out=st[par][:, 1:2], in0=st[par][:, 0:1], scalar1=-1.0, scalar2=None, op0=ALU.mult),
             reads=[b_st[par]], writes=[b_st[par]])
        P.op("act", lambda e, Sx=Sx, par=par, nk=nk: e.activation(out=Pb[par][:, 1:2 + nk], in_=Sx[:, 1:2 + nk], func=AF.Exp, bias=st[par][:, 1:2], scale=1.0,
                                                               accum_out=st[par][:, 2:3]),
             reads=[b_S[h], b_st[par]], writes=[b_Pb[par], b_st[par]])
        nch = nseq + 2
        ngrp = (nch + 7) // 8
        for gi in range(ngrp):
            ca, cb = gi * 8, min(nch, gi * 8 + 8)
            for c in range(ca, cb):
                P.op("pe", lambda e, c=c, ca=ca, par=par: e.transpose(out=ps_T[:, c - ca, :], in_=Pb[par][:, 2 + c * 128:2 + (c + 1) * 128], identity=ident[:]),
                     reads=[b_Pb[par], b_id], writes=[b_psT])
            if gi % 2 == 0:
                P.op("act", lambda e, ca=ca, cb=cb, par=par: e.copy(out=PT[par][:, ca:cb, :], in_=ps_T[:, 0:(cb - ca), :]), reads=[b_psT], writes=[b_PT[par][gi]])
            else:
                P.op("dve", lambda e, ca=ca, cb=cb, par=par: e.tensor_copy(out=PT[par][:, ca:cb, :], in_=ps_T[:, 0:(cb - ca), :]), reads=[b_psT], writes=[b_PT[par][gi]])
        for c in range(nch):
            vc = (c0 + c) if c < nseq else (NSEQCH + c - nseq)
            P.op("pe", lambda e, c=c, vc=vc, par=par, h=h, kvh=kvh, nch=nch: e.matmul(ps_O[:, h * 64:(h + 1) * 64], lhsT=PT[par][:, c, :],
                                                                                  rhs=v_sb[:, vc, kvh * 64:(kvh + 1) * 64], start=(c == 0), stop=(c == nch - 1)),
                 reads=[b_PT[par][c // 8], b_v], writes=[b_psO])
        tp = ti % 2
        P.op("dve", lambda e, par=par: e.reciprocal(out=st[par][:, 3:4], in_=st[par][:, 2:3]), reads=[b_st[par]], writes=[b_st[par]])
        P.op("dve", lambda e, par=par, tp=tp, h=h: e.tensor_scalar(out=O_sb[tp][:, h * 64:(h + 1) * 64], in0=ps_O[:, h * 64:(h + 1) * 64], scalar1=st[par][:, 3:4],
                                                                 scalar2=None, op0=ALU.mult),
             reads=[b_psO, b_st[par]], writes=[b_O[tp]])
        if h == H - 1:
            P.dma("sp", lambda e, tp=tp, qt=qt: e.dma_start(out=out[qt * 128:(qt + 1) * 128, :], in_=O_sb[tp][:]), b_O[tp], "r")

    emit_scores(0)
    for i in range(len(iters)):
        if i + 1 < len(iters):
            emit_scores(i + 1)
        emit_rest(i)
    P.emit()


def stage_s5(nc, D, l, TC=512):
    NTOK = T_ALL
    uTs = [D["suTf_d"], D["suTb_d"]]
    yTs = [D["yTf_d"], D["yTb_d"]]
    prm, bmat, cmat = D["s5_prm"][l], D["s5_bmat"][l], D["s5_cmat"][l]
    P = Prog(nc, f"s5{l}_")
    _, ident_f, b_id = make_ident(P)
    DJ = 16
    prm_sb = P.sb([128, 3, DJ], F32)
    b_sb = P.sb([128, 2, DJ, 16], F32)
    c_sb = P.sb([128, 2, DJ, 16], F32)
    bp = P.buf("prm")
    P.dma("sp", lambda e: e.dma_start(out=prm_sb[:], in_=prm), bp, "w")
    P.dma("sp", lambda e: e.dma_start(out=b_sb[:], in_=bmat), bp, "w")
    P.dma("sp", lambda e: e.dma_start(out=c_sb[:], in_=cmat), bp, "w")
    W = P.sb([128, 16, DJ], F32)
    bw = P.buf("W")
    LRE, DT, MAG, TH, SN, CS, T1, T2, ABR, ABI, DEN, KRE, KIM, LIM = range(14)

    def ts(out_i, in_i, s1, s2, o0, o1=None):
        if o1 is None:
            P.op("dve", lambda e: e.tensor_scalar(out=W[:, out_i, :], in0=W[:, in_i, :], scalar1=s1, scalar2=None, op0=o0), reads=[bw], writes=[bw])
        else:
            P.op("dve", lambda e: e.tensor_scalar(out=W[:, out_i, :], in0=W[:, in_i, :], scalar1=s1, scalar2=s2, op0=o0, op1=o1), reads=[bw], writes=[bw])

    def tt(out_i, a_i, b_i, o):
        P.op("dve", lambda e: e.tensor_tensor(out=W[:, out_i, :], in0=W[:, a_i, :], in1=W[:, b_i, :], op=o), reads=[bw], writes=[bw])

    P.op("dve", lambda e: e.tensor_scalar(out=W[:, LRE, :], in0=prm_sb[:, 0, :], scalar1=-1e-4, scalar2=None, op0=ALU.min), reads=[bp], writes=[bw])
    P.op("dve", lambda e: e.tensor_copy(out=W[:, LIM, :], in_=prm_sb[:, 1, :]), reads=[bp, bw], writes=[bw])
    P.op("act", lambda e: e.activation(out=W[:, DT, :], in_=prm_sb[:, 2, :], func=AF.Exp), reads=[bp, bw], writes=[bw])
    tt(T1, LRE, DT, ALU.mult)
    P.op("act", lambda e: e.activation(out=W[:, MAG, :], in_=W[:, T1, :], func=AF.Exp), reads=[bw], writes=[bw])
    tt(TH, LIM, DT, ALU.mult)
    P.op("act", lambda e: e.activation(out=W[:, SN, :], in_=W[:, TH, :], func=AF.Sin, scale=1.0 / 64), reads=[bw], writes=[bw])
    tt(T1, SN, SN, ALU.mult)
    ts(CS, T1, -2.0, 1.0, ALU.mult, ALU.add)
    P.op("act", lambda e: e.activation(out=W[:, SN, :], in_=W[:, TH, :], func=AF.Sin, scale=1.0 / 32), reads=[bw], writes=[bw])
    for _ in range(5):
        tt(T1, CS, CS, ALU.mult)
        tt(T2, SN, SN, ALU.mult)
        tt(SN, SN, CS, ALU.mult)
        ts(SN, SN, 2.0, None, ALU.mult)
        tt(CS, T1, T2, ALU.subtract)
    tt(ABR, MAG, CS, ALU.mult)
    tt(ABI, MAG, SN, ALU.mult)
    tt(T1, LRE, LRE, ALU.mult)
    tt(T2, LIM, LIM, ALU.mult)
    tt(DEN, T1, T2, ALU.add)
    P.op("dve", lambda e: e.reciprocal(out=W[:, DEN, :], in_=W[:, DEN, :]), reads=[bw], writes=[bw])
    ts(T1, ABR, -1.0, None, ALU.add)
    tt(KRE, T1, LRE, ALU.mult)
    tt(T2, ABI, LIM, ALU.mult)
    tt(KRE, KRE, T2, ALU.add)
    tt(KRE, KRE, DEN, ALU.mult)
    tt(KIM, ABI, LRE, ALU.mult)
    tt(T2, T1, LIM, ALU.mult)
    tt(KIM, KIM, T2, ALU.subtract)
    tt(KIM, KIM, DEN, ALU.mult)
    bbE = P.sb([128, 2, DJ, 128], F32)
    cE = P.sb([128, 2, DJ, 128], F32)
    b_bbE = P.buf("bbE")
    b_cE = P.buf("cE")
    P.op("pool", lambda e: e.memset(bbE[:], 0.0), writes=[b_bbE])
    P.op("pool", lambda e: e.memset(cE[:], 0.0), writes=[b_cE])
    tmpb = P.sb([128, 2, 16], F32)
    b_tmpb = P.buf("tmpb")
    for dj in range(DJ):
        j = dj % 8
        for half in range(2):
            p0, p1 = half * 64, half * 64 + 64
            ch0 = ((2 * j + half) % 8) * 16
            kre = W[p0:p1, KRE, dj:dj + 1]
            kim = W[p0:p1, KIM, dj:dj + 1]
            bre = b_sb[p0:p1, 0, dj, :]
            bim = b_sb[p0:p1, 1, dj, :]
            P.op("dve", lambda e, p0=p0, p1=p1, kim=kim, bim=bim: e.tensor_scalar(out=tmpb[p0:p1, 0, :], in0=bim, scalar1=kim, scalar2=None, op0=ALU.mult),
                 reads=[bw, bp], writes=[b_tmpb])
            P.op("dve", lambda e, p0=p0, p1=p1, kre=kre, bre=bre, dj=dj, ch0=ch0: e.scalar_tensor_tensor(out=bbE[p0:p1, 0, dj, ch0:ch0 + 16], in0=bre, scalar=kre,
                                                                                                     in1=tmpb[p0:p1, 0, :], op0=ALU.mult, op1=ALU.subtract),
                 reads=[bw, bp, b_tmpb], writes=[b_bbE])
            P.op("dve", lambda e, p0=p0, p1=p1, kim=kim, bre=bre: e.tensor_scalar(out=tmpb[p0:p1, 1, :], in0=bre, scalar1=kim, scalar2=None, op0=ALU.mult),
                 reads=[bw, bp], writes=[b_tmpb])
            P.op("dve", lambda e, p0=p0, p1=p1, kre=kre, bim=bim, dj=dj, ch0=ch0: e.scalar_tensor_tensor(out=bbE[p0:p1, 1, dj, ch0:ch0 + 16], in0=bim, scalar=kre,
                                                                                                     in1=tmpb[p0:p1, 1, :], op0=ALU.mult, op1=ALU.add),
                 reads=[bw, bp, b_tmpb], writes=[b_bbE])
            P.op("dve", lambda e, p0=p0, p1=p1, dj=dj, ch0=ch0: e.tensor_copy(out=cE[p0:p1, 0, dj, ch0:ch0 + 16], in_=c_sb[p0:p1, 0, dj, :]), reads=[bp], writes=[b_cE])
            P.op("dve", lambda e, p0=p0, p1=p1, dj=dj, ch0=ch0: e.tensor_scalar(out=cE[p0:p1, 1, dj, ch0:ch0 + 16], in0=c_sb[p0:p1, 1, dj, :], scalar1=-1.0,
                                                                              scalar2=None, op0=ALU.mult),
                 reads=[bp], writes=[b_cE])
    lhsB = P.sb([128, 2, DJ, 128], F32)
    b_lhsB = P.buf("lhsB")
    ptr = P.ps([128, 128], F32)
    b_ptr = P.buf("ptr")
    for ri in range(2):
        for dj in range(DJ):
            P.op("pe", lambda e, ri=ri, dj=dj: e.transpose(out=ptr[:], in_=bbE[:, ri, dj, :], identity=ident_f[:]), reads=[b_bbE, b_id], writes=[b_ptr])
            P.op("act", lambda e, ri=ri, dj=dj: e.copy(out=lhsB[:, ri, dj, :], in_=ptr[:]), reads=[b_ptr], writes=[b_lhsB])
    cosT = P.sb([128, DJ, TC], F32)
    sinT = P.sb([128, DJ, TC], F32)
    magT = P.sb([128, DJ, TC], F32)
    b_tab = [P.buf(f"tab{dj}") for dj in range(DJ)]
    ttmp = P.sb([128, TC // 2], F32)
    b_ttmp = P.buf("ttmp")
    ones = P.sb([128, TC], F32)
    b_ones = P.buf("ones")
    P.op("pool", lambda e: e.memset(ones[:], 1.0), writes=[b_ones])
    for dj in range(DJ):
        bt = b_tab[dj]
        P.op("dve", lambda e, dj=dj: e.tensor_scalar(out=magT[:, dj, :], in0=ones[:], scalar1=W[:, MAG, dj:dj + 1], scalar2=None, op0=ALU.mult),
             reads=[b_ones, bw], writes=[bt])
        P.op("dve", lambda e, dj=dj: e.tensor_copy(out=cosT[:, dj, 0:1], in_=W[:, CS, dj:dj + 1]), reads=[bw], writes=[bt])
        P.op("dve", lambda e, dj=dj: e.tensor_copy(out=sinT[:, dj, 0:1], in_=W[:, SN, dj:dj + 1]), reads=[bw], writes=[bt])
        m = 1
        while m < TC:
            cm = cosT[:, dj, m - 1:m]
            sm = sinT[:, dj, m - 1:m]
            P.op("dve", lambda e, dj=dj, m=m, sm=sm: e.tensor_scalar(out=ttmp[:, 0:m], in0=sinT[:, dj, 0:m], scalar1=sm, scalar2=None, op0=ALU.mult),
                 reads=[bt], writes=[b_ttmp])
            P.op("dve", lambda e, dj=dj, m=m, cm=cm: e.scalar_tensor_tensor(out=cosT[:, dj, m:2 * m], in0=cosT[:, dj, 0:m], scalar=cm, in1=ttmp[:, 0:m],
                                                                          op0=ALU.mult, op1=ALU.subtract),
                 reads=[bt, b_ttmp], writes=[bt])
            P.op("dve", lambda e, dj=dj, m=m, sm=sm: e.tensor_scalar(out=ttmp[:, 0:m], in0=cosT[:, dj, 0:m], scalar1=sm, scalar2=None, op0=ALU.mult),
                 reads=[bt], writes=[b_ttmp])
            P.op("dve", lambda e, dj=dj, m=m, cm=cm: e.scalar_tensor_tensor(out=sinT[:, dj, m:2 * m], in0=sinT[:, dj, 0:m], scalar=cm, in1=ttmp[:, 0:m],
                                                                          op0=ALU.mult, op1=ALU.add),
                 reads=[bt, b_ttmp], writes=[bt])
            m *= 2
    nchunks = (NTOK + TC - 1) // TC
    u_sb = [P.sb([128, 2, TC], F32) for _ in range(2)]
    b_u = [P.buf(f"u{i}") for i in range(2)]
    ps_b = [P.ps([128, 2, TC], F32) for _ in range(2)]
    b_psb = [P.buf(f"psb{i}") for i in range(2)]
    bu = [P.sb([128, 2, TC], F32) for _ in range(2)]
    b_bu = [P.buf(f"bu{i}") for i in range(2)]
    r1 = P.sb([128, TC], F32)
    r2 = P.sb([128, TC], F32)
    b_r1, b_r2 = P.buf("r1"), P.buf("r2")
    vv = [P.sb([128, 2, TC], F32) for _ in range(2)]
    b_vv = [P.buf(f"vv{i}") for i in range(2)]
    ww = P.sb([128, 2, TC], F32)
    b_ww = P.buf("ww")
    q1 = P.sb([128, TC], F32)
    q2 = P.sb([128, TC], F32)
    b_q1, b_q2 = P.buf("q1"), P.buf("q2")
    xx = [P.sb([128, 2, TC], F32) for _ in range(2)]
    b_xx = [P.buf(f"xx{i}") for i in range(2)]
    carry = P.sb([128, 8, 2], F32)
    b_carry = [P.buf(f"carry{j}") for j in range(8)]
    ps_y = [P.ps([128, TC], F32) for _ in range(2)]
    b_psy = [P.buf(f"psy{i}") for i in range(2)]
    y_sb = [P.sb([128, TC], F32) for _ in range(2)]
    b_y = [P.buf(f"y{i}") for i in range(2)]
    it = 0
    ity = 0
    for d in range(2):
        rev = (d == 1)
        uT = uTs[d].rearrange("(m p) t -> p m t", p=128)
        yT = yTs[d]
        for j in range(8):
            P.op("pool", lambda e, j=j: e.memset(carry[:, j, :], 0.0), writes=[b_carry[j]])
        order = list(range(nchunks))
        if rev:
            order = order[::-1]
        for ci, n in enumerate(order):
            L = min(TC, NTOK - n * TC)
            un = ci % 2
            P.dma("sp", lambda e, n=n, L=L, un=un, uT=uT: e.dma_start(out=u_sb[un][:, :, 0:L], in_=uT[:, :, n * TC:n * TC + L]), b_u[un], "w")
            for j in range(8):
                dj = d * 8 + j
                m = j // 4
                par = it % 2
                it += 1
                for ri in range(2):
                    P.op("pe", lambda e, ri=ri, dj=dj, par=par, L=L, un=un, m=m: e.matmul(ps_b[par][:, ri, 0:L], lhsT=lhsB[:, ri, dj, :], rhs=u_sb[un][:, m, 0:L],
                                                                                       start=True, stop=True),
                         reads=[b_lhsB, b_u[un]], writes=[b_psb[par]])
                for ri in range(2):
                    dst = bu[par][:, ri, 0:L]
                    if rev:
                        dst = dst[:, ::-1]
                    P.op("act", lambda e, par=par, L=L, ri=ri, dst=dst: e.copy(out=dst, in_=ps_b[par][:, ri, 0:L]), reads=[b_psb[par]], writes=[b_bu[par]])
                cT, sT, mT = cosT[:, dj, 0:L], sinT[:, dj, 0:L], magT[:, dj, 0:L]
                bt = b_tab[dj]
                bre, bim = bu[par][:, 0, 0:L], bu[par][:, 1, 0:L]
                P.op("pool", lambda e, L=L, bre=bre, cT=cT: e.tensor_tensor(out=r1[:, 0:L], in0=bre, in1=cT, op=ALU.mult), reads=[b_bu[par], bt], writes=[b_r1])
                P.op("pool", lambda e, L=L, bim=bim, sT=sT: e.tensor_tensor(out=r2[:, 0:L], in0=bim, in1=sT, op=ALU.mult), reads=[b_bu[par], bt], writes=[b_r2])
                P.op("pool", lambda e, L=L, par=par: e.tensor_tensor(out=vv[par][:, 0, 0:L], in0=r1[:, 0:L], in1=r2[:, 0:L], op=ALU.add), reads=[b_r1, b_r2], writes=[b_vv[par]])
                P.op("pool", lambda e, L=L, bim=bim, cT=cT: e.tensor_tensor(out=r1[:, 0:L], in0=bim, in1=cT, op=ALU.mult), reads=[b_bu[par], bt, b_vv[par]], writes=[b_r1])
                P.op("pool", lambda e, L=L, bre=bre, sT=sT: e.tensor_tensor(out=r2[:, 0:L], in0=bre, in1=sT, op=ALU.mult), reads=[b_bu[par], bt, b_vv[par]], writes=[b_r2])
                P.op("pool", lambda e, L=L, par=par: e.tensor_tensor(out=vv[par][:, 1, 0:L], in0=r1[:, 0:L], in1=r2[:, 0:L], op=ALU.subtract), reads=[b_r1, b_r2], writes=[b_vv[par]])
                for ri in range(2):
                    P.op("dve", lambda e, L=L, par=par, ri=ri, mT=mT, j=j: e.tensor_tensor_scan(out=ww[:, ri, 0:L], data0=mT, data1=vv[par][:, ri, 0:L],
                                                                                             initial=carry[:, j, ri:ri + 1], op0=ALU.mult, op1=ALU.add),
                         reads=[bt, b_vv[par], b_carry[j]], writes=[b_ww])
                wre, wim = ww[:, 0, 0:L], ww[:, 1, 0:L]
                xre, xim = xx[par][:, 0, 0:L], xx[par][:, 1, 0:L]
                if rev:
                    xre, xim = xre[:, ::-1], xim[:, ::-1]
                P.op("dve", lambda e, L=L, wre=wre, cT=cT: e.tensor_tensor(out=q1[:, 0:L], in0=wre, in1=cT, op=ALU.mult), reads=[b_ww, bt], writes=[b_q1])
                P.op("dve", lambda e, L=L, wim=wim, sT=sT: e.tensor_tensor(out=q2[:, 0:L], in0=wim, in1=sT, op=ALU.mult), reads=[b_ww, bt], writes=[b_q2])
                P.op("dve", lambda e, L=L, xre=xre: e.tensor_tensor(out=xre, in0=q1[:, 0:L], in1=q2[:, 0:L], op=ALU.subtract), reads=[b_q1, b_q2], writes=[b_xx[par]])
                P.op("dve", lambda e, L=L, wre=wre, sT=sT: e.tensor_tensor(out=q1[:, 0:L], in0=wre, in1=sT, op=ALU.mult), reads=[b_ww, bt, b_xx[par]], writes=[b_q1])
                P.op("dve", lambda e, L=L, wim=wim, cT=cT: e.tensor_tensor(out=q2[:, 0:L], in0=wim, in1=cT, op=ALU.mult), reads=[b_ww, bt, b_xx[par]], writes=[b_q2])
                P.op("dve", lambda e, L=L, xim=xim: e.tensor_tensor(out=xim, in0=q1[:, 0:L], in1=q2[:, 0:L], op=ALU.add), reads=[b_q1, b_q2], writes=[b_xx[par]])
                last = 0 if rev else L - 1
                P.op("act", lambda e, last=last, par=par, j=j: e.copy(out=carry[:, j, :], in_=xx[par][:, :, last]), reads=[b_xx[par]], writes=[b_carry[j]])
                jj = j % 4
                if jj == 0:
                    yn = ity % 2
                    ity += 1
                for ri in range(2):
                    P.op("pe", lambda e, ri=ri, dj=dj, par=par, L=L, yn=yn, jj=jj: e.matmul(ps_y[yn][:, 0:L], lhsT=cE[:, ri, dj, :], rhs=xx[par][:, ri, 0:L],
                                                                                         start=(jj == 0 and ri == 0), stop=(jj == 3 and ri == 1)),
                         reads=[b_cE, b_xx[par]], writes=[b_psy[yn]])
                if jj == 3:
                    P.op("act", lambda e, L=L, yn=yn: e.copy(out=y_sb[yn][:, 0:L], in_=ps_y[yn][:, 0:L]), reads=[b_psy[yn]], writes=[b_y[yn]])
                    P.dma("sp", lambda e, m=m, n=n, L=L, yn=yn, yT=yT: e.dma_start(out=yT[m * 128:(m + 1) * 128, n * TC:n * TC + L], in_=y_sb[yn][:, 0:L]), b_y[yn], "r")
    P.emit()


def s5_params_host(a_re, a_im, log_dt, b_re, b_im, c_re, c_im):
    prm = np.zeros((128, 3, 16), np.float32)
    bm = np.zeros((128, 2, 16, 16), np.float32)
    cm = np.zeros((128, 2, 16, 16), np.float32)
    for d in range(2):
        for j in range(8):
            for half in range(2):
                g = 2 * j + half
                sl = slice(half * 64, half * 64 + 64)
                dj = d * 8 + j
                prm[sl, 0, dj] = a_re[d, g]
                prm[sl, 1, dj] = a_im[d, g]
                prm[sl, 2, dj] = log_dt[d, g]
                bm[sl, 0, dj] = b_re[d, g]
                bm[sl, 1, dj] = b_im[d, g]
                cm[sl, 0, dj] = c_re[d, g].T
                cm[sl, 1, dj] = c_im[d, g].T
    return prm, bm, cm


def stage_p3a(nc, D, l, xsrc, NT, moe):
    NTX = NTXA
    mod = D["mod_d"][l]
    gvec = D["gvec"][l]
    s5v = D["s5v"][l]
    glu_w = D["s5_glu_w"][l]
    w_br = D["w_branch"][l]
    w_out = D["w_out"][l]
    router = D["moe_router"]
    gates = D["gates_d"]
    x1o, combo = D["x1_d"], D["comb_d"]
    hTo = D["hT_d"].rearrange("(k p) t -> p k t", p=128)
    oa_d, ob_d, od_d, zmix = D["oa_d"], D["ob_d"], D["od_d"], D["zmix_d"]
    yTf = D["yTf_d"].rearrange("(m p) t -> p m t", p=128)
    yTb = D["yTb_d"].rearrange("(m p) t -> p m t", p=128)
    P = Prog(nc, f"p3a{l}_")
    ident, ident_f, b_id = make_ident(P)
    wbr = P.sb([128, 8, DM], BF16)
    wo = P.sb([128, 8, DM], BF16)
    wg = P.sb([128, 2, 256], BF16)
    rt_sb = P.sb([128, 8, 8], F32)
    b_wt = P.buf("wt")
    P.dma("pool", lambda e: e.dma_start(out=wbr[:], in_=w_br.rearrange("(k p) n -> p k n", p=128)), b_wt, "w")
    P.dma("pool", lambda e: e.dma_start(out=wo[:], in_=w_out.rearrange("(k p) n -> p k n", p=128)), b_wt, "w")
    P.dma("pool", lambda e: e.dma_start(out=wg[:], in_=glu_w.rearrange("(k p) n -> p k n", p=128)), b_wt, "w")
    b_wr = P.buf("wr")
    P.dma("sp", lambda e: e.dma_start(out=rt_sb[:], in_=router.rearrange("(k p) n -> p k n", p=128)), b_wr, "w")
    drow = P.sb([128, 256], F32)
    gbrow = P.sb([128, 256], F32)
    P.dma("sp", lambda e: e.dma_start(out=drow[:], in_=s5v[0, :].partition_broadcast(128)), b_wr, "w")
    P.dma("sp", lambda e: e.dma_start(out=gbrow[:], in_=s5v[1, :].partition_broadcast(128)), b_wr, "w")
    gpm = P.sb([128, DM], F32)
    gpf = P.sb([128, DM], F32)
    b_gv = P.buf("gv")
    P.dma("sp", lambda e: e.dma_start(out=gpm[:], in_=gvec[0, :].partition_broadcast(128)), b_gv, "w")
    P.dma("sp", lambda e: e.dma_start(out=gpf[:], in_=gvec[1, :].partition_broadcast(128)), b_gv, "w")
    G2 = [P.sb([128, DM], F32) for _ in range(2)]
    G4 = [P.sb([128, DM], F32) for _ in range(2)]
    SH3 = [P.sb([128, DM], F32) for _ in range(2)]
    b_G = [P.buf(f"G{c}") for c in range(2)]
    for c in range(2):
        P.dma("sp", lambda e, c=c: e.dma_start(out=G2[c][:], in_=mod[c, 2 * DM:3 * DM].partition_broadcast(128)), b_G[c], "w")
        P.dma("sp", lambda e, c=c: e.dma_start(out=SH3[c][:], in_=mod[c, 3 * DM:4 * DM].partition_broadcast(128)), b_G[c], "w")
        P.dma("sp", lambda e, c=c: e.dma_start(out=G4[c][:], in_=mod[c, 4 * DM:5 * DM].partition_broadcast(128)), b_G[c], "w")
        P.op("dve", lambda e, c=c: e.tensor_tensor(out=G2[c][:], in0=G2[c][:], in1=gpm[:], op=ALU.mult), reads=[b_G[c], b_gv], writes=[b_G[c]])
        P.op("dve", lambda e, c=c: e.scalar_tensor_tensor(out=G4[c][:], in0=G4[c][:], scalar=1.0, in1=gpf[:], op0=ALU.add, op1=ALU.mult),
             reads=[b_G[c], b_gv], writes=[b_G[c]])
    x_sb = [P.sb([128, DM], F32) for _ in range(2)]
    b_x = [P.buf(f"x{i}") for i in range(2)]
    br_sb = [P.sb([128, 4, 256], F32) for _ in range(2)]
    b_br = [P.buf(f"br{i}") for i in range(2)]
    yT_sb = [P.sb([128, 2, 2, 128], F32) for _ in range(2)]
    b_yT = [P.buf(f"yT{i}") for i in range(2)]
    h2b = P.sb([128, DM], BF16); b_h2b = P.buf("h2b")
    hTs = [P.sb([128, 8, 128], BF16) for _ in range(2)]
    b_hTs = [P.buf(f"hTs{i}") for i in range(2)]
    gt_sb = [P.sb([128, 4096], BF16) for _ in range(2)]
    b_gt = [P.buf(f"gt{i}") for i in range(2)]
    yv = P.sb([128, 256], F32); b_yv = P.buf("yv")
    g1 = P.sb([128, 256], F32); b_g1 = P.buf("g1")
    g2 = P.sb([128, 256], F32); b_g2 = P.buf("g2")
    gy = P.sb([128, 256], F32); b_gy = P.buf("gy")
    gyb = P.sb([128, 256], BF16); b_gyb = P.buf("gyb")
    gyT = P.sb([128, 2, 128], BF16); b_gyT = P.buf("gyT")
    brb = P.sb([128, 4, 256], BF16); b_brb = P.buf("brb")
    brT = P.sb([128, 8, 128], BF16); b_brT = P.buf("brT")
    m_sb = P.sb([128, DM], F32); b_m = P.buf("m")
    tmp = P.sb([128, DM], F32); b_tmp = P.buf("tmp")
    mb = P.sb([128, DM], BF16); b_mb = P.buf("mb")
    mT = P.sb([128, 8, 128], BF16); b_mT = P.buf("mT")
    x1 = [P.sb([128, DM], F32) for _ in range(2)]
    b_x1 = [P.buf(f"x1{i}") for i in range(2)]
    h2 = [P.sb([128, DM], F32) for _ in range(2)]
    b_h2 = [P.buf(f"h2{i}") for i in range(2)]
    scr = P.sb([128, DM], F32); b_scr = P.buf("scr")
    st = P.sb([128, 8], F32); b_st = P.buf("st")
    hTf = P.sb([128, 8, 128], F32); b_hTf = P.buf("hTf")
    lg = P.sb([128, 4, 8], F32); b_lg = P.buf("lg")
    cmb = [P.sb([128, 8], F32) for _ in range(2)]
    b_cmb = [P.buf(f"cmb{i}") for i in range(2)]
    psT = P.ps([128, 8, 128], BF16); b_psT = P.buf("psT")
    psg = P.ps([128, 256], F32); b_psg = P.buf("psg")
    psb = [P.ps([128, 512], F32) for _ in range(2)]
    b_psb = [P.buf(f"psb{i}") for i in range(2)]
    psm = [P.ps([128, 512], F32) for _ in range(2)]
    b_psm = [P.buf(f"psm{i}") for i in range(2)]
    psTf = [P.ps([128, 4, 128], F32) for _ in range(2)]
    b_psTf = [P.buf(f"psTf{i}") for i in range(2)]
    for t in range(NT):
        j = t % 2
        cls = 0 if t < NTX else 1
        rows = slice(t * 128, (t + 1) * 128)
        xs = xsrc(t)
        posf = (2 + t) if t < NTX else (t - NTX)
        P.dma("sp", lambda e, j=j, xs=xs: e.dma_start(out=x_sb[j][:], in_=xs), b_x[j], "w")
        P.dma("sp", lambda e, j=j, rows=rows: e.dma_start(out=br_sb[j][:, 0, :], in_=oa_d[rows, :]), b_br[j], "w")
        P.dma("sp", lambda e, j=j, rows=rows: e.dma_start(out=br_sb[j][:, 1, :], in_=ob_d[rows, :]), b_br[j], "w")
        P.dma("sp", lambda e, j=j, rows=rows: e.dma_start(out=br_sb[j][:, 2, :], in_=od_d[rows, :]), b_br[j], "w")
        P.dma("sp", lambda e, j=j, rows=rows: e.dma_start(out=br_sb[j][:, 3, :], in_=zmix[rows, 1792:2048]), b_br[j], "w")
        P.dma("sp", lambda e, j=j, posf=posf: e.dma_start(out=yT_sb[j][:, 0, :, :], in_=yTf[:, :, posf * 128:(posf + 1) * 128]), b_yT[j], "w")
        P.dma("sp", lambda e, j=j, rows=rows: e.dma_start(out=yT_sb[j][:, 1, :, :], in_=yTb[:, :, rows]), b_yT[j], "w")
        for dd in range(2):
            for mm in range(2):
                P.op("pe", lambda e, dd=dd, mm=mm, j=j: e.transpose(out=psTf[0][:, dd * 2 + mm, :], in_=yT_sb[j][:, dd, mm, :], identity=ident_f[:]),
                     reads=[b_yT[j], b_id], writes=[b_psTf[0]])
        P.dma("sp", lambda e, j=j, rows=rows: e.dma_start(out=gt_sb[j][:], in_=gates[rows, :]), b_gt[j], "w")
        B = br_sb[j]
        P.op("dve", lambda e, B=B: e.tensor_tensor(out=yv[:], in0=B[:, 3, :], in1=drow[:], op=ALU.mult), reads=[b_br[j], b_wr], writes=[b_yv])
        P.op("dve", lambda e: e.tensor_tensor(out=yv[:], in0=yv[:], in1=psTf[0][:, 0:2, :].rearrange("p a c -> p (a c)"), op=ALU.add), reads=[b_psTf[0], b_yv], writes=[b_yv])
        P.op("dve", lambda e: e.tensor_tensor(out=yv[:], in0=yv[:], in1=psTf[0][:, 2:4, :].rearrange("p a c -> p (a c)"), op=ALU.add), reads=[b_psTf[0], b_yv], writes=[b_yv])
        gelu_ops(P, yv[:], gy[:], g1[:], g2[:], b_yv, b_g1, b_g2, b_gy)
        P.op("act", lambda e: e.copy(out=gyb[:], in_=gy[:]), reads=[b_gy], writes=[b_gyb])
        for k in range(2):
            P.op("pe", lambda e, k=k: e.transpose(out=psT[:, k, :], in_=gyb[:, k * 128:(k + 1) * 128], identity=ident[:]), reads=[b_gyb, b_id], writes=[b_psT])
        P.op("act", lambda e: e.copy(out=gyT[:], in_=psT[:, 0:2, :]), reads=[b_psT], writes=[b_gyT])
        for k in range(2):
            P.op("pe", lambda e, k=k: e.matmul(psg[:], lhsT=gyT[:, k, :], rhs=wg[:, k, :], start=(k == 0), stop=(k == 1)), reads=[b_gyT, b_wt], writes=[b_psg])
        P.op("dve", lambda e: e.tensor_tensor(out=g1[:], in0=psg[:], in1=gbrow[:], op=ALU.add), reads=[b_psg, b_wr], writes=[b_g1])
        P.op("act", lambda e: e.activation(out=g2[:], in_=g1[:], func=AF.Sigmoid), reads=[b_g1], writes=[b_g2])
        P.op("dve", lambda e: e.tensor_tensor(out=brb[:, 2, :], in0=gy[:], in1=g2[:], op=ALU.mult), reads=[b_gy, b_g2], writes=[b_brb])
        P.op("act", lambda e, B=B: e.copy(out=brb[:, 0:2, :], in_=B[:, 0:2, :]), reads=[b_br[j], b_brb], writes=[b_brb])
        P.op("act", lambda e, B=B: e.copy(out=brb[:, 3, :], in_=B[:, 2, :]), reads=[b_br[j], b_brb], writes=[b_brb])
        for i in range(4):
            for k in range(2):
                P.op("pe", lambda e, i=i, k=k: e.transpose(out=psT[:, i * 2 + k, :], in_=brb[:, i, k * 128:(k + 1) * 128], identity=ident[:]),
                     reads=[b_brb, b_id], writes=[b_psT])
        P.op("act", lambda e: e.copy(out=brT[:], in_=psT[:]), reads=[b_psT], writes=[b_brT])
        for i in range(4):
            for hf in range(2):
                for k in range(2):
                    P.op("pe", lambda e, i=i, hf=hf, k=k: e.matmul(psb[hf][:], lhsT=brT[:, i * 2 + k, :], rhs=wbr[:, i * 2 + k, hf * 512:(hf + 1) * 512],
                                                                   start=(k == 0), stop=(k == 1)),
                         reads=[b_brT, b_wt], writes=[b_psb[hf]])
                gap = gt_sb[j][:, i * DM + hf * 512:i * DM + (hf + 1) * 512]
                if i == 0:
                    P.op("dve", lambda e, hf=hf, gap=gap: e.tensor_tensor(out=m_sb[:, hf * 512:(hf + 1) * 512], in0=psb[hf][:], in1=gap, op=ALU.mult),
                         reads=[b_psb[hf], b_gt[j]], writes=[b_m])
                else:
                    P.op("dve", lambda e, hf=hf, gap=gap: e.tensor_tensor(out=tmp[:, hf * 512:(hf + 1) * 512], in0=psb[hf][:], in1=gap, op=ALU.mult),
                         reads=[b_psb[hf], b_gt[j]], writes=[b_tmp])
                    P.op("pool", lambda e, hf=hf: e.tensor_tensor(out=m_sb[:, hf * 512:(hf + 1) * 512], in0=m_sb[:, hf * 512:(hf + 1) * 512],
                                                                  in1=tmp[:, hf * 512:(hf + 1) * 512], op=ALU.add),
                         reads=[b_tmp, b_m], writes=[b_m])
        P.op("act", lambda e: e.copy(out=mb[:], in_=m_sb[:]), reads=[b_m], writes=[b_mb])
        for k in range(8):
            P.op("pe", lambda e, k=k: e.transpose(out=psT[:, k, :], in_=mb[:, k * 128:(k + 1) * 128], identity=ident[:]), reads=[b_mb, b_id], writes=[b_psT])
        P.op("act", lambda e: e.copy(out=mT[:], in_=psT[:]), reads=[b_psT], writes=[b_mT])
        for hf in range(2):
            for k in range(8):
                P.op("pe", lambda e, hf=hf, k=k: e.matmul(psm[hf][:], lhsT=mT[:, k, :], rhs=wo[:, k, hf * 512:(hf + 1) * 512], start=(k == 0), stop=(k == 7)),
                     reads=[b_mT, b_wt], writes=[b_psm[hf]])
        for hf in range(2):
            P.op("act", lambda e, hf=hf: e.activation(out=scr[:, hf * 512:(hf + 1) * 512], in_=psm[hf][:], func=AF.Square, accum_out=st[:, hf:hf + 1]),
                 reads=[b_psm[hf]], writes=[b_scr, b_st])
        P.op("dve", lambda e: e.tensor_tensor(out=st[:, 2:3], in0=st[:, 0:1], in1=st[:, 1:2], op=ALU.add), reads=[b_st], writes=[b_st])
        P.op("dve", lambda e: e.tensor_scalar(out=st[:, 2:3], in0=st[:, 2:3], scalar1=1.0 / DM, scalar2=EPS, op0=ALU.mult, op1=ALU.add), reads=[b_st], writes=[b_st])
        P.op("act", lambda e: e.activation(out=st[:, 2:3], in_=st[:, 2:3], func=AF.Sqrt), reads=[b_st], writes=[b_st])
        P.op("dve", lambda e: e.reciprocal(out=st[:, 3:4], in_=st[:, 2:3]), reads=[b_st], writes=[b_st])
        for hf in range(2):
            P.op("dve", lambda e, hf=hf, cls=cls: e.scalar_tensor_tensor(out=tmp[:, hf * 512:(hf + 1) * 512], in0=psm[hf][:], scalar=st[:, 3:4],
                                                                       in1=G2[cls][:, hf * 512:(hf + 1) * 512], op0=ALU.mult, op1=ALU.mult),
                 reads=[b_psm[hf], b_st, b_G[cls]], writes=[b_tmp])
        P.op("pool", lambda e, j=j: e.tensor_tensor(out=x1[j][:], in0=tmp[:], in1=x_sb[j][:], op=ALU.add), reads=[b_tmp, b_x[j]], writes=[b_x1[j]])
        P.dma("sp", lambda e, j=j, rows=rows: e.dma_start(out=x1o[rows, :], in_=x1[j][:]), b_x1[j], "r")
        rms_rstd(P, x1[j][:], scr[:], st[:, 4:5], st[:, 5:6], b_x1[j], b_scr, b_st)
        P.op("dve", lambda e, j=j, cls=cls: e.scalar_tensor_tensor(out=tmp[:], in0=x1[j][:], scalar=st[:, 5:6], in1=G4[cls][:], op0=ALU.mult, op1=ALU.mult),
             reads=[b_x1[j], b_st, b_G[cls]], writes=[b_tmp])
        P.op("pool", lambda e, j=j, cls=cls: e.tensor_tensor(out=h2[j][:], in0=tmp[:], in1=SH3[cls][:], op=ALU.add), reads=[b_tmp, b_G[cls]], writes=[b_h2[j]])
        P.op("act", lambda e, j=j: e.copy(out=h2b[:], in_=h2[j][:]), reads=[b_h2[j]], writes=[b_h2b])
        for k in range(8):
            P.op("pe", lambda e, k=k: e.transpose(out=psT[:, k, :], in_=h2b[:, k * 128:(k + 1) * 128], identity=ident[:]), reads=[b_h2b, b_id], writes=[b_psT])
        P.op("act", lambda e, j=j: e.copy(out=hTs[j][:], in_=psT[:]), reads=[b_psT], writes=[b_hTs[j]])
        P.dma("sp", lambda e, j=j, rows=rows: e.dma_start(out=hTo[:, :, rows], in_=hTs[j][:]), b_hTs[j], "r")
        if moe:
            for r in range(2):
                for k in range(4):
                    kk = r * 4 + k
                    P.op("pe", lambda e, r=r, k=k, kk=kk, j=j: e.transpose(out=psTf[r][:, k, :], in_=h2[j][:, kk * 128:(kk + 1) * 128], identity=ident_f[:]),
                         reads=[b_h2[j], b_id], writes=[b_psTf[r]])
                P.op("act", lambda e, r=r: e.copy(out=hTf[:, r * 4:(r + 1) * 4, :], in_=psTf[r][:]), reads=[b_psTf[r]], writes=[b_hTf])
            for k in range(8):
                P.op("pe", lambda e, k=k: e.matmul(psg[:, 0:8], lhsT=hTf[:, k, :], rhs=rt_sb[:, k, :], start=(k == 0), stop=(k == 7)),
                     reads=[b_hTf, b_wr], writes=[b_psg])
            L0, M1, L2, M2 = lg[:, 0, :], lg[:, 1, :], lg[:, 2, :], lg[:, 3, :]
            sa, sb_, sc, sd = st[:, 6:7], st[:, 7:8], st[:, 0:1], st[:, 1:2]
            P.op("dve", lambda e: e.tensor_copy(out=L0, in_=psg[:, 0:8]), reads=[b_psg], writes=[b_lg])
            P.op("dve", lambda e: e.reduce_max(out=sa, in_=L0, axis=AX.X), reads=[b_lg], writes=[b_st])
            P.op("dve", lambda e: e.tensor_scalar(out=M1, in0=L0, scalar1=sa, scalar2=None, op0=ALU.is_equal), reads=[b_lg, b_st], writes=[b_lg])
            P.op("dve", lambda e: e.scalar_tensor_tensor(out=L2, in0=M1, scalar=-1e30, in1=L0, op0=ALU.mult, op1=ALU.add), reads=[b_lg], writes=[b_lg])
            P.op("dve", lambda e: e.reduce_max(out=sb_, in_=L2, axis=AX.X), reads=[b_lg], writes=[b_st])
            P.op("dve", lambda e: e.tensor_scalar(out=M2, in0=L2, scalar1=sb_, scalar2=None, op0=ALU.is_equal), reads=[b_lg, b_st], writes=[b_lg])
            P.op("dve", lambda e: e.tensor_tensor(out=sc, in0=sb_, in1=sa, op=ALU.subtract), reads=[b_st], writes=[b_st])
            P.op("act", lambda e: e.activation(out=sc, in_=sc, func=AF.Exp), reads=[b_st], writes=[b_st])
            P.op("dve", lambda e: e.tensor_scalar(out=sd, in0=sc, scalar1=1.0, scalar2=None, op0=ALU.add), reads=[b_st], writes=[b_st])
            P.op("dve", lambda e: e.reciprocal(out=sd, in_=sd), reads=[b_st], writes=[b_st])
            P.op("dve", lambda e: e.tensor_tensor(out=sc, in0=sc, in1=sd, op=ALU.mult), reads=[b_st], writes=[b_st])
            P.op("dve", lambda e: e.tensor_scalar(out=M2, in0=M2, scalar1=sc, scalar2=None, op0=ALU.mult), reads=[b_lg, b_st], writes=[b_lg])
            P.op("dve", lambda e, j=j: e.scalar_tensor_tensor(out=cmb[j][:], in0=M1, scalar=sd, in1=M2, op0=ALU.mult, op1=ALU.add),
                 reads=[b_lg, b_st], writes=[b_cmb[j]])
        else:
            P.op("pool", lambda e, j=j: e.memset(cmb[j][:], 1.0), writes=[b_cmb[j]])
        P.dma("sp", lambda e, j=j, rows=rows: e.dma_start(out=combo[rows, :], in_=cmb[j][:]), b_cmb[j], "r")
    P.emit()


HXQ = 'pool'


def stage_p3b(nc, D, l, NT, E, FCH, w1, w3, w2, xo, sel, TBT=8):
    NTX = NTXA
    hT = D["hT_d"]
    x1d, combd = D["x1_d"], D["comb_d"]
    mod = D["mod_d"][l]
    gpost = D["g_post_ffn"][l]
    hfv = D["hfv"]
    OFFB = HALF
    P = Prog(nc, f"p3b{l}_")
    hf_sb = P.sb([128, 2], F32)
    b_hf = P.buf("hf")
    P.dma("sp", lambda e: e.dma_start(out=hf_sb[:], in_=hfv), b_hf, "w")
    stg = [P.sb([128, DM], BF16) for _ in range(2)]
    b_stg = [P.buf(f"stg{i}") for i in range(2)]
    cmbB = P.sb([128, TBT, 8], F32)
    b_cmbB = P.buf("cmbB")
    grow = P.sb([128, DM], F32)
    b_grow = P.buf("grow")
    P.dma("sp", lambda e: e.dma_start(out=grow[:], in_=gpost.partition_broadcast(128)), b_grow, "w")
    G5 = [P.sb([128, DM], F32) for _ in range(2)]
    b_G5 = [P.buf(f"G5{c}") for c in range(2)]
    for c in range(2):
        P.dma("sp", lambda e, c=c: e.dma_start(out=G5[c][:], in_=mod[c, 5 * DM:6 * DM].partition_broadcast(128)), b_G5[c], "w")
        P.op("dve", lambda e, c=c: e.tensor_tensor(out=G5[c][:], in0=G5[c][:], in1=grow[:], op=ALU.mult), reads=[b_G5[c], b_grow], writes=[b_G5[c]])
    TBK = TBT * 128
    hx = P.sb([128, 8, TBK], BF16)
    b_hxk = [P.buf(f"hx{k}") for k in range(8)]
    aT = P.sb([128, FCH, TBK], BF16)
    b_aT = [P.buf(f"aT{f}") for f in range(FCH)]
    w2_sb = P.sb([128, FCH, DM], BF16); b_w2 = P.buf("w2")
    FG = 2
    w1g = [P.sb([128, 8, FG * 128], BF16) for _ in range(2)]
    w3g = [P.sb([128, 8, FG * 128], BF16) for _ in range(2)]
    b_w1g = [P.buf(f"w1g{i}") for i in range(2)]
    b_w3g = [P.buf(f"w3g{i}") for i in range(2)]
    yacc = P.sb([128, TBT, DM], F32)
    b_yacc = [P.buf(f"yacc{i}") for i in range(TBT)]
    cmb = P.sb([128, TBT, 8], F32); b_cmb = P.buf("cmb")
    s_sb = [P.sb([128, 512], BF16) for _ in range(2)]
    b_s = [P.buf(f"s{i}") for i in range(2)]
    x1_sb = [P.sb([128, DM], F32) for _ in range(2)]
    b_x1 = [P.buf(f"x1{i}") for i in range(2)]
    scr = P.sb([128, DM], F32); b_scr = P.buf("scr")
    stgf = scr
    b_stgf = b_scr
    st = [P.sb([128, 2], F32) for _ in range(2)]
    b_st = [P.buf(f"st{i}") for i in range(2)]
    ps1 = [P.ps([128, 512], F32) for _ in range(2)]
    ps3 = [P.ps([128, 512], F32) for _ in range(2)]
    b_ps1 = [P.buf(f"ps1{i}") for i in range(2)]
    b_ps3 = [P.buf(f"ps3{i}") for i in range(2)]
    psy = [P.ps([128, 512], F32) for _ in range(2)]
    b_psy = [P.buf(f"psy{i}") for i in range(2)]
    it1 = 0
    it2 = 0
    itg = 0
    ito = 0
    for tb0 in range(0, NT, TBT):
        ntile = min(TBT, NT - tb0)
        ntok = ntile * 128
        tok0 = tb0 * 128
        for k in range(8):
            P.dma(HXQ, lambda e, k=k, ntok=ntok, tok0=tok0: e.dma_start(out=hx[:, k, 0:ntok], in_=hT[k * 128:(k + 1) * 128, tok0:tok0 + ntok]), b_hxk[k], "w")
            if sel:
                sg = k % 2
                P.dma(HXQ, lambda e, k=k, ntok=ntok, tok0=tok0, sg=sg: e.dma_start(out=stg[sg][:, 0:ntok], in_=hT[k * 128:(k + 1) * 128, OFFB + tok0:OFFB + tok0 + ntok]),
                      b_stg[sg], "w")
                P.op("dve", lambda e, sg=sg, ntok=ntok: e.tensor_scalar(out=stg[sg][:, 0:ntok], in0=stg[sg][:, 0:ntok], scalar1=hf_sb[:, 0:1], scalar2=None, op0=ALU.mult),
                     reads=[b_stg[sg], b_hf], writes=[b_stg[sg]])
                P.op("dve", lambda e, k=k, sg=sg, ntok=ntok: e.scalar_tensor_tensor(out=hx[:, k, 0:ntok], in0=hx[:, k, 0:ntok], scalar=hf_sb[:, 1:2], in1=stg[sg][:, 0:ntok],
                                                                                 op0=ALU.mult, op1=ALU.add),
                     reads=[b_hxk[k], b_stg[sg], b_hf], writes=[b_hxk[k]])
        P.dma("sp", lambda e, ntile=ntile, tok0=tok0, ntok=ntok: e.dma_start(out=cmb[:, 0:ntile, :],
                                                                  in_=combd[tok0:tok0 + ntok, :].rearrange("(t p) e -> p t e", p=128)), b_cmb, "w")
        if sel:
            P.dma("sp", lambda e, ntile=ntile, tok0=tok0, ntok=ntok: e.dma_start(out=cmbB[:, 0:ntile, :],
                                                                      in_=combd[OFFB + tok0:OFFB + tok0 + ntok, :].rearrange("(t p) e -> p t e", p=128)), b_cmbB, "w")
            P.op("dve", lambda e, ntile=ntile: e.tensor_scalar(out=cmbB[:, 0:ntile, :], in0=cmbB[:, 0:ntile, :], scalar1=hf_sb[:, 0:1], scalar2=None, op0=ALU.mult),
                 reads=[b_cmbB, b_hf], writes=[b_cmbB])
            P.op("dve", lambda e, ntile=ntile: e.scalar_tensor_tensor(out=cmb[:, 0:ntile, :], in0=cmb[:, 0:ntile, :], scalar=hf_sb[:, 1:2], in1=cmbB[:, 0:ntile, :],
                                                                    op0=ALU.mult, op1=ALU.add),
                 reads=[b_cmb, b_cmbB, b_hf], writes=[b_cmb])
        subs = [(s0, min(512, ntok - s0)) for s0 in range(0, ntok, 512)]
        for ex in range(E):
            for f0 in range(0, FCH, 4):
                f1 = min(FCH, f0 + 4)
                P.dma("pool", lambda e, ex=ex, f0=f0, f1=f1: e.dma_start(out=w2_sb[:, f0:f1, :],
                                                                    in_=w2[ex, f0 * 128:f1 * 128, :].rearrange("(f p) n -> p f n", p=128)), b_w2, "w")
            for f0 in range(0, FCH, FG):
                f1 = min(FCH, f0 + FG)
                gi = itg % 2
                itg += 1
                wcols = (f1 - f0) * 128
                P.dma("pool", lambda e, ex=ex, f0=f0, wcols=wcols, gi=gi: e.dma_start(out=w1g[gi][:, :, 0:wcols],
                                                                            in_=w1[ex, :, f0 * 128:f0 * 128 + wcols].rearrange("(k p) n -> p k n", p=128)), b_w1g[gi], "w")
                P.dma("pool", lambda e, ex=ex, f0=f0, wcols=wcols, gi=gi: e.dma_start(out=w3g[gi][:, :, 0:wcols],
                                                                            in_=w3[ex, :, f0 * 128:f0 * 128 + wcols].rearrange("(k p) n -> p k n", p=128)), b_w3g[gi], "w")
                for f in range(f0, f1):
                    fo = (f - f0) * 128
                    for (s0, sl) in subs:
                        q = it1 % 2
                        it1 += 1
                        for k in range(8):
                            P.op("pe", lambda e, q=q, k=k, gi=gi, fo=fo, s0=s0, sl=sl: e.matmul(ps1[q][:, 0:sl], lhsT=w1g[gi][:, k, fo:fo + 128], rhs=hx[:, k, s0:s0 + sl],
                                                                                             start=(k == 0), stop=(k == 7)),
                                 reads=[b_w1g[gi], b_hxk[k]], writes=[b_ps1[q]])
                        for k in range(8):
                            P.op("pe", lambda e, q=q, k=k, gi=gi, fo=fo, s0=s0, sl=sl: e.matmul(ps3[q][:, 0:sl], lhsT=w3g[gi][:, k, fo:fo + 128], rhs=hx[:, k, s0:s0 + sl],
                                                                                             start=(k == 0), stop=(k == 7)),
                                 reads=[b_w3g[gi], b_hxk[k]], writes=[b_ps3[q]])
                        P.op("act", lambda e, q=q, sl=sl: e.activation(out=s_sb[q][:, 0:sl], in_=ps1[q][:, 0:sl], func=AF.Silu), reads=[b_ps1[q]], writes=[b_s[q]])
                        P.op("dve", lambda e, q=q, sl=sl, f=f, s0=s0: e.tensor_tensor(out=aT[:, f, s0:s0 + sl], in0=ps3[q][:, 0:sl], in1=s_sb[q][:, 0:sl], op=ALU.mult),
                             reads=[b_ps3[q], b_s[q]], writes=[b_aT[f]])
            for tl in range(ntile):
                for hf in range(2):
                    q = it2 % 2
                    it2 += 1
                    for f in range(FCH):
                        P.op("pe", lambda e, q=q, f=f, tl=tl, hf=hf: e.matmul(psy[q][:], lhsT=aT[:, f, tl * 128:(tl + 1) * 128], rhs=w2_sb[:, f, hf * 512:(hf + 1) * 512],
                                                                             start=(f == 0), stop=(f == FCH - 1)),
                             reads=[b_aT[f], b_w2], writes=[b_psy[q]])
                    ya = yacc[:, tl, hf * 512:(hf + 1) * 512]
                    if ex == 0:
                        P.op("dve", lambda e, q=q, ya=ya, tl=tl: e.tensor_scalar(out=ya, in0=psy[q][:], scalar1=cmb[:, tl, 0:1], scalar2=None, op0=ALU.mult),
                             reads=[b_psy[q], b_cmb], writes=[b_yacc[tl]])
                    else:
                        P.op("dve", lambda e, q=q, ya=ya, tl=tl, ex=ex: e.scalar_tensor_tensor(out=ya, in0=psy[q][:], scalar=cmb[:, tl, ex:ex + 1], in1=ya,
                                                                                            op0=ALU.mult, op1=ALU.add),
                             reads=[b_psy[q], b_cmb, b_yacc[tl]], writes=[b_yacc[tl]])
        for tl in range(ntile):
            t = tb0 + tl
            cls = 0 if t < NTX else 1
            j = ito % 2
            ito += 1
            rows = slice(t * 128, (t + 1) * 128)
            P.dma("sp", lambda e, j=j, rows=rows: e.dma_start(out=x1_sb[j][:], in_=x1d[rows, :]), b_x1[j], "w")
            if sel:
                rowsB = slice(OFFB + t * 128, OFFB + (t + 1) * 128)
                P.dma("sp", lambda e, rowsB=rowsB: e.dma_start(out=stgf[:], in_=x1d[rowsB, :]), b_stgf, "w")
                P.op("dve", lambda e: e.tensor_scalar(out=stgf[:], in0=stgf[:], scalar1=hf_sb[:, 0:1], scalar2=None, op0=ALU.mult), reads=[b_stgf, b_hf], writes=[b_stgf])
                P.op("dve", lambda e, j=j: e.scalar_tensor_tensor(out=x1_sb[j][:], in0=x1_sb[j][:], scalar=hf_sb[:, 1:2], in1=stgf[:], op0=ALU.mult, op1=ALU.add),
                     reads=[b_x1[j], b_stgf, b_hf], writes=[b_x1[j]])
            rms_rstd(P, yacc[:, tl, :], scr[:], st[j][:, 0:1], st[j][:, 1:2], b_yacc[tl], b_scr, b_st[j])
            P.op("dve", lambda e, j=j, tl=tl, cls=cls: e.scalar_tensor_tensor(out=scr[:], in0=yacc[:, tl, :], scalar=st[j][:, 1:2], in1=G5[cls][:],
                                                                            op0=ALU.mult, op1=ALU.mult),
                 reads=[b_yacc[tl], b_st[j], b_G5[cls]], writes=[b_scr])
            P.op("pool", lambda e, j=j: e.tensor_tensor(out=x1_sb[j][:], in0=scr[:], in1=x1_sb[j][:], op=ALU.add), reads=[b_scr, b_x1[j]], writes=[b_x1[j]])
            P.dma("sp", lambda e, j=j, rows=rows: e.dma_start(out=xo[rows, :], in_=x1_sb[j][:]), b_x1[j], "r")
    P.emit()


_DEBUG_ARGS = ()


def build_fused(debug=False, stop_after=None):
    nc = bass.Bass("TRN2", target_bir_lowering=False)
    D = {}

    declared = set()
    D["_inputs"] = declared

    def inp(name, shape, dt=F32):
        if stop_after is not None and name.startswith("moe_w"):
            return
        declared.add(name)
        D[name] = nc.dram_tensor(name, list(shape), dt, kind="ExternalInput").ap()

    dbg_names = ("zmix_d", "ob_d", "oa_d", "od_d", "yTf_d", "yTb_d", "x1_d", "xcur_d", "qTa_d", "qTd_d", "kTd_d", "comb_d")

    big_scratch = ("zmix_d", "gates_d", "x1_d", "xcur_d", "hT_d")

    def scratch(name, shape, dt=F32):
        if name in big_scratch or (debug and name in dbg_names):
            D[name] = nc.dram_tensor(name, list(shape), dt, kind="ExternalOutput").ap()
        else:
            D[name] = nc.dram_tensor(name, list(shape), dt).ap()

    inp("x_b", [SEQ, DM]); inp("ctx_b", [CTX, DM]); inp("cT", [128, 8, 2]); inp("hfv", [128, 2])
    inp("w_mod", [2, DM, 6 * DM]); inp("b_mod", [2, 6 * DM])
    inp("g_pre_mix", [2, DM]); inp("gvec", [2, 2, DM]); inp("g_post_ffn", [2, DM])
    inp("w_in", [2, DM, 6144])
    inp("ropeT", [64, 2, 4, T_ALL])
    inp("gm_ln", [2, 2, 256]); inp("gm_wsT", [2, 128, 4, 128]); inp("gm_bsT", [2, 128, 4])
    inp("na_tabI", [2, 128, 4, 640]); inp("na_tabE", [2, 128, 16, 896]); inp("swa_tab", [128, 3, 384]); inp("swa_sink", [2, 4])
    inp("s5_prm", [2, 128, 3, 16]); inp("s5_bmat", [2, 128, 2, 16, 16]); inp("s5_cmat", [2, 128, 2, 16, 16])
    inp("s5v", [2, 2, 256]); inp("s5_glu_w", [2, 256, 256])
    inp("w_branch", [2, 1024, DM]); inp("w_out", [2, DM, DM])
    inp("ffn_w1", [1, DM, 2816]); inp("ffn_w3", [1, DM, 2816]); inp("ffn_w2", [1, 2816, DM])
    inp("moe_router", [DM, 8]); inp("moe_w1", [8, DM, 3584]); inp("moe_w3", [8, DM, 3584]); inp("moe_w2", [8, 3584, DM])
    D["out"] = nc.dram_tensor("out", [HALF, DM], F32, kind="ExternalOutput").ap()
    scratch("mod_d", [2, 2, 6 * DM])
    scratch("zmix_d", [T_ALL, 2048]); scratch("gates_d", [T_ALL, 4096], BF16); scratch("ob_d", [T_ALL, 256])
    scratch("qTa_d", [64, 4, T_ALL], BF16); scratch("kTa_d", [64, 4, T_ALL], BF16)
    scratch("qTd_d", [64, 4, T_ALL], BF16); scratch("kTd_d", [64, 2, T_ALL], BF16)
    scratch("suTf_d", [256, T_ALL]); scratch("suTb_d", [256, T_ALL])
    scratch("yTf_d", [256, T_ALL]); scratch("yTb_d", [256, T_ALL])
    scratch("oa_d", [T_ALL, 256]); scratch("od_d", [T_ALL, 256])
    scratch("x1_d", [T_ALL, DM]); scratch("hT_d", [DM, T_ALL], BF16); scratch("comb_d", [T_ALL, 8])
    scratch("xcur_d", [T_ALL, DM])
    stage_p0(nc, D)
    for l in range(2):
        ctx_out = (l == 0)
        if l == 0:
            xsrc = lambda t: (D["x_b"][t * 128:(t + 1) * 128, :] if t < NTXA else D["ctx_b"][(t - NTXA) * 128:(t - NTXA + 1) * 128, :])
        else:
            xsrc = lambda t: D["xcur_d"][t * 128:(t + 1) * 128, :]
        stage_p1(nc, D, l, xsrc)
        if stop_after == ("p1", l):
            return nc, declared
        stage_attn(nc, D, l, "na", ctx_out)
        stage_attn(nc, D, l, "swa", ctx_out)
        stage_s5(nc, D, l)
        if stop_after == ("mix", l):
            return nc, declared
        if l == 0:
            stage_p3a(nc, D, l, xsrc, NT_ALL, False)
            stage_p3b(nc, D, l, NT_ALL, 1, 22, D["ffn_w1"], D["ffn_w3"], D["ffn_w2"], D["xcur_d"], False)
            if stop_after == ("ffn", l):
                return nc, declared
        else:
            stage_p3a(nc, D, l, xsrc, NTXA, True)
            if stop_after == ("p3a", l):
                return nc, declared
            stage_p3b(nc, D, l, 32, 8, 28, D["moe_w1"], D["moe_w3"], D["moe_w2"], D["out"], True)
    return nc, declared


def kernel(x, c, ctx, c_ctx, w_mod, b_mod, g_pre_mix, g_post_mix, g_pre_ffn, g_post_ffn, w_in,
           na_rpb, swa_sink, gmlp_ln_g, gmlp_ln_b, gmlp_ws, gmlp_bs,
           s5_a_re, s5_a_im, s5_log_dt, s5_b_re, s5_b_im, s5_c_re, s5_c_im, s5_d, s5_glu_w, s5_glu_b,
           w_branch, w_out, ffn_w1, ffn_w3, ffn_w2, moe_router, moe_w1, moe_w3, moe_w2):
    A = lambda a: np.ascontiguousarray(np.asarray(a, dtype=np.float32))
    x = A(x); ctx = A(ctx); c = A(c); c_ctx = A(c_ctx)
    nc, names = build_fused(*_DEBUG_ARGS)
    tabI = np.stack([np.ascontiguousarray(na_table(A(na_rpb[l]), 68, 64, 10).transpose(1, 0, 2)) for l in range(2)])
    tabE = np.stack([np.ascontiguousarray(np.stack([na_table(A(na_rpb[l]), 2 * i, 2 * i - 6, 14) for i in (0, 1, 62, 63)], axis=0)
                                          .reshape(16, 128, 896).transpose(1, 0, 2)) for l in range(2)])
    swa_tab = np.ascontiguousarray(np.stack([swa_table(0), swa_table(1), swa_table(63)], axis=0).transpose(1, 0, 2))
    s5p = [s5_params_host(A(s5_a_re[l]), A(s5_a_im[l]), A(s5_log_dt[l]), A(s5_b_re[l]), A(s5_b_im[l]), A(s5_c_re[l]), A(s5_c_im[l])) for l in range(2)]
    shared = {
        "w_mod": A(w_mod), "b_mod": A(b_mod), "g_pre_mix": A(g_pre_mix),
        "gvec": A(np.stack([g_post_mix, g_pre_ffn], axis=1)), "g_post_ffn": A(g_post_ffn),
        "w_in": A(w_in), "ropeT": rope_table_fm(),
        "gm_ln": A(np.stack([gmlp_ln_g, gmlp_ln_b], axis=1)),
        "gm_wsT": A(np.transpose(np.asarray(gmlp_ws), (0, 3, 1, 2))), "gm_bsT": A(np.transpose(np.asarray(gmlp_bs), (0, 2, 1))),
        "na_tabI": A(tabI), "na_tabE": A(tabE), "swa_tab": swa_tab, "swa_sink": A(swa_sink),
        "s5_prm": A(np.stack([p[0] for p in s5p])), "s5_bmat": A(np.stack([p[1] for p in s5p])), "s5_cmat": A(np.stack([p[2] for p in s5p])),
        "s5v": A(np.stack([s5_d, s5_glu_b], axis=1)), "s5_glu_w": A(s5_glu_w),
        "w_branch": A(np.reshape(np.asarray(w_branch), (2, 1024, DM))), "w_out": A(w_out),
        "ffn_w1": A(ffn_w1), "ffn_w3": A(ffn_w3), "ffn_w2": A(ffn_w2),
        "moe_router": A(moe_router[0]), "moe_w1": A(moe_w1[0]), "moe_w3": A(moe_w3[0]), "moe_w2": A(moe_w2[0]),
    }
    in_maps = []
    for core in range(NCORES):
        b, hf = core // 2, core % 2
        cc = np.stack([c[b], c_ctx], axis=-1)
        cT = np.ascontiguousarray(cc.reshape(8, 128, 2).transpose(1, 0, 2))
        hfv = np.zeros((128, 2), np.float32)
        hfv[:, 0] = float(hf)
        hfv[:, 1] = 1.0 - float(hf)
        m = dict(shared)
        m.update({"x_b": x[b], "ctx_b": ctx[b], "cT": cT, "hfv": hfv})
        in_maps.append({k: v for k, v in m.items() if k in names})
    res = _run(nc, in_maps)
    if _DEBUG_ARGS:
        return res
    out = np.zeros((BATCH, SEQ, DM), np.float32)
    for core in range(NCORES):
        b, hf = core // 2, core % 2
        out[b, hf * HALF:(hf + 1) * HALF] = np.asarray(res[core]["out"])
    return out
```
